# Optimizing a Trainium2 kernel written in Bass

```python
import numpy as np
import jax, jax.numpy as jnp
from jax import lax

D_MODEL = 1024
BATCH = 8
SEQ = 4096
DEPTH = 1

NSA_HEADS = 8
NSA_GROUPS = 2
NSA_HPG = NSA_HEADS // NSA_GROUPS
NSA_DH = 64
CMP_STRIDE = 16
CMP_LEN = 2 * CMP_STRIDE
CMP_HIDDEN = 128
SEL_BLOCK = 64
SEL_TOPK = 16
WINDOW = 512
NSA_QBLOCK = 64
FORCE_BONUS = 1e4
RET_HEADS = 8
RET_DK = 64
RET_DV = 64
RET_CHUNK = 128
ROPE_BASE = 10000.0
D_FF = 2816
EPS = 1e-6
NEG = -1e30

NSA_QW = NSA_HEADS * NSA_DH
NSA_KVW = NSA_GROUPS * NSA_DH
RET_QW = RET_HEADS * RET_DK
RET_VW = RET_HEADS * RET_DV
IN_WIDTHS = [NSA_QW, NSA_KVW, NSA_KVW, NSA_KVW, NSA_KVW, NSA_KVW, NSA_KVW, 3 * NSA_HEADS,
             RET_QW, RET_QW, RET_VW, RET_VW, D_MODEL, D_MODEL]
IN_W = sum(IN_WIDTHS)

kernel_name = "hybrid_nsa_retention_macaron_adaln"


def rmsnorm(x, w):
    xf = x.astype(jnp.float32)
    y = xf * lax.rsqrt(jnp.mean(xf * xf, axis=-1, keepdims=True) + EPS)
    return (y * w.astype(jnp.float32)).astype(x.dtype)


def modulate(h, shift, scale):
    return h * (1 + scale[:, None, :]) + shift[:, None, :]


def swiglu(h, w_in, w_out):
    a, b = jnp.split(h @ w_in, 2, axis=-1)
    return (jax.nn.silu(a) * b) @ w_out


def masked_softmax(s, mask):
    return jax.nn.softmax(jnp.where(mask, s.astype(jnp.float32), NEG), axis=-1)


def rope(x, pos):
    half = x.shape[-1] // 2
    inv = ROPE_BASE ** (-jnp.arange(half, dtype=jnp.float32) / half)
    ang = pos.astype(jnp.float32)[:, None] * inv[None, :]
    cos = jnp.cos(ang)[None, :, None, :]
    sin = jnp.sin(ang)[None, :, None, :]
    x1 = x[..., :half].astype(jnp.float32)
    x2 = x[..., half:].astype(jnp.float32)
    return jnp.concatenate([x1 * cos - x2 * sin, x1 * sin + x2 * cos], axis=-1).astype(x.dtype)


def compress(raw, pe, w1, w2):
    B, S, G, dh = raw.shape
    ch = raw.reshape(B, S // CMP_STRIDE, CMP_STRIDE, G, dh)
    blocks = jnp.concatenate([ch[:, :-1], ch[:, 1:]], axis=2)
    blocks = blocks + pe[None, None, :, None, :]
    nc = blocks.shape[1]
    flat = blocks.transpose(0, 1, 3, 2, 4).reshape(B, nc, G, CMP_LEN * dh)
    return jax.nn.silu(flat @ w1) @ w2


def nsa_attention(q, k_cmp, v_cmp, k_slc, v_slc, k_win, v_win, gates,
                  cmp_k_pe, cmp_k_w1, cmp_k_w2, cmp_v_pe, cmp_v_w1, cmp_v_w2):
    B, S = q.shape[:2]
    G, HPG, dh, QB = NSA_GROUPS, NSA_HPG, NSA_DH, NSA_QBLOCK
    kc = compress(k_cmp, cmp_k_pe, cmp_k_w1, cmp_k_w2)
    vc = compress(v_cmp, cmp_v_pe, cmp_v_w1, cmp_v_w2)
    nc = kc.shape[1]
    ns = S // SEL_BLOCK
    n_sel = min(SEL_TOPK, ns)
    cmp_end = jnp.arange(nc) * CMP_STRIDE + CMP_LEN - 1
    c_start = np.arange(nc) * CMP_STRIDE
    s_start = np.arange(ns) * SEL_BLOCK
    overlap = jnp.asarray((c_start[:, None] < s_start[None, :] + SEL_BLOCK) &
                          (c_start[:, None] + CMP_LEN > s_start[None, :]), dtype=jnp.float32)
    ks_blk = k_slc.reshape(B, ns, SEL_BLOCK, G, dh).transpose(0, 3, 1, 2, 4)
    vs_blk = v_slc.reshape(B, ns, SEL_BLOCK, G, dh).transpose(0, 3, 1, 2, 4)
    kw_pad = jnp.pad(k_win, ((0, 0), (WINDOW, 0), (0, 0), (0, 0)))
    vw_pad = jnp.pad(v_win, ((0, 0), (WINDOW, 0), (0, 0), (0, 0)))
    scale = NSA_DH ** -0.5
    b_ix = jnp.arange(B)[:, None, None, None]
    g_ix = jnp.arange(G)[None, :, None, None]
    j_blk = jnp.arange(ns)
    sel_off = jnp.arange(SEL_BLOCK)

    def block(nb):
        q0 = nb * QB
        t = q0 + jnp.arange(QB)
        qb = lax.dynamic_slice_in_dim(q, q0, QB, axis=1).reshape(B, QB, G, HPG, dh) * scale
        m_c = cmp_end[None, :] <= t[:, None]
        p_c = masked_softmax(jnp.einsum('bqghd,bngd->bghqn', qb, kc), m_c)
        p_c = p_c * jnp.any(m_c, axis=-1)[:, None].astype(jnp.float32)
        o_cmp = jnp.einsum('bghqn,bngd->bqghd', p_c, vc)
        imp = jnp.einsum('bghqn,nj->bgqj', p_c, overlap)
        cur = t // SEL_BLOCK
        forced = (j_blk[None, :] == 0) | (j_blk[None, :] == cur[:, None]) | (j_blk[None, :] == cur[:, None] - 1)
        valid = j_blk[None, :] * SEL_BLOCK <= t[:, None]
        imp = jnp.where(forced, imp + FORCE_BONUS, imp)
        imp = jnp.where(valid, imp, -FORCE_BONUS)
        _, idx = lax.top_k(imp, n_sel)
        kg = ks_blk[b_ix, g_ix, idx]
        vg = vs_blk[b_ix, g_ix, idx]
        kpos = idx[..., None] * SEL_BLOCK + sel_off
        m_s = (kpos <= t[None, None, :, None, None])[:, :, None]
        s_s = jnp.einsum('bqghd,bgqnld->bghqnl', qb, kg)
        s_s = jnp.where(m_s, s_s.astype(jnp.float32), NEG).reshape(B, G, HPG, QB, n_sel * SEL_BLOCK)
        p_s = jax.nn.softmax(s_s, axis=-1).reshape(B, G, HPG, QB, n_sel, SEL_BLOCK)
        o_slc = jnp.einsum('bghqnl,bgqnld->bqghd', p_s, vg)
        kw = lax.dynamic_slice_in_dim(kw_pad, q0, WINDOW + QB, axis=1)
        vw = lax.dynamic_slice_in_dim(vw_pad, q0, WINDOW + QB, axis=1)
        wpos = q0 - WINDOW + jnp.arange(WINDOW + QB)
        m_w = (wpos[None, :] <= t[:, None]) & (wpos[None, :] > t[:, None] - WINDOW) & (wpos[None, :] >= 0)
        p_w = masked_softmax(jnp.einsum('bqghd,bkgd->bghqk', qb, kw), m_w)
        o_win = jnp.einsum('bghqk,bkgd->bqghd', p_w, vw)
        g = lax.dynamic_slice_in_dim(gates, q0, QB, axis=1).reshape(B, QB, G, HPG, 3).astype(jnp.float32)
        o = g[..., 0:1] * o_cmp + g[..., 1:2] * o_slc + g[..., 2:3] * o_win
        return o.reshape(B, QB, G * HPG * dh).astype(q.dtype)

    out = lax.map(block, jnp.arange(S // QB))
    return out.transpose(1, 0, 2, 3).reshape(B, S, NSA_QW)


def retention(q, k, v, pos):
    B, S, H, dk = q.shape
    dv = v.shape[-1]
    C = RET_CHUNK
    n_chunk = S // C
    q = rope(q, pos)
    k = rope(k, pos) * (dk ** -0.5)
    log_g = jnp.log(1.0 - 2.0 ** (-5.0 - jnp.arange(H, dtype=jnp.float32)))
    i = jnp.arange(C, dtype=jnp.float32)
    diff = i[:, None] - i[None, :]
    decay_mat = jnp.where(diff >= 0, jnp.exp(log_g[:, None, None] * jnp.maximum(diff, 0.0)), 0.0)
    q_decay = jnp.exp(log_g[:, None] * (i + 1.0))[..., None]
    k_decay = jnp.exp(log_g[:, None] * (C - 1.0 - i))[..., None]
    chunk_decay = jnp.exp(log_g * C)[:, None, None]
    qc = q.reshape(B, n_chunk, C, H, dk).transpose(1, 0, 3, 2, 4)
    kc = k.reshape(B, n_chunk, C, H, dk).transpose(1, 0, 3, 2, 4)
    vc = v.reshape(B, n_chunk, C, H, dv).transpose(1, 0, 3, 2, 4)

    def step(state, inp):
        qi, ki, vi = inp
        inner = jnp.einsum('bhid,bhjd->bhij', qi, ki) * decay_mat
        o = jnp.einsum('bhij,bhjv->bhiv', inner, vi) + jnp.einsum('bhid,bhdv->bhiv', qi, state) * q_decay
        state = state * chunk_decay + jnp.einsum('bhjd,bhjv->bhdv', ki * k_decay, vi)
        return state, o

    state0 = jnp.zeros((B, H, dk, dv), jnp.float32)
    _, o = lax.scan(step, state0, (qc, kc, vc))
    return o.transpose(1, 0, 3, 2, 4).reshape(B, S, H, dv)


def setup_inputs(seed: int = 0) -> dict:
    key = jax.random.key(seed)
    ks = jax.random.split(key, 26)
    f32 = jnp.float32
    L, D = DEPTH, D_MODEL

    def nrm(k, shape, s):
        return jax.random.normal(k, shape, f32) * s

    return {
        "x": nrm(ks[0], (BATCH, SEQ, D), 1.0),
        "c": nrm(ks[1], (BATCH, D), 1.0),
        "ada_w": nrm(ks[2], (L, D, 9 * D), D ** -0.5),
        "ada_b": nrm(ks[3], (L, 9 * D), 0.01),
        "norm1_w": 1.0 + nrm(ks[4], (L, D), 0.02),
        "ffn1_w_in": nrm(ks[5], (L, D, 2 * D_FF), D ** -0.5),
        "ffn1_w_out": nrm(ks[6], (L, D_FF, D), D_FF ** -0.5),
        "norm2_w": 1.0 + nrm(ks[7], (L, D), 0.02),
        "w_in": nrm(ks[8], (L, D, IN_W), D ** -0.5),
        "cmp_k_pe": nrm(ks[9], (L, CMP_LEN, NSA_DH), 0.1),
        "cmp_k_w1": nrm(ks[10], (L, CMP_LEN * NSA_DH, CMP_HIDDEN), (CMP_LEN * NSA_DH) ** -0.5),
        "cmp_k_w2": nrm(ks[11], (L, CMP_HIDDEN, NSA_DH), CMP_HIDDEN ** -0.5),
        "cmp_v_pe": nrm(ks[12], (L, CMP_LEN, NSA_DH), 0.1),
        "cmp_v_w1": nrm(ks[13], (L, CMP_LEN * NSA_DH, CMP_HIDDEN), (CMP_LEN * NSA_DH) ** -0.5),
        "cmp_v_w2": nrm(ks[14], (L, CMP_HIDDEN, NSA_DH), CMP_HIDDEN ** -0.5),
        "ret_norm_w": 1.0 + nrm(ks[15], (L, RET_VW), 0.02),
        "w_nsa_up": nrm(ks[16], (L, NSA_QW, D), NSA_QW ** -0.5),
        "w_ret_up": nrm(ks[17], (L, RET_VW, D), RET_VW ** -0.5),
        "w_out": nrm(ks[18], (L, D, D), D ** -0.5),
        "norm3_w": 1.0 + nrm(ks[19], (L, D), 0.02),
        "ffn2_w_in": nrm(ks[20], (L, D, 2 * D_FF), D ** -0.5),
        "ffn2_w_out": nrm(ks[21], (L, D_FF, D), D_FF ** -0.5),
        "final_norm_w": 1.0 + nrm(ks[22], (D,), 0.02),
    }


def reference(x, c, ada_w, ada_b, norm1_w, ffn1_w_in, ffn1_w_out, norm2_w, w_in,
              cmp_k_pe, cmp_k_w1, cmp_k_w2, cmp_v_pe, cmp_v_w1, cmp_v_w2, ret_norm_w,
              w_nsa_up, w_ret_up, w_out, norm3_w, ffn2_w_in, ffn2_w_out, final_norm_w):
    B, S, D = x.shape
    pos = jnp.arange(S)
    split_pts = np.cumsum(IN_WIDTHS)[:-1].tolist()
    h = x
    for l in range(DEPTH):
        mod = jax.nn.silu(c) @ ada_w[l] + ada_b[l]
        sh1, sc1, gt1, sh2, sc2, gt2, sh3, sc3, gt3 = jnp.split(mod, 9, axis=-1)
        u = modulate(rmsnorm(h, norm1_w[l]), sh1, sc1)
        h = h + 0.5 * gt1[:, None, :] * swiglu(u, ffn1_w_in[l], ffn1_w_out[l])
        u = modulate(rmsnorm(h, norm2_w[l]), sh2, sc2)
        proj = u @ w_in[l]
        (nq, nkc, nvc, nks, nvs, nkw, nvw, ngt, rq, rk, rv, rg, ga, gb) = jnp.split(proj, split_pts, axis=-1)
        g4 = lambda a: a.reshape(B, S, NSA_GROUPS, NSA_DH)
        o_nsa = nsa_attention(nq.reshape(B, S, NSA_HEADS, NSA_DH), g4(nkc), g4(nvc), g4(nks), g4(nvs),
                              g4(nkw), g4(nvw), jax.nn.sigmoid(ngt).reshape(B, S, NSA_HEADS, 3),
                              cmp_k_pe[l], cmp_k_w1[l], cmp_k_w2[l], cmp_v_pe[l], cmp_v_w1[l], cmp_v_w2[l])
        o_ret = retention(rq.reshape(B, S, RET_HEADS, RET_DK), rk.reshape(B, S, RET_HEADS, RET_DK),
                          rv.reshape(B, S, RET_HEADS, RET_DV), pos)
        mu = jnp.mean(o_ret, axis=-1, keepdims=True)
        var = jnp.mean(jnp.square(o_ret - mu), axis=-1, keepdims=True)
        o_ret = ((o_ret - mu) * lax.rsqrt(var + EPS)).reshape(B, S, RET_VW) * ret_norm_w[l].astype(jnp.float32)
        o_ret = (o_ret * jax.nn.silu(rg.astype(jnp.float32))).astype(x.dtype)
        y_nsa = o_nsa @ w_nsa_up[l]
        y_ret = o_ret @ w_ret_up[l]
        mixed = jax.nn.sigmoid(ga) * y_nsa + jax.nn.sigmoid(gb) * y_ret
        h = h + gt2[:, None, :] * (mixed @ w_out[l])
        u = modulate(rmsnorm(h, norm3_w[l]), sh3, sc3)
        h = h + 0.5 * gt3[:, None, :] * swiglu(u, ffn2_w_in[l], ffn2_w_out[l])
    return rmsnorm(h, final_norm_w)
```

```python
import numpy as np
from contextlib import ExitStack
import concourse.bass as bass
import concourse.mybir as mybir
from concourse.bass_utils import run_bass_kernel_spmd

F32 = mybir.dt.float32
BF16 = mybir.dt.bfloat16
AF = mybir.ActivationFunctionType
ALU = mybir.AluOpType
AX = mybir.AxisListType

S = 4096
D = 1024
DFF = 2816
INW = 5400
NCH = 8
CT = 512
EPS = 1e-6
NEGM = -30000.0
ENGS = ["pe", "act", "dve", "pool", "sp"]

C_NQ, C_NKC, C_NVC, C_NKS, C_NVS, C_NKW, C_NVW, C_NGT, C_RQ, C_RK, C_RV, C_RG, C_GA, C_GB = (
    0, 512, 640, 768, 896, 1024, 1152, 1280, 1304, 1816, 2328, 2840, 3352, 4376)


class Buf:
    __slots__ = ("name", "last_w", "readers", "excl")

    def __init__(self, name, excl=False):
        self.name = name
        self.last_w = None
        self.readers = []
        self.excl = excl


class Op:
    __slots__ = ("eng", "fn", "deps", "signal", "val", "dma_key", "dma_val", "prog")

    def __init__(self, eng, fn):
        self.prog = None
        self.eng = eng
        self.fn = fn
        self.deps = []
        self.signal = False
        self.val = None
        self.dma_key = None
        self.dma_val = None


class Prog:
    def __init__(self, nc, name):
        self.nc = nc
        self.name = name
        self.ops = {e: [] for e in ENGS}
        self.dma_cnt = {}
        self.dma_last = {}

    def add(self, eng, fn, reads=(), writes=(), dma_key=None):
        op = Op(eng, fn)
        op.prog = self
        deps = []
        for b in reads:
            if b.last_w is not None:
                deps.append(b.last_w)
            if b.excl:
                deps.extend(b.readers)
        for b in writes:
            if b.last_w is not None:
                deps.append(b.last_w)
            deps.extend(b.readers)
        seen = set()
        for d in deps:
            if d is op or id(d) in seen or d.prog is not self:
                continue
            seen.add(id(d))
            if d.dma_key is None and d.eng == eng and eng == "pe":
                continue
            op.deps.append(d)
            if d.dma_key is None:
                d.signal = True
        if dma_key is not None:
            op.dma_key = dma_key
            self.dma_cnt[dma_key] = self.dma_cnt.get(dma_key, 0) + 16
            op.dma_val = self.dma_cnt[dma_key]
            self.dma_last[dma_key] = op
        for b in reads:
            b.readers.append(op)
        for b in writes:
            b.last_w = op
            b.readers = []
        self.ops[eng].append(op)
        return op

    def finish(self):
        op = Op("sp", lambda e: e.nop())
        op.deps = list(self.dma_last.values())
        self.ops["sp"].append(op)

    def emit(self, stack):
        nc = self.nc
        self.finish()
        esem = {e: stack.enter_context(nc.semaphore("s_%s_%s" % (self.name, e))) for e in ENGS}
        dsem = {k: stack.enter_context(nc.semaphore("d_%s_%s" % (self.name, str(k)))) for k in self.dma_cnt}
        for e in ENGS:
            c = 0
            for op in self.ops[e]:
                if op.dma_key is None and op.signal:
                    c += 1
                    op.val = c
        block = stack.enter_context(nc.Block())

        def run(e, engobj):
            waited = {}
            for op in self.ops[e]:
                for d in op.deps:
                    if d.dma_key is not None:
                        s, v = dsem[d.dma_key], d.dma_val
                    else:
                        s, v = esem[d.eng], d.val
                    key = id(s)
                    if waited.get(key, 0) >= v:
                        continue
                    waited[key] = v
                    engobj.wait_ge(s, v)
                ins = op.fn(engobj)
                if op.dma_key is not None:
                    ins.then_inc(dsem[op.dma_key], 16)
                elif op.signal:
                    ins.then_inc(esem[e], 1)

        @block.tensor
        def _(eng):
            run("pe", eng)

        @block.scalar
        def _(eng):
            run("act", eng)

        @block.vector
        def _(eng):
            run("dve", eng)

        @block.gpsimd
        def _(eng):
            run("pool", eng)

        @block.sync
        def _(eng):
            run("sp", eng)


class Ctx:
    def __init__(self, nc, P, st):
        self.nc, self.P, self.st = nc, P, st
        self.banks = []
        self.bank_i = 0

    def sb(self, name, shape, dt=F32):
        return self.st.enter_context(self.nc.sbuf_tensor(self.P.name + "_" + name, shape, dt))

    def psum_banks(self, n, ncols=512, dt=F32):
        for i in range(n):
            t = self.st.enter_context(self.nc.psum_tensor("%s_ps%d" % (self.P.name, len(self.banks)), [128, ncols], dt))
            self.banks.append((t, Buf("ps%d" % len(self.banks), excl=True)))

    def bank(self):
        b = self.banks[self.bank_i % len(self.banks)]
        self.bank_i += 1
        return b


class WStream:
    def __init__(self, cx, name, nbuf, slot_elems, dt=BF16, eng="pool"):
        self.cx, self.name, self.nbuf, self.eng = cx, name, nbuf, eng
        self.tiles = [cx.sb("%s%d" % (name, i), [128, slot_elems], dt) for i in range(nbuf)]
        self.bufs = [Buf("%s%d" % (name, i)) for i in range(nbuf)]
        self.loads = []
        self.issued = 0

    def plan(self, loads):
        self.loads = loads

    def _issue(self, i):
        kt, ncols, src = self.loads[i]
        slot = i % self.nbuf
        view = self.tiles[slot][:, 0:kt * ncols].rearrange("p (k c) -> p k c", k=kt)
        self.cx.P.add(self.eng, lambda e, view=view, src=src: e.dma_start(out=view, in_=src),
                      writes=[self.bufs[slot]], dma_key=(self.name, slot))

    def get(self, i):
        while self.issued < min(len(self.loads), i + self.nbuf - 1):
            self._issue(self.issued)
            self.issued += 1
        kt, ncols, src = self.loads[i]
        slot = i % self.nbuf
        view = self.tiles[slot][:, 0:kt * ncols].rearrange("p (k c) -> p k c", k=kt)
        return view, self.bufs[slot]


def wslab(w_ap, kt, c0, ncols):
    return w_ap.rearrange("(k p) n -> p k n", p=128)[:, 0:kt, c0:c0 + ncols]


def build(debug=False, phases=(0, 1, 2, 3)):
    nc = bass.Bass("TRN2", target_bir_lowering=False)
    dram_in = lambda name, shape, dt=F32: nc.dram_tensor(name, shape, dt, kind="ExternalInput").ap()
    x = dram_in("x", [S, D])
    cT = dram_in("cT", [128, 8])
    ada_w = dram_in("ada_w", [D, 9 * D])
    ada_bT = dram_in("ada_bT", [128, 72])
    nwT = dram_in("nwT", [128, 32])
    ffn1_w_in = dram_in("ffn1_w_in", [D, 2 * DFF])
    ffn1_w_out = dram_in("ffn1_w_out", [DFF, D])
    w_in = dram_in("w_in", [D, INW])
    cmp_k_pe = dram_in("cmp_k_pe", [32, 64])
    cmp_k_w1 = dram_in("cmp_k_w1", [2048, 128])
    cmp_k_w2 = dram_in("cmp_k_w2", [128, 64])
    cmp_v_pe = dram_in("cmp_v_pe", [32, 64])
    cmp_v_w1 = dram_in("cmp_v_w1", [2048, 128])
    cmp_v_w2 = dram_in("cmp_v_w2", [128, 64])
    ret_norm_w = dram_in("ret_norm_w", [1, 512])
    w_nsa_up = dram_in("w_nsa_up", [512, D])
    w_ret_up = dram_in("w_ret_up", [512, D])
    w_out = dram_in("w_out", [D, D])
    ffn2_w_in = dram_in("ffn2_w_in", [D, 2 * DFF])
    ffn2_w_out = dram_in("ffn2_w_out", [DFF, D])
    rope_tab = dram_in("rope_tab", [128, 32, 64])
    ret_tab = dram_in("ret_tab", [128, 24])
    out = nc.dram_tensor("out", [S, D], F32, kind="ExternalOutput").ap()

    skind = "ExternalOutput" if debug else "Internal"
    scr = lambda name, shape, dt: nc.dram_tensor(name, shape, dt, kind=skind).ap()
    h1T = scr("h1T", [D, S], F32)
    u2T = scr("u2T", [D, S], BF16)
    QT = scr("QT", [8, 64, S], BF16)
    KVT = scr("KVT", [4, 128, S], BF16)
    TOKF = scr("TOKF", [S, 1560], F32)
    TOKB = scr("TOKB", [S, 768], BF16)
    ONT = scr("ONT", [512, S], BF16)
    ORT = scr("ORT", [512, S], BF16)
    modT_d = scr("modT_d", [128, 72], F32)
    GT = scr("GT", [24, S], F32)

    with ExitStack() as gst:
        modT = gst.enter_context(nc.sbuf_tensor("modT", [128, 72], F32))
        coef = gst.enter_context(nc.sbuf_tensor("coef", [128, 96], F32))
        ones_bf = gst.enter_context(nc.sbuf_tensor("ones_bf", [128, 128], BF16))
        ident_f = gst.enter_context(nc.sbuf_tensor("ident_f", [128, 128], F32))
        ident_b = gst.enter_context(nc.sbuf_tensor("ident_b", [128, 128], BF16))
        B_modT, B_coef, B_const = Buf("modT"), Buf("coef"), Buf("const")
        G1, SH1, HG1, G2, SH2, GT2, G3, SH3, HG3, FW = [coef[:, i * 8:(i + 1) * 8] for i in range(10)]

        with ExitStack() as st:
            P = Prog(nc, "p0")
            cx = Ctx(nc, P, st)
            cx.psum_banks(4)
            pm, Bpm = cx.banks.pop(0)
            csb = cx.sb("csb", [128, 8])
            sil = cx.sb("sil", [128, 8])
            abT = cx.sb("abT", [128, 72])
            nw = cx.sb("nw", [128, 32])
            tmp = cx.sb("tmp", [128, 8])
            Bc, Bs, Bab, Bnw, Btmp = Buf("c"), Buf("sil"), Buf("ab"), Buf("nw"), Buf("tmp")
            P.add("sp", lambda e: e.dma_start(out=csb[:], in_=cT), writes=[Bc], dma_key="c")
            P.add("sp", lambda e: e.dma_start(out=abT[:], in_=ada_bT), writes=[Bab], dma_key="ab")
            P.add("sp", lambda e: e.dma_start(out=nw[:], in_=nwT), writes=[Bnw], dma_key="nw")
            P.add("pool", lambda e: e.memset(ones_bf[:], 1.0), writes=[B_const])
            P.add("pool", lambda e: e.memset(ident_f[:], 1.0), writes=[B_const])
            P.add("pool", lambda e: e.affine_select(out=ident_f[:], in_=ident_f[:], pattern=[[-1, 128]], compare_op=ALU.is_equal,
                                                    fill=0.0, base=0, channel_multiplier=1), reads=[B_const], writes=[B_const])
            P.add("pool", lambda e: e.tensor_copy(out=ident_b[:], in_=ident_f[:]), reads=[B_const], writes=[B_const])
            P.add("act", lambda e: e.activation(out=sil[:], in_=csb[:], func=AF.Silu), reads=[Bc], writes=[Bs])
            modrow = cx.sb("modrow", [1, 9216])
            Bmr = Buf("modrow")
            one_f = cx.sb("one_f", [1, 1])
            P.add("pool", lambda e: e.memset(one_f[:], 1.0), writes=[B_const])
            ws = WStream(cx, "ada", 4, 4096, dt=F32, eng="sp")
            ws.plan([(8, 512, wslab(ada_w, 8, s_ * 512, 512)) for s_ in range(18)])
            for s_ in range(18):
                wv, Bw = ws.get(s_)
                pr_, Bpr_ = cx.bank()
                for k in range(8):
                    P.add("pe", lambda e, wv=wv, k=k, pr_=pr_: e.matmul(pr_[0:1, :], lhsT=sil[:, k:k + 1], rhs=wv[:, k, :], start=(k == 0), stop=(k == 7)),
                          reads=[Bw, Bs], writes=[Bpr_])
                if s_ % 2 == 0:
                    P.add("act", lambda e, pr_=pr_, s_=s_: e.copy(out=modrow[0:1, s_ * 512:(s_ + 1) * 512], in_=pr_[0:1, :]), reads=[Bpr_], writes=[Bmr])
                else:
                    P.add("dve", lambda e, pr_=pr_, s_=s_: e.tensor_copy(out=modrow[0:1, s_ * 512:(s_ + 1) * 512], in_=pr_[0:1, :]), reads=[Bpr_], writes=[Bmr])
            for j in range(72):
                P.add("pe", lambda e, j=j: e.matmul(pm[:, j:j + 1], lhsT=modrow[0:1, j * 128:(j + 1) * 128], rhs=one_f[0:1, 0:1], start=True, stop=True, skip_group_check=True),
                      reads=[Bmr, B_const], writes=[Bpm])
            P.add("dve", lambda e: e.tensor_tensor(out=modT[:], in0=pm[:, 0:72], in1=abT[:], op=ALU.add), reads=[Bpm, Bab], writes=[B_modT])
            mv = lambda v: modT[:, v * 8:(v + 1) * 8]
            for (gi, nwi, sci) in ((G1, 0, 1), (G2, 1, 4), (G3, 2, 7)):
                P.add("dve", lambda e, sci=sci: e.tensor_scalar(out=tmp[:], in0=mv(sci), scalar1=1.0, scalar2=None, op0=ALU.add),
                      reads=[B_modT], writes=[Btmp])
                P.add("dve", lambda e, gi=gi, nwi=nwi: e.tensor_tensor(out=gi, in0=tmp[:], in1=nw[:, nwi * 8:(nwi + 1) * 8], op=ALU.mult),
                      reads=[Btmp, Bnw], writes=[B_coef])
            for (dst, src, sc) in ((SH1, 0, 1.0), (HG1, 2, 0.5), (SH2, 3, 1.0), (GT2, 5, 1.0), (SH3, 6, 1.0), (HG3, 8, 0.5)):
                P.add("dve", lambda e, dst=dst, src=src, sc=sc: e.tensor_scalar(out=dst, in0=mv(src), scalar1=sc, scalar2=None, op0=ALU.mult),
                      reads=[B_modT], writes=[B_coef])
            P.add("dve", lambda e: e.tensor_copy(out=FW, in_=nw[:, 24:32]), reads=[Bnw], writes=[B_coef])
            if debug:
                P.add("sp", lambda e: e.dma_start(out=modT_d, in_=modT[:]), reads=[B_modT], dma_key="dbg")
            P.emit(st)

        def rms_stats(cx, P, hT, Bh, sq, Bsq, rstd, Brstd):
            pss, Bpss = cx.bank()
            for kt in range(8):
                P.add("act", lambda e, kt=kt: e.activation(out=sq[kt % 2][:], in_=hT[:, kt, :], func=AF.Square),
                      reads=[Bh[kt]], writes=[Bsq[kt % 2]])
                P.add("pe", lambda e, kt=kt: e.matmul(pss[:], lhsT=ones_bf[:], rhs=sq[kt % 2][:], start=(kt == 0), stop=(kt == 7)),
                      reads=[Bsq[kt % 2], B_const], writes=[Bpss])
            P.add("act", lambda e: e.activation(out=rstd[:], in_=pss[:], func=AF.Sqrt, bias=eps_t[:, 0:1], scale=1.0 / D),
                  reads=[Bpss, B_const], writes=[Brstd])
            P.add("dve", lambda e: e.reciprocal(out=rstd[:], in_=rstd[:]), reads=[Brstd], writes=[Brstd])

        def rms_mod(cx, P, hT, Bh, uT, Bu, g, sh, sq, Bsq, rstd, Brstd, tmpf, Btmpf):
            rms_stats(cx, P, hT, Bh, sq, Bsq, rstd, Brstd)
            for kt in range(8):
                P.add("dve", lambda e, kt=kt: e.tensor_tensor(out=tmpf[kt % 2][:], in0=hT[:, kt, :], in1=rstd[:], op=ALU.mult),
                      reads=[Bh[kt], Brstd], writes=[Btmpf[kt % 2]])
                P.add("act", lambda e, kt=kt: e.activation(out=uT[:, kt, :], in_=tmpf[kt % 2][:], func=AF.Identity,
                                                           bias=sh[:, kt:kt + 1], scale=g[:, kt:kt + 1]),
                      reads=[Btmpf[kt % 2], B_coef], writes=[Bu[kt]])

        def ffn(cx, P, ws, wi0, uT, Bu, hT, Bh, hidT, Bhid, sa, Bsa, hg):
            wi = wi0
            for s6 in range(6):
                ncols = 512 if s6 < 5 else 256
                wa, Bwa = ws.get(wi)
                wb, Bwb = ws.get(wi + 1)
                wi += 2
                for jj in range(ncols // 128):
                    j = s6 * 4 + jj
                    pa, Bpa = cx.bank()
                    pb, Bpb = cx.bank()
                    for kt in range(8):
                        P.add("pe", lambda e, pa=pa, wa=wa, jj=jj, kt=kt: e.matmul(pa[:], lhsT=wa[:, kt, jj * 128:(jj + 1) * 128], rhs=uT[:, kt, :],
                                                                                  start=(kt == 0), stop=(kt == 7)),
                              reads=[Bwa, Bu[kt]], writes=[Bpa])
                    for kt in range(8):
                        P.add("pe", lambda e, pb=pb, wb=wb, jj=jj, kt=kt: e.matmul(pb[:], lhsT=wb[:, kt, jj * 128:(jj + 1) * 128], rhs=uT[:, kt, :],
                                                                                  start=(kt == 0), stop=(kt == 7)),
                              reads=[Bwb, Bu[kt]], writes=[Bpb])
                    P.add("act", lambda e, pa=pa, j=j: e.activation(out=sa[j % 2][:], in_=pa[:], func=AF.Silu),
                          reads=[Bpa], writes=[Bsa[j % 2]])
                    P.add("dve", lambda e, pb=pb, j=j: e.tensor_tensor(out=hidT[:, j, :], in0=pb[:], in1=sa[j % 2][:], op=ALU.mult),
                          reads=[Bpb, Bsa[j % 2]], writes=[Bhid[j]])
            for m2 in range(4):
                wo, Bwo = ws.get(wi)
                wi += 1
                for mm in range(2):
                    m = m2 * 2 + mm
                    py, Bpy = cx.bank()
                    for ht in range(22):
                        P.add("pe", lambda e, py=py, wo=wo, mm=mm, ht=ht: e.matmul(py[:], lhsT=wo[:, ht, mm * 128:(mm + 1) * 128], rhs=hidT[:, ht, :],
                                                                                  start=(ht == 0), stop=(ht == 21)),
                              reads=[Bwo, Bhid[ht]], writes=[Bpy])
                    P.add("dve", lambda e, py=py, m=m: e.scalar_tensor_tensor(out=hT[:, m, :], in0=py[:], scalar=hg[:, m:m + 1], in1=hT[:, m, :],
                                                                            op0=ALU.mult, op1=ALU.add),
                          reads=[Bpy, Bh[m], B_coef], writes=[Bh[m]])
            return wi

        def ffn_loads(w_in_ap, w_out_ap):
            L = []
            for s6 in range(6):
                ncols = 512 if s6 < 5 else 256
                L.append((8, ncols, wslab(w_in_ap, 8, s6 * 512, ncols)))
                L.append((8, ncols, wslab(w_in_ap, 8, DFF + s6 * 512, ncols)))
            for m2 in range(4):
                L.append((22, 256, wslab(w_out_ap, 22, m2 * 256, 256)))
            return L

        eps_t = gst.enter_context(nc.sbuf_tensor("eps_t", [128, 1], F32))

        if 1 in phases:
            with ExitStack() as st:
                P = Prog(nc, "p1")
                cx = Ctx(nc, P, st)
                cx.psum_banks(7)
                P.add("pool", lambda e: e.memset(eps_t[:], EPS), writes=[B_const])
                xs = [cx.sb("xs%d" % i, [128, 4, D]) for i in range(1)] * 2
                Bxs = [Buf("xs%d" % i) for i in range(1)] * 2
                hT = [cx.sb("hT%d" % i, [128, 8, CT]) for i in range(2)]
                Bh = [[Buf("hT%d_%d" % (i, k)) for k in range(8)] for i in range(2)]
                u1 = cx.sb("u1", [128, 8, CT], BF16)
                Bu1 = [Buf("u1_%d" % k) for k in range(8)]
                u2 = cx.sb("u2", [128, 8, CT], BF16)
                Bu2 = [Buf("u2_%d" % k) for k in range(8)]
                hid = cx.sb("hid", [128, 22, CT], BF16)
                Bhid = [Buf("hid%d" % k) for k in range(22)]
                sa = [cx.sb("sa%d" % i, [128, CT], BF16) for i in range(2)]
                Bsa = [Buf("sa%d" % i) for i in range(2)]
                sq = [cx.sb("sq%d" % i, [128, CT], BF16) for i in range(2)]
                Bsq = [Buf("sq%d" % i) for i in range(2)]
                tmpf = [cx.sb("tmpf%d" % i, [128, CT]) for i in range(2)]
                Btmpf = [Buf("tmpf%d" % i) for i in range(2)]
                rstd = cx.sb("rstd", [128, CT])
                Brstd = Buf("rstd")
                qst = cx.sb("qst", [64, 8, CT], BF16)
                Bqst = Buf("qst")
                kvst = cx.sb("kvst", [128, 4, CT], BF16)
                Bkvst = Buf("kvst")
                gst = cx.sb("gst", [24, CT])
                Bgst = Buf("gst")
                tokf = cx.sb("tokf", [128, 4, 1560])
                Btokf = Buf("tokf")
                tokb = cx.sb("tokb", [128, 4, 768], BF16)
                Btokb = Buf("tokb")
                ws = WStream(cx, "w1", 4, 5632)
                PSL = [(C_NQ, 512), (C_NKC, 512), (C_NKW, 280), (C_RQ, 512), (C_RK, 512), (C_RV, 512), (C_RG, 512)]
                loads = []
                for c in range(NCH):
                    loads += ffn_loads(ffn1_w_in, ffn1_w_out)
                    loads += [(8, n, wslab(w_in, 8, c0, n)) for (c0, n) in PSL]
                ws.plan(loads)
                LPC = len(loads) // NCH
                xv = x.rearrange("(c j p) d -> c p j d", j=4, p=128)
                h1v = h1T.rearrange("(k p) t -> p k t", p=128)
                u2v = u2T.rearrange("(k p) t -> p k t", p=128)
                QTv = QT.rearrange("h d t -> d h t")
                KVTv = KVT.rearrange("f p t -> p f t")
                for c in range(NCH):
                    b = c % 2
                    t0 = c * CT
                    P.add("sp", lambda e, c=c, b=b: e.dma_start(out=xs[b][:], in_=xv[c]), writes=[Bxs[b]], dma_key="xs")
                    for j in range(4):
                        for a in range(2):
                            pt, Bpt = cx.bank()
                            for q in range(4):
                                kt = a * 4 + q
                                P.add("pe", lambda e, pt=pt, j=j, kt=kt, q=q, b=b: e.transpose(pt[:, q * 128:(q + 1) * 128], xs[b][:, j, kt * 128:(kt + 1) * 128], ident_f[:]),
                                      reads=[Bxs[b], B_const], writes=[Bpt])
                            eng = "act" if (j * 2 + a) % 2 == 0 else "dve"
                            dst = hT[b][:, a * 4:(a + 1) * 4, j * 128:(j + 1) * 128]
                            srcv = pt[:].rearrange("p (q t) -> p q t", q=4)
                            if eng == "act":
                                P.add("act", lambda e, dst=dst, srcv=srcv: e.copy(out=dst, in_=srcv), reads=[Bpt], writes=Bh[b][a * 4:(a + 1) * 4])
                            else:
                                P.add("dve", lambda e, dst=dst, srcv=srcv: e.tensor_copy(out=dst, in_=srcv), reads=[Bpt], writes=Bh[b][a * 4:(a + 1) * 4])
                    rms_mod(cx, P, hT[b], Bh[b], u1, Bu1, G1, SH1, sq, Bsq, rstd, Brstd, tmpf, Btmpf)
                    wi = ffn(cx, P, ws, c * LPC, u1, Bu1, hT[b], Bh[b], hid, Bhid, sa, Bsa, HG1)
                    P.add("sp", lambda e, b=b, t0=t0: e.dma_start(out=h1v[:, :, t0:t0 + CT], in_=hT[b][:]), reads=Bh[b], dma_key=("h1", b))
                    rms_mod(cx, P, hT[b], Bh[b], u2, Bu2, G2, SH2, sq, Bsq, rstd, Brstd, tmpf, Btmpf)
                    P.add("sp", lambda e, t0=t0: e.dma_start(out=u2v[:, :, t0:t0 + CT], in_=u2[:]), reads=Bu2, dma_key="u2st")
                    TOKFv = TOKF.rearrange("(c j p) n -> c p j n", j=4, p=128)
                    TOKBv = TOKB.rearrange("(c j p) n -> c p j n", j=4, p=128)

                    def tokmm(wsl, Bwsl, cc, ncol, evac):
                        for j in range(4):
                            pp, Bpp = cx.bank()
                            for kt in range(8):
                                P.add("pe", lambda e, pp=pp, kt=kt, j=j: e.matmul(
                                    pp[:, 0:ncol], lhsT=u2[:, kt, j * 128:(j + 1) * 128], rhs=wsl[:, kt, cc:cc + ncol],
                                    start=(kt == 0), stop=(kt == 7)), reads=[Bwsl, Bu2[kt]], writes=[Bpp])
                            evac(pp, Bpp, j)

                    def ev_b(off, ncol):
                        def f(pp, Bpp, j):
                            P.add("dve", lambda e: e.tensor_copy(out=tokb[:, j, off:off + ncol], in_=pp[:, 0:ncol]), reads=[Bpp], writes=[Btokb])
                        return f

                    def ev_f(off, fn, eng):
                        def f(pp, Bpp, j):
                            if eng == "dve":
                                P.add("dve", lambda e: e.tensor_copy(out=tokf[:, j, off:off + 512], in_=pp[:, 0:512]), reads=[Bpp], writes=[Btokf])
                            else:
                                P.add("act", lambda e: e.activation(out=tokf[:, j, off:off + 512], in_=pp[:, 0:512], func=fn), reads=[Bpp], writes=[Btokf])
                        return f

                    def ev_vwg(pp, Bpp, j):
                        P.add("dve", lambda e: e.tensor_copy(out=tokb[:, j, 128:256], in_=pp[:, 0:128]), reads=[Bpp], writes=[Btokb])
                        P.add("act", lambda e: e.activation(out=tokf[:, j, 1536:1560], in_=pp[:, 128:152], func=AF.Sigmoid), reads=[Bpp], writes=[Btokf])

                    def featmm(wsl, Bwsl, cc, f):
                        pk, Bpk = cx.bank()
                        for kt in range(8):
                            P.add("pe", lambda e, kt=kt: e.matmul(pk[:], lhsT=wsl[:, kt, cc:cc + 128], rhs=u2[:, kt, :], start=(kt == 0), stop=(kt == 7)),
                                  reads=[Bwsl, Bu2[kt]], writes=[Bpk])
                        if f % 2 == 0:
                            P.add("act", lambda e: e.copy(out=kvst[:, f, :], in_=pk[:]), reads=[Bpk], writes=[Bkvst])
                        else:
                            P.add("dve", lambda e: e.tensor_copy(out=kvst[:, f, :], in_=pk[:]), reads=[Bpk], writes=[Bkvst])

                    wq, Bwq = ws.get(wi)
                    for h in range(8):
                        pq, Bpq = cx.bank()
                        for kt in range(8):
                            P.add("pe", lambda e, pq=pq, h=h, kt=kt, wq=wq: e.matmul(pq[0:64, :], lhsT=wq[:, kt, h * 64:(h + 1) * 64], rhs=u2[:, kt, :],
                                                                             start=(kt == 0), stop=(kt == 7)),
                                  reads=[Bwq, Bu2[kt]], writes=[Bpq])
                        if h % 2 == 0:
                            P.add("act", lambda e, pq=pq, h=h: e.mul(out=qst[:, h, :], in_=pq[0:64, :], mul=0.125), reads=[Bpq], writes=[Bqst])
                        else:
                            P.add("dve", lambda e, pq=pq, h=h: e.tensor_scalar(out=qst[:, h, :], in0=pq[0:64, :], scalar1=0.125, scalar2=None, op0=ALU.mult),
                                  reads=[Bpq], writes=[Bqst])
                    P.add("sp", lambda e, t0=t0: e.dma_start(out=QTv[:, :, t0:t0 + CT], in_=qst[:]), reads=[Bqst], dma_key="qst")
                    wk1, Bwk1 = ws.get(wi + 1)
                    featmm(wk1, Bwk1, 0, 0)
                    featmm(wk1, Bwk1, 128, 1)
                    featmm(wk1, Bwk1, 256, 2)
                    tokmm(wk1, Bwk1, 384, 128, ev_b(0, 128))
                    wk2, Bwk2 = ws.get(wi + 2)
                    featmm(wk2, Bwk2, 0, 3)
                    pg_, Bpg_ = cx.bank()
                    for kt in range(8):
                        P.add("pe", lambda e, kt=kt, pg_=pg_, wk2=wk2: e.matmul(pg_[0:24, :], lhsT=wk2[:, kt, 256:280], rhs=u2[:, kt, :], start=(kt == 0), stop=(kt == 7)),
                              reads=[Bwk2, Bu2[kt]], writes=[Bpg_])
                    P.add("act", lambda e, pg_=pg_: e.activation(out=gst[:], in_=pg_[0:24, :], func=AF.Sigmoid), reads=[Bpg_], writes=[Bgst])
                    P.add("sp", lambda e, t0=t0: e.dma_start(out=GT[:, t0:t0 + CT], in_=gst[:]), reads=[Bgst], dma_key="gst")
                    P.add("sp", lambda e, t0=t0: e.dma_start(out=KVTv[:, :, t0:t0 + CT], in_=kvst[:]), reads=[Bkvst], dma_key="kvst")
                    tokmm(wk2, Bwk2, 128, 152, ev_vwg)
                    wr, Bwr = ws.get(wi + 3)
                    tokmm(wr, Bwr, 0, 512, ev_f(0, None, "dve"))
                    wr, Bwr = ws.get(wi + 4)
                    tokmm(wr, Bwr, 0, 512, ev_f(512, AF.Copy, "act"))
                    wr, Bwr = ws.get(wi + 5)
                    tokmm(wr, Bwr, 0, 512, ev_b(256, 512))
                    wr, Bwr = ws.get(wi + 6)
                    tokmm(wr, Bwr, 0, 512, ev_f(1024, AF.Silu, "act"))
                    P.add("sp", lambda e, c=c: e.dma_start(out=TOKFv[c], in_=tokf[:]), reads=[Btokf], dma_key="tokf")
                    P.add("sp", lambda e, c=c: e.dma_start(out=TOKBv[c], in_=tokb[:]), reads=[Btokb], dma_key="tokb")
                P.emit(st)

        if 2 in phases:
            with ExitStack() as st:
                P = Prog(nc, "p2a")
                cx = Ctx(nc, P, st)
                cx.psum_banks(8)
                Sbanks = cx.banks[0:3]
                Obanks = cx.banks[3:5]
                Cbanks = cx.banks[5:7]
                Mbank = cx.banks[7]
                rr = {"s": 0, "o": 0, "pt": 0}
                Qaug = [cx.sb("qaug%d" % i, [128, S], BF16) for i in range(4)]
                BQ = [[Buf("q%d_%d" % (i, c)) for c in range(NCH)] for i in range(4)]
                Ksl = cx.sb("ksl", [128, S], BF16)
                BKsl = Buf("ksl")
                Kw = cx.sb("kw", [128, S], BF16)
                BKw = Buf("kw")
                Vs = cx.sb("vs", [128, 32, 65], BF16)
                BVs = Buf("vs")
                Vw = cx.sb("vw", [128, 32, 65], BF16)
                BVw = Buf("vw")
                kcr = cx.sb("kcr", [64, S], BF16)
                vcr = cx.sb("vcr", [64, S], BF16)
                Bkcr, Bvcr = Buf("kcr"), Buf("vcr")
                Kc = cx.sb("kc", [128, 256], BF16)
                BKc = Buf("kc")
                Vc = cx.sb("vc", [128, 2, 129], BF16)
                BVc = Buf("vc")
                cmask = cx.sb("cmask", [128, 2, S], BF16)
                G = cx.sb("gates", [128, 32, 24])
                BG = Buf("gates")
                BIAS = cx.sb("bias", [128, 32, 64])
                caus = cx.sb("caus", [128, 128], BF16)
                anti = cx.sb("anti", [128, 128], BF16)
                Bc2 = Buf("const2")
                acc = [cx.sb("acc%d" % i, [128, 4, 256]) for i in range(2)]
                Bacc = [[Buf("acc%d_%d" % (i, h)) for h in range(4)] for i in range(2)]
                accb = cx.sb("accb", [128, 4, 256], BF16)
                Baccb = Buf("accb")
                impacc = [cx.sb("imp%d" % i, [128, 4, 64]) for i in range(2)]
                Bimp = [Buf("imp%d" % i) for i in range(2)]
                PT = [cx.sb("pt%d" % i, [128, 512], BF16) for i in range(8)]
                BPT = [Buf("pt%d" % i) for i in range(8)]
                w1k = cx.sb("w1k", [64, 32, 128], BF16)
                w1v = cx.sb("w1v", [64, 32, 128], BF16)
                w2k = cx.sb("w2k", [128, 64], BF16)
                w2v = cx.sb("w2v", [128, 64], BF16)
                peTk = cx.sb("peTk", [64, 32], BF16)
                peTv = cx.sb("peTv", [64, 32], BF16)
                Bcw = Buf("cw")
                cbias = cx.sb("cbias", [128, 2])
                Bcb = Buf("cbias")
                hidc = cx.sb("hidc", [128, 2, 256], BF16)
                Bhidc = [Buf("hidck"), Buf("hidcv")]
                sm = cx.sb("sm", [128, 64])
                Bsm = Buf("sm")
                vt = cx.sb("vt", [128, 2, 64])
                Bvt = Buf("vt")
                nm = cx.sb("nm", [128, 64], BF16)
                Bnm = Buf("nm")
                nmT = cx.sb("nmT", [128, CT], BF16)
                BnmT = Buf("nmT")
                onst = cx.sb("onst", [128, 2, CT], BF16)
                Bonst = Buf("onst")

                P.add("dve", lambda e: e.memset(Ksl[64:128, :], 1.0), writes=[Bc2])
                P.add("pool", lambda e: e.affine_select(out=Ksl[64:128, :], in_=Ksl[64:128, :], pattern=[[1, S]], compare_op=ALU.is_ge, fill=0.0,
                                                        base=0, channel_multiplier=-64), reads=[Bc2], writes=[Bc2])
                P.add("pool", lambda e: e.affine_select(out=Ksl[64:128, :], in_=Ksl[64:128, :], pattern=[[-1, S]], compare_op=ALU.is_ge, fill=0.0,
                                                        base=63, channel_multiplier=64), reads=[Bc2], writes=[Bc2])
                P.add("dve", lambda e: e.memset(Kw[64:128, :], 0.0), writes=[Bc2])
                for hh in range(4):
                    P.add("dve", lambda e, hh=hh: e.memset(Qaug[hh][64:128, :], 0.0), writes=BQ[hh])
                P.add("pool", lambda e: e.memset(Kc[64:128, :], 0.0), writes=[Bc2])
                P.add("dve", lambda e: e.memset(cmask[:], 0.0), writes=[Bc2])
                for nt in range(2):
                    P.add("pool", lambda e, nt=nt: e.affine_select(out=cmask[:, nt, :], in_=cmask[:, nt, :], pattern=[[1, S]], compare_op=ALU.is_ge, fill=NEGM,
                                                                  base=-31 - 16 * 128 * nt, channel_multiplier=-16), reads=[Bc2], writes=[Bc2])
                P.add("pool", lambda e: e.memset(caus[:], 0.0), writes=[Bc2])
                P.add("pool", lambda e: e.affine_select(out=caus[:], in_=caus[:], pattern=[[1, 128]], compare_op=ALU.is_ge, fill=NEGM, base=0, channel_multiplier=-1),
                      reads=[Bc2], writes=[Bc2])
                P.add("pool", lambda e: e.memset(anti[:], 0.0), writes=[Bc2])
                P.add("pool", lambda e: e.affine_select(out=anti[:], in_=anti[:], pattern=[[-1, 128]], compare_op=ALU.is_gt, fill=NEGM, base=0, channel_multiplier=1),
                      reads=[Bc2], writes=[Bc2])
                P.add("dve", lambda e: e.memset(BIAS[:], 0.0), writes=[Bc2])
                for half in range(2):
                    rows = BIAS[half * 64:(half + 1) * 64, :, :]
                    P.add("pool", lambda e, rows=rows, half=half: e.affine_select(out=rows, in_=rows, pattern=[[2, 32], [-1, 64]], compare_op=ALU.is_ge, fill=-1e4,
                                                                                 base=half, channel_multiplier=0), reads=[Bc2], writes=[Bc2])
                    P.add("pool", lambda e, rows=rows, half=half: e.affine_select(out=rows, in_=rows, pattern=[[-2, 32], [1, 64]], compare_op=ALU.not_equal, fill=1e4,
                                                                                 base=-half, channel_multiplier=0), reads=[Bc2], writes=[Bc2])
                    P.add("pool", lambda e, rows=rows, half=half: e.affine_select(out=rows, in_=rows, pattern=[[-2, 32], [1, 64]], compare_op=ALU.not_equal, fill=1e4,
                                                                                 base=1 - half, channel_multiplier=0), reads=[Bc2], writes=[Bc2])
                    P.add("pool", lambda e, rows=rows: e.affine_select(out=rows, in_=rows, pattern=[[0, 32], [1, 64]], compare_op=ALU.not_equal, fill=1e4,
                                                                      base=0, channel_multiplier=0), reads=[Bc2], writes=[Bc2])
                P.add("pool", lambda e: e.memset(Vs[:, :, 64:65], 1.0), writes=[Bc2])
                P.add("pool", lambda e: e.memset(Vw[:, :, 64:65], 1.0), writes=[Bc2])
                P.add("pool", lambda e: e.memset(Vc[:, :, 64:129], 1.0), writes=[Bc2])
                for nt in range(2):
                    ov = Vc[:, nt, 65:129]
                    P.add("pool", lambda e, ov=ov, nt=nt: e.affine_select(out=ov, in_=ov, pattern=[[-4, 64]], compare_op=ALU.is_ge, fill=0.0,
                                                                         base=nt * 128 + 1, channel_multiplier=1), reads=[Bc2], writes=[Bc2])
                    P.add("pool", lambda e, ov=ov, nt=nt: e.affine_select(out=ov, in_=ov, pattern=[[4, 64]], compare_op=ALU.is_ge, fill=0.0,
                                                                         base=3 - nt * 128, channel_multiplier=-1), reads=[Bc2], writes=[Bc2])
                P.add("dve", lambda e: e.memset(hidc[:], 0.0), writes=Bhidc)
                P.add("pool", lambda e: e.dma_start(out=w1k[:], in_=cmp_k_w1.rearrange("(l d) h -> d l h", d=64)), writes=[Bcw], dma_key="cw0")
                P.add("pool", lambda e: e.dma_start(out=w1v[:], in_=cmp_v_w1.rearrange("(l d) h -> d l h", d=64)), writes=[Bcw], dma_key="cw1")
                P.add("pool", lambda e: e.dma_start(out=w2k[:], in_=cmp_k_w2), writes=[Bcw], dma_key="cw2")
                P.add("pool", lambda e: e.dma_start(out=w2v[:], in_=cmp_v_w2), writes=[Bcw], dma_key="cw3")
                P.add("pool", lambda e: e.dma_start(out=peTk[:], in_=cmp_k_pe.rearrange("l d -> d l"), allow_slow_non_contiguous=True), writes=[Bcw], dma_key="cw4")
                P.add("pool", lambda e: e.dma_start(out=peTv[:], in_=cmp_v_pe.rearrange("l d -> d l"), allow_slow_non_contiguous=True), writes=[Bcw], dma_key="cw5")
                P.add("sp", lambda e: e.dma_start(out=G[:], in_=TOKF[:, 1536:1560].rearrange("(k p) n -> p k n", p=128)), writes=[BG], dma_key="gates")
                for wi_, (w1sb, peT) in enumerate(((w1k, peTk), (w1v, peTv))):
                    pbk, Bpbk = Cbanks[wi_]
                    for l in range(32):
                        P.add("pe", lambda e, pbk=pbk, w1sb=w1sb, peT=peT, l=l: e.matmul(pbk[:, 0:1], lhsT=w1sb[:, l, :], rhs=peT[:, l:l + 1], start=(l == 0), stop=(l == 31)),
                              reads=[Bcw], writes=[Bpbk])
                    P.add("dve", lambda e, pbk=pbk, wi_=wi_: e.tensor_copy(out=cbias[:, wi_:wi_ + 1], in_=pbk[:, 0:1]), reads=[Bpbk], writes=[Bcb])

                def sbank():
                    b = Sbanks[rr["s"] % 3]
                    rr["s"] += 1
                    return b

                def ptbuf():
                    i = rr["pt"] % 8
                    rr["pt"] += 1
                    return PT[i], BPT[i]

                def run_tiles(tiles, depth=2):
                    pend = []

                    def pv(t):
                        for j in range(t["col0"] // 128, (t["col0"] + t["ncols"]) // 128):
                            o_ap, Bo = t["O"][j]
                            P.add("pe", lambda e, t=t, j=j, o_ap=o_ap: e.matmul(o_ap, lhsT=t["pt"][:, j * 128:(j + 1) * 128], rhs=t["V"], start=False, stop=True,
                                                                               skip_group_check=True),
                                  reads=[t["Bpt"], t["BV"]], writes=[Bo])
                        if t.get("after") is not None:
                            t["after"]()

                    for t in tiles:
                        while len(pend) >= depth:
                            pv(pend.pop(0))
                        if t.get("before") is not None:
                            t["before"]()
                        ps, Bps = sbank()
                        c0, ncl = t["col0"], t["ncols"]
                        mk = t.get("mask")
                        P.add("pe", lambda e, t=t, ps=ps, c0=c0, ncl=ncl, mk=mk: e.matmul(ps[:, c0:c0 + ncl], lhsT=t["K"], rhs=t["Q"][:, t["q0"] + c0:t["q0"] + c0 + ncl],
                                                                                        start=True, stop=(mk is None)),
                              reads=[t["BK"], t["BQ"]], writes=[Bps])
                        if mk is not None:
                            m_ap, mcol, mn = mk
                            P.add("pe", lambda e, ps=ps, m_ap=m_ap, mcol=mcol, mn=mn: e.matmul(ps[:, mcol:mcol + mn], lhsT=ident_b[:], rhs=m_ap, start=False, stop=True),
                                  reads=[Bc2, B_const], writes=[Bps])
                        pt, Bpt = ptbuf()
                        t["pt"], t["Bpt"] = pt, Bpt
                        P.add("act", lambda e, pt=pt, ps=ps, c0=c0, ncl=ncl: e.activation(out=pt[:, c0:c0 + ncl], in_=ps[:, c0:c0 + ncl], func=AF.Exp),
                              reads=[Bps], writes=[Bpt])
                        pend.append(t)
                    while pend:
                        pv(pend.pop(0))

                OTs = [cx.sb("ots%d" % i, [65, CT]) for i in range(2)]
                BOTs = [Buf("ots%d" % i) for i in range(2)]

                def score_exp2(K_ap, BK, hh, qc, c0, ncl, mk):
                    q0 = qc * CT
                    ps, Bps = sbank()
                    P.add("pe", lambda e: e.matmul(ps[:, c0:c0 + ncl], lhsT=K_ap, rhs=Qaug[hh][:, q0 + c0:q0 + c0 + ncl], start=True, stop=(mk is None)),
                          reads=[BK, BQ[hh][qc], Bc2], writes=[Bps])
                    if mk is not None:
                        m_ap, mcol, mn = mk
                        P.add("pe", lambda e: e.matmul(ps[:, mcol:mcol + mn], lhsT=ident_b[:], rhs=m_ap, start=False, stop=True),
                              reads=[Bc2, B_const], writes=[Bps])
                    pt, Bpt = ptbuf()
                    P.add("act", lambda e: e.activation(out=pt[:, c0:c0 + ncl], in_=ps[:, c0:c0 + ncl], func=AF.Exp), reads=[Bps], writes=[Bpt])
                    return pt, Bpt

                def combine_T(k, hh, h, br, qc, ab):
                    OTt, BOTt = Obanks[k]
                    P.add("dve", lambda e: e.tensor_copy(out=OTs[k][:], in_=OTt[0:65, :]), reads=[BOTt], writes=[BOTs[k]])
                    pm_, Bpm_ = Mbank
                    for j in range(4):
                        P.add("pe", lambda e, j=j: e.transpose(pm_[:, j * 65:(j + 1) * 65], OTs[k][0:65, j * 128:(j + 1) * 128], ident_f[0:65, 0:65]),
                              reads=[BOTs[k], B_const], writes=[Bpm_])
                    den = pm_[:, 0:260].rearrange("p (j c) -> p j c", j=4)[:, :, 64]
                    P.add("dve", lambda e: e.tensor_scalar(out=sm[:, 0:4], in0=den, scalar1=1e-30, scalar2=None, op0=ALU.max), reads=[Bpm_], writes=[Bsm])
                    P.add("dve", lambda e: e.reciprocal(out=sm[:, 0:4], in_=sm[:, 0:4]), reads=[Bsm], writes=[Bsm])
                    gsl = G[:, 4 * qc:4 * qc + 4, 3 * h + br]
                    P.add("dve", lambda e: e.tensor_tensor(out=sm[:, 4:8], in0=sm[:, 0:4], in1=gsl, op=ALU.mult), reads=[Bsm, BG], writes=[Bsm])
                    for j in range(4):
                        P.add("dve", lambda e, j=j: e.scalar_tensor_tensor(out=acc[ab][:, j, hh * 64:(hh + 1) * 64], in0=pm_[:, j * 65:j * 65 + 64],
                                                                        scalar=sm[:, 4 + j:5 + j], in1=acc[ab][:, j, hh * 64:(hh + 1) * 64],
                                                                        op0=ALU.mult, op1=ALU.add),
                              reads=[Bpm_, Bsm, Bacc[ab][hh]], writes=[Bacc[ab][hh]])

                QTv2 = QT
                for g in range(2):
                    for hh in range(4):
                        P.add("sp", lambda e, hh=hh, g=g: e.dma_start(out=Qaug[hh][0:64, :], in_=QTv2[4 * g + hh]), writes=BQ[hh], dma_key=("q", hh))
                    P.add("sp", lambda e, g=g: e.dma_start(out=Ksl[0:64, :], in_=KVT[2, g * 64:(g + 1) * 64, :]), writes=[BKsl], dma_key="ksl")
                    P.add("sp", lambda e, g=g: e.dma_start(out=Kw[0:64, :], in_=KVT[3, g * 64:(g + 1) * 64, :]), writes=[BKw], dma_key="kw")
                    P.add("sp", lambda e, g=g: e.dma_start(out=kcr[:], in_=KVT[0, g * 64:(g + 1) * 64, :]), writes=[Bkcr], dma_key="kcr")
                    P.add("sp", lambda e, g=g: e.dma_start(out=vcr[:], in_=KVT[1, g * 64:(g + 1) * 64, :]), writes=[Bvcr], dma_key="vcr")
                    P.add("sp", lambda e, g=g: e.dma_start(out=Vs[:, :, 0:64], in_=TOKB[:, g * 64:(g + 1) * 64].rearrange("(k p) d -> p k d", p=128)),
                          writes=[BVs], dma_key="vs")
                    P.add("sp", lambda e, g=g: e.dma_start(out=Vw[:, :, 0:64], in_=TOKB[:, 128 + g * 64:128 + (g + 1) * 64].rearrange("(k p) d -> p k d", p=128)),
                          writes=[BVw], dma_key="vw")
                    for wi_, (raw, Braw, w1sb, w2sb) in enumerate(((kcr, Bkcr, w1k, w2k), (vcr, Bvcr, w1v, w2v))):
                        ph, Bph = Cbanks[wi_]
                        for l in range(32):
                            P.add("pe", lambda e, ph=ph, w1sb=w1sb, raw=raw, l=l: e.matmul(ph[:, 0:255], lhsT=w1sb[:, l, :], rhs=raw[:, l:l + 16 * 254 + 1:16],
                                                                                          start=(l == 0), stop=(l == 31)),
                                  reads=[Bcw, Braw], writes=[Bph])
                        P.add("act", lambda e, ph=ph, wi_=wi_: e.activation(out=hidc[:, wi_, 0:255], in_=ph[:, 0:255], func=AF.Silu, bias=cbias[:, wi_:wi_ + 1]),
                              reads=[Bph, Bcb], writes=[Bhidc[wi_]])
                    pk, Bpk = Cbanks[0]
                    P.add("pe", lambda e, pk=pk: e.matmul(pk[0:64, 0:256], lhsT=w2k[:], rhs=hidc[:, 0, :], start=True, stop=True), reads=[Bcw, Bhidc[0]], writes=[Bpk])
                    P.add("dve", lambda e, pk=pk: e.tensor_copy(out=Kc[0:64, :], in_=pk[0:64, 0:256]), reads=[Bpk], writes=[BKc])
                    pv_, Bpv_ = Cbanks[1]
                    for nt in range(2):
                        P.add("pe", lambda e, pv_=pv_, nt=nt: e.matmul(pv_[:, nt * 64:(nt + 1) * 64], lhsT=hidc[:, 1, nt * 128:(nt + 1) * 128], rhs=w2v[:], start=True, stop=True),
                              reads=[Bcw, Bhidc[1]], writes=[Bpv_])
                    P.add("dve", lambda e, pv_=pv_: e.tensor_copy(out=Vc[:, :, 0:64], in_=pv_[:, 0:128].rearrange("p (n d) -> p n d", n=2)), reads=[Bpv_], writes=[BVc])

                    for qc in range(NCH):
                        q0 = qc * CT
                        ab = qc % 2
                        tiles = []
                        nts = [0] if qc <= 3 else [0, 1]
                        for hh in range(4):
                            h = 4 * g + hh
                            OA, BOA = (Cbanks if hh % 2 == 0 else Obanks)[0]
                            OB, BOB = (Cbanks if hh % 2 == 0 else Obanks)[1]
                            Oj = [(OA[:, 0:129], BOA), (OA[:, 129:258], BOA), (OB[:, 0:129], BOB), (OB[:, 129:258], BOB)]

                            def before(OA=OA, OB=OB, BOA=BOA, BOB=BOB):
                                P.add("dve", lambda e: e.memset(OA[:, 0:258], 0.0), writes=[BOA])
                                P.add("dve", lambda e: e.memset(OB[:, 0:258], 0.0), writes=[BOB])

                            def after(OA=OA, OB=OB, BOA=BOA, BOB=BOB, hh=hh, h=h, qc=qc, ab=ab):
                                for half, (Ob, BOb) in enumerate(((OA, BOA), (OB, BOB))):
                                    den = Ob[:, 0:258].rearrange("p (j c) -> p j c", j=2)[:, :, 64]
                                    rd = sm[:, half * 2:half * 2 + 2]
                                    P.add("dve", lambda e, den=den, rd=rd: e.tensor_scalar(out=rd, in0=den, scalar1=1e-30, scalar2=None, op0=ALU.max), reads=[BOb], writes=[Bsm])
                                    P.add("dve", lambda e, rd=rd: e.reciprocal(out=rd, in_=rd), reads=[Bsm], writes=[Bsm])
                                    fc = sm[:, 4 + half * 2:4 + half * 2 + 2]
                                    gsl = G[:, 4 * qc + half * 2:4 * qc + half * 2 + 2, 3 * h + 0]
                                    P.add("dve", lambda e, fc=fc, rd=rd, gsl=gsl: e.tensor_tensor(out=fc, in0=rd, in1=gsl, op=ALU.mult), reads=[Bsm, BG], writes=[Bsm])
                                    for jj in range(2):
                                        j = half * 2 + jj
                                        P.add("dve", lambda e, Ob=Ob, jj=jj, j=j: e.tensor_scalar(out=acc[ab][:, j, hh * 64:(hh + 1) * 64], in0=Ob[:, jj * 129:jj * 129 + 64],
                                                                                              scalar1=sm[:, 4 + j:5 + j], scalar2=None, op0=ALU.mult),
                                              reads=[BOb, Bsm], writes=[Bacc[ab][hh]])
                                        if hh == 0:
                                            P.add("dve", lambda e, Ob=Ob, jj=jj, j=j: e.tensor_scalar(out=impacc[ab][:, j, :], in0=Ob[:, jj * 129 + 65:jj * 129 + 129],
                                                                                                  scalar1=sm[:, j:j + 1], scalar2=None, op0=ALU.mult),
                                                  reads=[BOb, Bsm], writes=[Bimp[ab]])
                                        else:
                                            P.add("dve", lambda e, Ob=Ob, jj=jj, j=j: e.scalar_tensor_tensor(out=impacc[ab][:, j, :], in0=Ob[:, jj * 129 + 65:jj * 129 + 129],
                                                                                                         scalar=sm[:, j:j + 1], in1=impacc[ab][:, j, :], op0=ALU.mult, op1=ALU.add),
                                                  reads=[BOb, Bsm, Bimp[ab]], writes=[Bimp[ab]])

                            for ti, nt in enumerate(nts):
                                need_mask = (nt == 1) or (qc <= 4)
                                tiles.append(dict(K=Kc[:, nt * 128:(nt + 1) * 128], BK=BKc, Q=Qaug[hh], BQ=BQ[hh][qc], q0=q0, col0=0, ncols=512,
                                                  mask=(cmask[:, nt, q0:q0 + CT], 0, 512) if need_mask else None,
                                                  V=Vc[:, nt, :], BV=BVc, O=Oj,
                                                  before=before if ti == 0 else None, after=after if ti == len(nts) - 1 else None))
                        run_tiles(tiles)
                        pm_, Bpm_ = Mbank
                        for j in range(4):
                            qt = 4 * qc + j
                            vv = vt[:, j % 2, :]
                            P.add("dve", lambda e, vv=vv, j=j, qt=qt, ab=ab: e.tensor_tensor(out=vv, in0=impacc[ab][:, j, :], in1=BIAS[:, qt, :], op=ALU.add),
                                  reads=[Bimp[ab], Bc2], writes=[Bvt])
                            P.add("dve", lambda e, vv=vv: e.max(out=sm[:, 8:16], in_=vv), reads=[Bvt], writes=[Bsm])
                            P.add("dve", lambda e, vv=vv, j=j: e.match_replace(out=vt[:, (j + 1) % 2, :], in_to_replace=sm[:, 8:16], in_values=vv, imm_value=-1e9),
                                  reads=[Bvt, Bsm], writes=[Bvt])
                            P.add("dve", lambda e, j=j: e.max(out=sm[:, 16:24], in_=vt[:, (j + 1) % 2, :]), reads=[Bvt], writes=[Bsm])
                            P.add("dve", lambda e, vv=vv: e.tensor_scalar(out=nm[:], in0=vv, scalar1=sm[:, 23:24], scalar2=NEGM, op0=ALU.is_lt, op1=ALU.mult),
                                  reads=[Bvt, Bsm], writes=[Bnm])
                            P.add("pe", lambda e, pm_=pm_, j=j: e.matmul(pm_[64:128, j * 128:(j + 1) * 128], lhsT=nm[:], rhs=ident_b[:], start=True, stop=True),
                                  reads=[Bnm, B_const], writes=[Bpm_])
                        P.add("act", lambda e, pm_=pm_: e.copy(out=nmT[64:128, :], in_=pm_[64:128, :]), reads=[Bpm_], writes=[BnmT])
                        for hh in range(4):
                            eng = "pool" if hh % 2 == 0 else "dve"
                            P.add(eng, lambda e, hh=hh, q0=q0: e.tensor_copy(out=Qaug[hh][64:128, q0:q0 + CT], in_=nmT[64:128, :]), reads=[BnmT], writes=[BQ[hh][qc]])
                        for br, (Ka, BKa, Va, BVa) in ((1, (Ksl, BKsl, Vs, BVs)), (2, (Kw, BKw, Vw, BVw))):
                            tl = []
                            if br == 1:
                                for kt in range(4 * qc):
                                    tl.append((kt, 0, 512, None))
                            else:
                                for i in range(4):
                                    kt = 4 * qc - 4 + i
                                    if kt >= 0:
                                        tl.append((kt, 0, 128 * (i + 1), (anti[:], 128 * i, 128)))
                            for i in range(4):
                                tl.append((4 * qc + i, 128 * i, 512 - 128 * i, (caus[:], 128 * i, 128)))
                            for pair in range(2):
                                heads = (2 * pair, 2 * pair + 1)
                                for k in range(2):
                                    P.add("dve", lambda e, k=k: e.memset(Obanks[k][0][0:65, :], 0.0), writes=[Obanks[k][1]])

                                def flush(prev, heads=heads, Va=Va, BVa=BVa):
                                    pkt, pc0, pncl, ppts = prev
                                    for k, (pt, Bpt) in enumerate(ppts):
                                        P.add("pe", lambda e, k=k, pt=pt: e.matmul(Obanks[k][0][0:65, pc0:pc0 + pncl], lhsT=Va[:, pkt, :], rhs=pt[:, pc0:pc0 + pncl],
                                                                                  start=False, stop=True, skip_group_check=True),
                                              reads=[BVa, Bpt, Bc2], writes=[Obanks[k][1]])

                                prev = None
                                for (kt, c0, ncl, mk) in tl:
                                    pts = [score_exp2(Ka[:, kt * 128:(kt + 1) * 128], BKa, hh, qc, c0, ncl, mk) for hh in heads]
                                    if prev is not None:
                                        flush(prev)
                                    prev = (kt, c0, ncl, pts)
                                flush(prev)
                                for k, hh in enumerate(heads):
                                    combine_T(k, hh, 4 * g + hh, br, qc, ab)
                        P.add("act", lambda e, ab=ab: e.copy(out=accb[:], in_=acc[ab][:]), reads=Bacc[ab], writes=[Baccb])
                        ptb, Bptb = Mbank
                        ptv = ptb[:].bitcast(BF16)
                        for f in range(2):
                            for j in range(4):
                                P.add("pe", lambda e, f=f, j=j, ptv=ptv: e.transpose(ptv[:, f * 512 + j * 128:f * 512 + (j + 1) * 128], accb[:, j, f * 128:(f + 1) * 128], ident_b[:]),
                                      reads=[Baccb, B_const], writes=[Bptb])
                        P.add("dve", lambda e, ptv=ptv: e.tensor_copy(out=onst[:], in_=ptv.rearrange("p (f t) -> p f t", f=2)), reads=[Bptb], writes=[Bonst])
                        P.add("sp", lambda e, g=g, q0=q0: e.dma_start(out=ONT[g * 256:(g + 1) * 256, q0:q0 + CT].rearrange("(f p) t -> p f t", p=128), in_=onst[:]),
                              reads=[Bonst], dma_key="onst")
                P.emit(st)

        if 2 in phases:
            with ExitStack() as st:
                P = Prog(nc, "p2b")
                cx = Ctx(nc, P, st)
                cx.psum_banks(8)
                bE, bO, bOo, bS, bTq, bTk, bTy = cx.banks[0:7]
                rope = cx.sb("rope", [128, 32, 64])
                rt = cx.sb("rt", [128, 24])
                rnw = cx.sb("rnw", [128, 512])
                mask01 = cx.sb("mask01", [128, 128])
                Bk = Buf("consts")
                qk = [cx.sb("qk%d" % i, [128, 1024]) for i in range(2)]
                rgs = [cx.sb("rg%d" % i, [128, 512]) for i in range(2)]
                rv = [cx.sb("rv%d" % i, [128, 512], BF16) for i in range(2)]
                Bqk = [Buf("qk%d" % i) for i in range(2)]
                Brg = [Buf("rg%d" % i) for i in range(2)]
                Brv = [Buf("rv%d" % i) for i in range(2)]
                tq = cx.sb("tq", [128, 4, 256])
                tk = cx.sb("tk", [128, 4, 256])
                Btq, Btk = Buf("tq"), Buf("tk")
                qr = cx.sb("qr", [128, 512])
                kr = cx.sb("kr", [128, 512])
                Bqr, Bkr = Buf("qr"), Buf("kr")
                qd = cx.sb("qd", [128, 512], BF16)
                kinv = cx.sb("kinv", [128, 512], BF16)
                Bqd, Bkinv = Buf("qd"), Buf("kinv")
                QdT = cx.sb("QdT", [128, 4, 128], BF16)
                KinvT = cx.sb("KinvT", [128, 4, 128], BF16)
                BQdT, BKinvT = Buf("QdT"), Buf("KinvT")
                innerT = cx.sb("innerT", [128, 8, 128], BF16)
                Binn = [Buf("innE"), Buf("innO")]
                St = cx.sb("St", [128, 4, 64])
                Sbf = cx.sb("Sbf", [128, 8, 64], BF16)
                BSt, BSbf = Buf("St"), Buf("Sbf")
                osb = cx.sb("osb", [128, 512])
                osq = cx.sb("osq", [128, 512])
                Bosb, Bosq = Buf("osb"), Buf("osq")
                stt = cx.sb("stt", [128, 40])
                Bstt = Buf("stt")
                ybf = cx.sb("ybf", [128, 512], BF16)
                Bybf = Buf("ybf")
                orst = cx.sb("orst", [128, 4, 128], BF16)
                Borst = Buf("orst")
                P.add("sp", lambda e: e.dma_start(out=rope[:], in_=rope_tab), writes=[Bk], dma_key="c0")
                P.add("sp", lambda e: e.dma_start(out=rt[:], in_=ret_tab), writes=[Bk], dma_key="c1")
                P.add("sp", lambda e: e.dma_start(out=rnw[:], in_=ret_norm_w.to_broadcast([128, 512])), writes=[Bk], dma_key="c2")
                P.add("pool", lambda e: e.memset(mask01[:], 1.0), writes=[Bk])
                P.add("pool", lambda e: e.affine_select(out=mask01[:], in_=mask01[:], pattern=[[1, 128]], compare_op=ALU.is_ge, fill=0.0, base=0, channel_multiplier=-1),
                      reads=[Bk], writes=[Bk])
                P.add("pool", lambda e: e.memset(St[:], 0.0), writes=[BSt])
                P.add("pool", lambda e: e.memset(Sbf[:], 0.0), writes=[BSbf])

                def load(i):
                    b = i % 2
                    r0 = i * 128
                    P.add("sp", lambda e: e.dma_start(out=qk[b][:], in_=TOKF[r0:r0 + 128, 0:1024]), writes=[Bqk[b]], dma_key=("qk", b))
                    P.add("sp", lambda e: e.dma_start(out=rgs[b][:], in_=TOKF[r0:r0 + 128, 1024:1536]), writes=[Brg[b]], dma_key=("rg", b))
                    P.add("sp", lambda e: e.dma_start(out=rv[b][:], in_=TOKB[r0:r0 + 128, 256:768]), writes=[Brv[b]], dma_key=("rv", b))

                def rope_apply(eng, src, Bsrc, tt, Btt, dst, Bdst, i):
                    x = src.rearrange("p (h d) -> p h d", h=8)
                    x1, x2 = x[:, :, 0:32], x[:, :, 32:64]
                    cos = rope[:, i:i + 1, 0:32].to_broadcast([128, 8, 32])
                    sin = rope[:, i:i + 1, 32:64].to_broadcast([128, 8, 32])
                    tv = [tt[:, k, :].rearrange("p (h d) -> p h d", h=8) for k in range(4)]
                    d = dst.rearrange("p (h d) -> p h d", h=8)
                    P.add(eng, lambda e: e.tensor_tensor(out=tv[0], in0=x1, in1=cos, op=ALU.mult), reads=[Bsrc, Bk], writes=[Btt])
                    P.add(eng, lambda e: e.tensor_tensor(out=tv[1], in0=x2, in1=sin, op=ALU.mult), reads=[Bsrc, Bk], writes=[Btt])
                    P.add(eng, lambda e: e.tensor_tensor(out=tv[2], in0=x1, in1=sin, op=ALU.mult), reads=[Bsrc, Bk], writes=[Btt])
                    P.add(eng, lambda e: e.tensor_tensor(out=tv[3], in0=x2, in1=cos, op=ALU.mult), reads=[Bsrc, Bk], writes=[Btt])
                    P.add(eng, lambda e: e.tensor_tensor(out=d[:, :, 0:32], in0=tv[0], in1=tv[1], op=ALU.subtract), reads=[Btt], writes=[Bdst])
                    P.add(eng, lambda e: e.tensor_tensor(out=d[:, :, 32:64], in0=tv[2], in1=tv[3], op=ALU.add), reads=[Btt], writes=[Bdst])

                ORv = ORT.rearrange("(f p) t -> p f t", p=128)
                tqv = bTq[0][:].bitcast(BF16)
                tkv = bTk[0][:].bitcast(BF16)

                def front(i):
                    b = i % 2
                    rope_apply("dve", qk[b][:, 0:512], Bqk[b], tq, Btq, qr[:], Bqr, i)
                    rope_apply("pool", qk[b][:, 512:1024], Bqk[b], tk, Btk, kr[:], Bkr, i)
                    P.add("dve", lambda e: e.tensor_tensor(out=qd[:].rearrange("p (h d) -> p h d", h=8), in0=qr[:].rearrange("p (h d) -> p h d", h=8),
                                                           in1=rt[:, 0:8].unsqueeze(2).to_broadcast([128, 8, 64]), op=ALU.mult), reads=[Bqr, Bk], writes=[Bqd])
                    P.add("pool", lambda e: e.tensor_tensor(out=kinv2[b][:].rearrange("p (h d) -> p h d", h=8), in0=kr[:].rearrange("p (h d) -> p h d", h=8),
                                                            in1=rt[:, 8:16].unsqueeze(2).to_broadcast([128, 8, 64]), op=ALU.mult), reads=[Bkr, Bk], writes=[Bkinv2[b]])
                    for pr in range(4):
                        P.add("pe", lambda e, pr=pr: e.transpose(tqv[:, pr * 128:(pr + 1) * 128], qd[:, pr * 128:(pr + 1) * 128], ident_b[:]),
                              reads=[Bqd, B_const], writes=[bTq[1]])
                    for pr in range(4):
                        P.add("pe", lambda e, pr=pr: e.transpose(tkv[:, pr * 128:(pr + 1) * 128], kinv2[b][:, pr * 128:(pr + 1) * 128], ident_b[:]),
                              reads=[Bkinv2[b], B_const], writes=[bTk[1]])
                    P.add("act", lambda e: e.copy(out=QdT2[b][:], in_=tqv[:, 0:512].rearrange("p (a t) -> p a t", a=4)), reads=[bTq[1]], writes=[BQdT2[b]])
                    P.add("act", lambda e: e.copy(out=KinvT2[b][:], in_=tkv[:, 0:512].rearrange("p (a t) -> p a t", a=4)), reads=[bTk[1]], writes=[BKinvT2[b]])

                kinv2 = [kinv, cx.sb("kinvB", [128, 512], BF16)]
                Bkinv2 = [Bkinv, Buf("kinvB")]
                QdT2 = [QdT, cx.sb("QdTB", [128, 4, 128], BF16)]
                BQdT2 = [BQdT, Buf("QdTB")]
                KinvT2 = [KinvT, cx.sb("KinvTB", [128, 4, 128], BF16)]
                BKinvT2 = [BKinvT, Buf("KinvTB")]
                def mid(i):
                    b = i % 2
                    QdT, BQdT, KinvT, BKinvT, kinv, Bkinv = QdT2[b], BQdT2[b], KinvT2[b], BKinvT2[b], kinv2[b], Bkinv2[b]
                    for par, (bk_, ) in enumerate(((bE,), (bO,))):
                        pb_, Bpb_ = bk_
                        lo = par * 64
                        for pr in range(4):
                            P.add("pe", lambda e, pb_=pb_, pr=pr, lo=lo, KinvT=KinvT, QdT=QdT: e.matmul(pb_[:, pr * 128:(pr + 1) * 128], lhsT=KinvT[lo:lo + 64, pr, :], rhs=QdT[lo:lo + 64, pr, :],
                                                                               start=True, stop=True), reads=[BKinvT, BQdT], writes=[Bpb_])
                        P.add("dve", lambda e, pb_=pb_, par=par: e.tensor_tensor(out=innerT[:, par * 4:(par + 1) * 4, :], in0=pb_[:].rearrange("p (a t) -> p a t", a=4),
                                                                                in1=mask01[:].unsqueeze(1).to_broadcast([128, 4, 128]), op=ALU.mult),
                              reads=[Bpb_, Bk], writes=[Binn[par]])
                    po, Bpo = bOo
                    for h in range(8):
                        pr, par = h // 2, h % 2
                        P.add("pe", lambda e, h=h, pr=pr, par=par, b=b: e.matmul(po[:, h * 64:(h + 1) * 64], lhsT=innerT[:, par * 4 + pr, :], rhs=rv[b][:, h * 64:(h + 1) * 64],
                                                                              start=True, stop=False, skip_group_check=True), reads=[Binn[par], Brv[b]], writes=[Bpo])
                        P.add("pe", lambda e, h=h, pr=pr, QdT=QdT: e.matmul(po[:, h * 64:(h + 1) * 64], lhsT=QdT[:, pr, :], rhs=Sbf[:, h, :], start=False, stop=True, skip_group_check=True),
                              reads=[BQdT, BSbf], writes=[Bpo])
                    pS, BpS = bS
                    for h in range(8):
                        pr, par = h // 2, h % 2
                        P.add("pe", lambda e, h=h, pr=pr, par=par, b=b, kinv=kinv: e.matmul(pS[par * 64:(par + 1) * 64, pr * 64:(pr + 1) * 64], lhsT=kinv[:, h * 64:(h + 1) * 64],
                                                                              rhs=rv[b][:, h * 64:(h + 1) * 64], start=True, stop=True, skip_group_check=True),
                              reads=[Bkinv, Brv[b]], writes=[BpS])
                    P.add("dve", lambda e: e.tensor_tensor(out=St[:], in0=pS[:, 0:256].rearrange("p (a d) -> p a d", a=4), in1=St[:], op=ALU.add), reads=[BpS, BSt], writes=[BSt])
                    P.add("dve", lambda e: e.tensor_tensor(out=St[:], in0=St[:], in1=rt[:, 16:20].unsqueeze(2).to_broadcast([128, 4, 64]), op=ALU.mult), reads=[BSt, Bk], writes=[BSt])
                    sbv = Sbf[:].rearrange("p (a two) d -> p a two d", two=2)
                    P.add("pool", lambda e: e.tensor_copy(out=sbv[0:64, :, 0, :], in_=St[0:64, :, :]), reads=[BSt], writes=[BSbf])
                    P.add("pool", lambda e: e.tensor_copy(out=sbv[64:128, :, 1, :], in_=St[64:128, :, :]), reads=[BSt], writes=[BSbf])
                    P.add("act", lambda e: e.copy(out=osb[:], in_=po[:]), reads=[Bpo], writes=[Bosb])
                    P.add("act", lambda e: e.activation(out=osq[:], in_=po[:], func=AF.Square), reads=[Bpo], writes=[Bosq])

                def tail(i):
                    b = i % 2
                    o3 = osb[:].rearrange("p (h d) -> p h d", h=8)
                    P.add("dve", lambda e: e.tensor_reduce(out=stt[:, 0:8], in_=o3, axis=AX.X, op=ALU.add), reads=[Bosb], writes=[Bstt])
                    P.add("dve", lambda e: e.tensor_reduce(out=stt[:, 8:16], in_=osq[:].rearrange("p (h d) -> p h d", h=8), axis=AX.X, op=ALU.add), reads=[Bosq], writes=[Bstt])
                    P.add("dve", lambda e: e.tensor_scalar(out=stt[:, 16:24], in0=stt[:, 0:8], scalar1=1.0 / 64, scalar2=None, op0=ALU.mult), reads=[Bstt], writes=[Bstt])
                    P.add("dve", lambda e: e.tensor_tensor(out=stt[:, 32:40], in0=stt[:, 16:24], in1=stt[:, 16:24], op=ALU.mult), reads=[Bstt], writes=[Bstt])
                    P.add("dve", lambda e: e.scalar_tensor_tensor(out=stt[:, 24:32], in0=stt[:, 8:16], scalar=1.0 / 64, in1=stt[:, 32:40], op0=ALU.mult, op1=ALU.subtract),
                          reads=[Bstt], writes=[Bstt])
                    P.add("act", lambda e: e.activation(out=stt[:, 24:32], in_=stt[:, 24:32], func=AF.Sqrt, bias=eps_t[:, 0:1]), reads=[Bstt, B_const], writes=[Bstt])
                    P.add("dve", lambda e: e.reciprocal(out=stt[:, 24:32], in_=stt[:, 24:32]), reads=[Bstt], writes=[Bstt])
                    P.add("dve", lambda e: e.tensor_tensor(out=o3, in0=o3, in1=stt[:, 16:24].unsqueeze(2).to_broadcast([128, 8, 64]), op=ALU.subtract), reads=[Bosb, Bstt], writes=[Bosb])
                    P.add("dve", lambda e: e.tensor_tensor(out=o3, in0=o3, in1=stt[:, 24:32].unsqueeze(2).to_broadcast([128, 8, 64]), op=ALU.mult), reads=[Bosb, Bstt], writes=[Bosb])
                    P.add("pool", lambda e: e.tensor_tensor(out=osb[:], in0=osb[:], in1=rnw[:], op=ALU.mult), reads=[Bosb, Bk], writes=[Bosb])
                    P.add("pool", lambda e, b=b: e.tensor_tensor(out=ybf[:], in0=osb[:], in1=rgs[b][:], op=ALU.mult), reads=[Bosb, Brg[b]], writes=[Bybf])
                    tyv = bTy[0][:].bitcast(BF16)
                    for f in range(4):
                        P.add("pe", lambda e, f=f, tyv=tyv: e.transpose(tyv[:, f * 128:(f + 1) * 128], ybf[:, f * 128:(f + 1) * 128], ident_b[:]), reads=[Bybf, B_const], writes=[bTy[1]])
                    P.add("act", lambda e, tyv=tyv: e.copy(out=orst[:], in_=tyv[:, 0:512].rearrange("p (a t) -> p a t", a=4)), reads=[bTy[1]], writes=[Borst])
                    P.add("sp", lambda e, i=i: e.dma_start(out=ORv[:, :, i * 128:(i + 1) * 128], in_=orst[:]), reads=[Borst], dma_key="orst")

                load(0)
                load(1)
                front(0)
                for i in range(32):
                    mid(i)
                    if i + 1 < 32:
                        front(i + 1)
                    tail(i)
                    if i + 2 < 32:
                        load(i + 2)
                P.emit(st)

        if 3 in phases:
            with ExitStack() as st:
                P = Prog(nc, "p3")
                cx = Ctx(nc, P, st)
                cx.psum_banks(7)
                hT = [cx.sb("hT%d" % i, [128, 8, CT]) for i in range(2)]
                Bh = [[Buf("hT%d_%d" % (i, k)) for k in range(8)] for i in range(2)]
                ont = [cx.sb("ont%d" % i, [128, 4, CT], BF16) for i in range(2)]
                Bont = [Buf("ont%d" % i) for i in range(2)]
                ort = [cx.sb("ort%d" % i, [128, 4, CT], BF16) for i in range(2)]
                Bort = [Buf("ort%d" % i) for i in range(2)]
                u2c = [cx.sb("u2c%d" % i, [128, 8, CT], BF16) for i in range(2)]
                Bu2c = [Buf("u2c%d" % i) for i in range(2)]
                mixed = cx.sb("mixed", [128, 8, CT], BF16)
                Bmixed = [Buf("mixed%d" % k) for k in range(8)]
                u3 = cx.sb("u3", [128, 8, CT], BF16)
                Bu3 = [Buf("u3_%d" % k) for k in range(8)]
                hid = cx.sb("hid", [128, 22, CT], BF16)
                Bhid = [Buf("hid%d" % k) for k in range(22)]
                sa = [cx.sb("sa%d" % i, [128, CT], BF16) for i in range(2)]
                Bsa = [Buf("sa%d" % i) for i in range(2)]
                sq = [cx.sb("sq%d" % i, [128, CT], BF16) for i in range(2)]
                Bsq = [Buf("sq%d" % i) for i in range(2)]
                tmpf = [cx.sb("tmpf%d" % i, [128, CT]) for i in range(2)]
                Btmpf = [Buf("tmpf%d" % i) for i in range(2)]
                rstd = cx.sb("rstd", [128, CT])
                Brstd = Buf("rstd")
                sga = cx.sb("sga", [128, 4, CT])
                Bsga = [Buf("sga%d" % k) for k in range(4)]
                sgb = cx.sb("sgb", [128, 4, CT])
                Bsgb = [Buf("sgb%d" % k) for k in range(4)]
                ostage = cx.sb("ostage", [128, 4, D])
                Bost = Buf("ostage")
                ws = WStream(cx, "w3", 4, 5632)
                loads = []
                for c in range(NCH):
                    for mh in range(2):
                        loads.append((8, 512, wslab(w_in, 8, C_GA + mh * 512, 512)))
                        loads.append((8, 512, wslab(w_in, 8, C_GB + mh * 512, 512)))
                        loads.append((4, 512, wslab(w_nsa_up, 4, mh * 512, 512)))
                        loads.append((4, 512, wslab(w_ret_up, 4, mh * 512, 512)))
                    for mh in range(2):
                        loads.append((8, 512, wslab(w_out, 8, mh * 512, 512)))
                    loads += ffn_loads(ffn2_w_in, ffn2_w_out)
                ws.plan(loads)
                LPC = len(loads) // NCH
                h1v = h1T.rearrange("(k p) t -> p k t", p=128)
                u2v = u2T.rearrange("(k p) t -> p k t", p=128)
                ONv = ONT.rearrange("(k p) t -> p k t", p=128)
                ORv = ORT.rearrange("(k p) t -> p k t", p=128)
                outv = out.rearrange("(c j p) d -> c p j d", j=4, p=128)

                def load_chunk(c):
                    b = c % 2
                    t0 = c * CT
                    P.add("sp", lambda e: e.dma_start(out=u2c[b][:], in_=u2v[:, :, t0:t0 + CT]), writes=[Bu2c[b]], dma_key=("u2c", b))
                    P.add("sp", lambda e: e.dma_start(out=ont[b][:], in_=ONv[:, :, t0:t0 + CT]), writes=[Bont[b]], dma_key=("ont", b))
                    P.add("sp", lambda e: e.dma_start(out=ort[b][:], in_=ORv[:, :, t0:t0 + CT]), writes=[Bort[b]], dma_key=("ort", b))
                    P.add("sp", lambda e: e.dma_start(out=hT[b][:], in_=h1v[:, :, t0:t0 + CT]), writes=Bh[b], dma_key=("h1l", b))

                def mm_group(wsl, Bwsl, nk, mm, rhs_t, Brhs):
                    pg, Bpg = cx.bank()
                    for kt in range(nk):
                        P.add("pe", lambda e, kt=kt: e.matmul(pg[:], lhsT=wsl[:, kt, mm * 128:(mm + 1) * 128], rhs=rhs_t[:, kt, :],
                                                             start=(kt == 0), stop=(kt == nk - 1)),
                              reads=[Bwsl] + Brhs, writes=[Bpg])
                    return pg, Bpg

                load_chunk(0)
                for c in range(NCH):
                    b = c % 2
                    if c + 1 < NCH:
                        load_chunk(c + 1)
                    wi = c * LPC
                    for mh in range(2):
                        wsl, Bwsl = ws.get(wi)
                        for mm in range(4):
                            pg, Bpg = mm_group(wsl, Bwsl, 8, mm, u2c[b], [Bu2c[b]])
                            P.add("act", lambda e, pg=pg, mm=mm: e.activation(out=sga[:, mm, :], in_=pg[:], func=AF.Sigmoid), reads=[Bpg], writes=[Bsga[mm]])
                        wsl, Bwsl = ws.get(wi + 1)
                        for mm in range(4):
                            pg, Bpg = mm_group(wsl, Bwsl, 8, mm, u2c[b], [Bu2c[b]])
                            P.add("act", lambda e, pg=pg, mm=mm: e.activation(out=sgb[:, mm, :], in_=pg[:], func=AF.Sigmoid), reads=[Bpg], writes=[Bsgb[mm]])
                        wsl, Bwsl = ws.get(wi + 2)
                        for mm in range(4):
                            pg, Bpg = mm_group(wsl, Bwsl, 4, mm, ont[b], [Bont[b]])
                            P.add("dve", lambda e, pg=pg, mm=mm: e.tensor_tensor(out=sga[:, mm, :], in0=pg[:], in1=sga[:, mm, :], op=ALU.mult),
                                  reads=[Bpg, Bsga[mm]], writes=[Bsga[mm]])
                        wsl, Bwsl = ws.get(wi + 3)
                        for mm in range(4):
                            pg, Bpg = mm_group(wsl, Bwsl, 4, mm, ort[b], [Bort[b]])
                            P.add("dve", lambda e, pg=pg, mm=mm: e.tensor_tensor(out=sgb[:, mm, :], in0=pg[:], in1=sgb[:, mm, :], op=ALU.mult),
                                  reads=[Bpg, Bsgb[mm]], writes=[Bsgb[mm]])
                            m = mh * 4 + mm
                            P.add("pool", lambda e, m=m, mm=mm: e.tensor_tensor(out=mixed[:, m, :], in0=sga[:, mm, :], in1=sgb[:, mm, :], op=ALU.add),
                                  reads=[Bsga[mm], Bsgb[mm]], writes=[Bmixed[m]])
                        wi += 4
                    for mh in range(2):
                        wsl, Bwsl = ws.get(wi)
                        wi += 1
                        for mm in range(4):
                            m = mh * 4 + mm
                            pg, Bpg = mm_group(wsl, Bwsl, 8, mm, mixed, Bmixed)
                            P.add("dve", lambda e, pg=pg, m=m, b=b: e.scalar_tensor_tensor(out=hT[b][:, m, :], in0=pg[:], scalar=GT2[:, m:m + 1], in1=hT[b][:, m, :],
                                                                                        op0=ALU.mult, op1=ALU.add),
                                  reads=[Bpg, Bh[b][m], B_coef], writes=[Bh[b][m]])
                    rms_mod(cx, P, hT[b], Bh[b], u3, Bu3, G3, SH3, sq, Bsq, rstd, Brstd, tmpf, Btmpf)
                    wi = ffn(cx, P, ws, wi, u3, Bu3, hT[b], Bh[b], hid, Bhid, sa, Bsa, HG3)
                    rms_stats(cx, P, hT[b], Bh[b], sq, Bsq, rstd, Brstd)
                    for kt in range(8):
                        P.add("dve", lambda e, kt=kt, b=b: e.scalar_tensor_tensor(out=hT[b][:, kt, :], in0=hT[b][:, kt, :], scalar=FW[:, kt:kt + 1], in1=rstd[:],
                                                                                 op0=ALU.mult, op1=ALU.mult),
                              reads=[Bh[b][kt], Brstd, B_coef], writes=[Bh[b][kt]])
                    for j in range(4):
                        for a in range(2):
                            pt, Bpt = cx.bank()
                            for q in range(4):
                                kt = a * 4 + q
                                P.add("pe", lambda e, pt=pt, j=j, kt=kt, q=q, b=b: e.transpose(pt[:, q * 128:(q + 1) * 128], hT[b][:, kt, j * 128:(j + 1) * 128], ident_f[:]),
                                      reads=[Bh[b][kt], B_const], writes=[Bpt])
                            dst = ostage[:, j, a * 512:(a + 1) * 512]
                            if (j * 2 + a) % 2 == 0:
                                P.add("act", lambda e, dst=dst, pt=pt: e.copy(out=dst, in_=pt[:]), reads=[Bpt], writes=[Bost])
                            else:
                                P.add("dve", lambda e, dst=dst, pt=pt: e.tensor_copy(out=dst, in_=pt[:]), reads=[Bpt], writes=[Bost])
                    P.add("sp", lambda e, c=c: e.dma_start(out=outv[c], in_=ostage[:]), reads=[Bost], dma_key="ost")
                P.emit(st)
    return nc


def host_tables():
    pos = np.arange(S, dtype=np.float32)
    inv = (10000.0 ** (-np.arange(32, dtype=np.float32) / 32)).astype(np.float32)
    ang = pos[:, None] * inv[None, :]
    tab = np.concatenate([np.cos(ang), np.sin(ang)], axis=1).astype(np.float32)
    rope_tab = np.ascontiguousarray(tab.reshape(32, 128, 64).transpose(1, 0, 2))
    H = 8
    log_g = np.log(1.0 - 2.0 ** (-5.0 - np.arange(H, dtype=np.float64)))
    p = np.arange(128, dtype=np.float64)
    qdec = np.exp(log_g[None, :] * (p[:, None] + 1.0))
    kinv = np.exp(-log_g[None, :] * (p[:, None] + 1.0)) * (64 ** -0.5)
    gC = np.exp(log_g * 128.0)
    gcp = np.zeros((128, 8))
    for pr in range(4):
        gcp[0:64, pr] = gC[2 * pr]
        gcp[64:128, pr] = gC[2 * pr + 1]
    ret_tab = np.concatenate([qdec, kinv, gcp], axis=1).astype(np.float32)
    return rope_tab, ret_tab


def make_in_maps(inputs):
    f = lambda a: np.ascontiguousarray(np.asarray(a, dtype=np.float32))
    colT = lambda v: np.ascontiguousarray(f(v).reshape(-1, 128).T)
    rope_tab, ret_tab = host_tables()
    nwT = np.concatenate([colT(inputs["norm1_w"][0]), colT(inputs["norm2_w"][0]), colT(inputs["norm3_w"][0]), colT(inputs["final_norm_w"])], axis=1)
    shared = {
        "ada_w": f(inputs["ada_w"][0]), "ada_bT": colT(inputs["ada_b"][0]), "nwT": np.ascontiguousarray(nwT),
        "ffn1_w_in": f(inputs["ffn1_w_in"][0]), "ffn1_w_out": f(inputs["ffn1_w_out"][0]), "w_in": f(inputs["w_in"][0]),
        "cmp_k_pe": f(inputs["cmp_k_pe"][0]), "cmp_k_w1": f(inputs["cmp_k_w1"][0]), "cmp_k_w2": f(inputs["cmp_k_w2"][0]),
        "cmp_v_pe": f(inputs["cmp_v_pe"][0]), "cmp_v_w1": f(inputs["cmp_v_w1"][0]), "cmp_v_w2": f(inputs["cmp_v_w2"][0]),
        "ret_norm_w": f(inputs["ret_norm_w"][0]).reshape(1, 512), "w_nsa_up": f(inputs["w_nsa_up"][0]), "w_ret_up": f(inputs["w_ret_up"][0]),
        "w_out": f(inputs["w_out"][0]), "ffn2_w_in": f(inputs["ffn2_w_in"][0]), "ffn2_w_out": f(inputs["ffn2_w_out"][0]),
        "rope_tab": rope_tab, "ret_tab": ret_tab,
    }
    maps = []
    for b in range(8):
        m = dict(shared)
        m["x"] = f(inputs["x"][b])
        m["cT"] = colT(inputs["c"][b])
        maps.append(m)
    return maps


def kernel(**inputs):
    nc = build()
    in_maps = make_in_maps(inputs)
    res = run_bass_kernel_spmd(nc, in_maps, core_ids=list(range(8)))
    return np.stack([np.asarray(r["out"], dtype=np.float32) for r in res.results], axis=0)
```

```python
import numpy as np
from contextlib import ExitStack
import concourse.bass as bass
import concourse.mybir as mybir
from concourse.bass_utils import run_bass_kernel_spmd

F32 = mybir.dt.float32
BF16 = mybir.dt.bfloat16
AF = mybir.ActivationFunctionType
ALU = mybir.AluOpType
AX = mybir.AxisListType

S = 4096
D = 1024
DFF = 2816
INW = 5400
NCH = 8
CT = 512
EPS = 1e-6
NEGM = -30000.0
ENGS = ["pe", "act", "dve", "pool", "sp"]

C_NQ, C_NKC, C_NVC, C_NKS, C_NVS, C_NKW, C_NVW, C_NGT, C_RQ, C_RK, C_RV, C_RG, C_GA, C_GB = (
    0, 512, 640, 768, 896, 1024, 1152, 1280, 1304, 1816, 2328, 2840, 3352, 4376)


class Buf:
    __slots__ = ("name", "last_w", "readers", "excl")

    def __init__(self, name, excl=False):
        self.name = name
        self.last_w = None
        self.readers = []
        self.excl = excl


class Op:
    __slots__ = ("eng", "fn", "deps", "signal", "val", "dma_key", "dma_val", "prog")

    def __init__(self, eng, fn):
        self.prog = None
        self.eng = eng
        self.fn = fn
        self.deps = []
        self.signal = False
        self.val = None
        self.dma_key = None
        self.dma_val = None


class Prog:
    def __init__(self, nc, name):
        self.nc = nc
        self.name = name
        self.ops = {e: [] for e in ENGS}
        self.dma_cnt = {}
        self.dma_last = {}

    def add(self, eng, fn, reads=(), writes=(), dma_key=None):
        op = Op(eng, fn)
        op.prog = self
        deps = []
        for b in reads:
            if b.last_w is not None:
                deps.append(b.last_w)
            if b.excl:
                deps.extend(b.readers)
        for b in writes:
            if b.last_w is not None:
                deps.append(b.last_w)
            deps.extend(b.readers)
        seen = set()
        for d in deps:
            if d is op or id(d) in seen or d.prog is not self:
                continue
            seen.add(id(d))
            if d.dma_key is None and d.eng == eng and eng == "pe":
                continue
            op.deps.append(d)
            if d.dma_key is None:
                d.signal = True
        if dma_key is not None:
            op.dma_key = dma_key
            self.dma_cnt[dma_key] = self.dma_cnt.get(dma_key, 0) + 16
            op.dma_val = self.dma_cnt[dma_key]
            self.dma_last[dma_key] = op
        for b in reads:
            b.readers.append(op)
        for b in writes:
            b.last_w = op
            b.readers = []
        self.ops[eng].append(op)
        return op

    def finish(self):
        op = Op("sp", lambda e: e.nop())
        op.deps = list(self.dma_last.values())
        self.ops["sp"].append(op)

    def emit(self, stack):
        nc = self.nc
        self.finish()
        esem = {e: stack.enter_context(nc.semaphore("s_%s_%s" % (self.name, e))) for e in ENGS}
        dsem = {k: stack.enter_context(nc.semaphore("d_%s_%s" % (self.name, str(k)))) for k in self.dma_cnt}
        for e in ENGS:
            c = 0
            for op in self.ops[e]:
                if op.dma_key is None and op.signal:
                    c += 1
                    op.val = c
        block = stack.enter_context(nc.Block())

        def run(e, engobj):
            waited = {}
            for op in self.ops[e]:
                for d in op.deps:
                    if d.dma_key is not None:
                        s, v = dsem[d.dma_key], d.dma_val
                    else:
                        s, v = esem[d.eng], d.val
                    key = id(s)
                    if waited.get(key, 0) >= v:
                        continue
                    waited[key] = v
                    engobj.wait_ge(s, v)
                ins = op.fn(engobj)
                if op.dma_key is not None:
                    ins.then_inc(dsem[op.dma_key], 16)
                elif op.signal:
                    ins.then_inc(esem[e], 1)

        @block.tensor
        def _(eng):
            run("pe", eng)

        @block.scalar
        def _(eng):
            run("act", eng)

        @block.vector
        def _(eng):
            run("dve", eng)

        @block.gpsimd
        def _(eng):
            run("pool", eng)

        @block.sync
        def _(eng):
            run("sp", eng)


class Ctx:
    def __init__(self, nc, P, st):
        self.nc, self.P, self.st = nc, P, st
        self.banks = []
        self.bank_i = 0

    def sb(self, name, shape, dt=F32):
        return self.st.enter_context(self.nc.sbuf_tensor(self.P.name + "_" + name, shape, dt))

    def psum_banks(self, n, ncols=512, dt=F32):
        for i in range(n):
            t = self.st.enter_context(self.nc.psum_tensor("%s_ps%d" % (self.P.name, len(self.banks)), [128, ncols], dt))
            self.banks.append((t, Buf("ps%d" % len(self.banks), excl=True)))

    def bank(self):
        b = self.banks[self.bank_i % len(self.banks)]
        self.bank_i += 1
        return b


class WStream:
    def __init__(self, cx, name, nbuf, slot_elems, dt=BF16, eng="pool"):
        self.cx, self.name, self.nbuf, self.eng = cx, name, nbuf, eng
        self.tiles = [cx.sb("%s%d" % (name, i), [128, slot_elems], dt) for i in range(nbuf)]
        self.bufs = [Buf("%s%d" % (name, i)) for i in range(nbuf)]
        self.loads = []
        self.issued = 0

    def plan(self, loads):
        self.loads = loads

    def _issue(self, i):
        kt, ncols, src = self.loads[i]
        slot = i % self.nbuf
        view = self.tiles[slot][:, 0:kt * ncols].rearrange("p (k c) -> p k c", k=kt)
        self.cx.P.add(self.eng, lambda e, view=view, src=src: e.dma_start(out=view, in_=src),
                      writes=[self.bufs[slot]], dma_key=(self.name, slot))

    def get(self, i):
        while self.issued < min(len(self.loads), i + self.nbuf - 1):
            self._issue(self.issued)
            self.issued += 1
        kt, ncols, src = self.loads[i]
        slot = i % self.nbuf
        view = self.tiles[slot][:, 0:kt * ncols].rearrange("p (k c) -> p k c", k=kt)
        return view, self.bufs[slot]


def wslab(w_ap, kt, c0, ncols):
    return w_ap.rearrange("(k p) n -> p k n", p=128)[:, 0:kt, c0:c0 + ncols]


def build(debug=False, phases=(0, 1, 2, 3)):
    nc = bass.Bass("TRN2", target_bir_lowering=False)
    dram_in = lambda name, shape, dt=F32: nc.dram_tensor(name, shape, dt, kind="ExternalInput").ap()
    x = dram_in("x", [S, D])
    cT = dram_in("cT", [128, 8])
    ada_w = dram_in("ada_w", [D, 9 * D])
    ada_bT = dram_in("ada_bT", [128, 72])
    nwT = dram_in("nwT", [128, 32])
    ffn1_w_in = dram_in("ffn1_w_in", [D, 2 * DFF])
    ffn1_w_out = dram_in("ffn1_w_out", [DFF, D])
    w_in = dram_in("w_in", [D, INW])
    cmp_k_pe = dram_in("cmp_k_pe", [32, 64])
    cmp_k_w1 = dram_in("cmp_k_w1", [2048, 128])
    cmp_k_w2 = dram_in("cmp_k_w2", [128, 64])
    cmp_v_pe = dram_in("cmp_v_pe", [32, 64])
    cmp_v_w1 = dram_in("cmp_v_w1", [2048, 128])
    cmp_v_w2 = dram_in("cmp_v_w2", [128, 64])
    ret_norm_w = dram_in("ret_norm_w", [1, 512])
    w_nsa_up = dram_in("w_nsa_up", [512, D])
    w_ret_up = dram_in("w_ret_up", [512, D])
    w_out = dram_in("w_out", [D, D])
    ffn2_w_in = dram_in("ffn2_w_in", [D, 2 * DFF])
    ffn2_w_out = dram_in("ffn2_w_out", [DFF, D])
    rope_tab = dram_in("rope_tab", [128, 32, 64])
    ret_tab = dram_in("ret_tab", [128, 24])
    out = nc.dram_tensor("out", [S, D], F32, kind="ExternalOutput").ap()

    skind = "ExternalOutput" if debug else "Internal"
    scr = lambda name, shape, dt: nc.dram_tensor(name, shape, dt, kind=skind).ap()
    h1T = scr("h1T", [D, S], F32)
    u2T = scr("u2T", [D, S], BF16)
    QT = scr("QT", [8, 64, S], BF16)
    KVT = scr("KVT", [4, 128, S], BF16)
    TOKF = scr("TOKF", [S, 1560], F32)
    TOKB = scr("TOKB", [S, 768], BF16)
    ONT = scr("ONT", [512, S], BF16)
    ORT = scr("ORT", [512, S], BF16)
    modT_d = scr("modT_d", [128, 72], F32)
    GT = scr("GT", [24, S], F32)

    with ExitStack() as gst:
        modT = gst.enter_context(nc.sbuf_tensor("modT", [128, 72], F32))
        coef = gst.enter_context(nc.sbuf_tensor("coef", [128, 96], F32))
        ones_bf = gst.enter_context(nc.sbuf_tensor("ones_bf", [128, 128], BF16))
        ident_f = gst.enter_context(nc.sbuf_tensor("ident_f", [128, 128], F32))
        ident_b = gst.enter_context(nc.sbuf_tensor("ident_b", [128, 128], BF16))
        B_modT, B_coef, B_const = Buf("modT"), Buf("coef"), Buf("const")
        G1, SH1, HG1, G2, SH2, GT2, G3, SH3, HG3, FW = [coef[:, i * 8:(i + 1) * 8] for i in range(10)]

        with ExitStack() as st:
            P = Prog(nc, "p0")
            cx = Ctx(nc, P, st)
            cx.psum_banks(4)
            pm, Bpm = cx.banks.pop(0)
            csb = cx.sb("csb", [128, 8])
            sil = cx.sb("sil", [128, 8])
            abT = cx.sb("abT", [128, 72])
            nw = cx.sb("nw", [128, 32])
            tmp = cx.sb("tmp", [128, 8])
            Bc, Bs, Bab, Bnw, Btmp = Buf("c"), Buf("sil"), Buf("ab"), Buf("nw"), Buf("tmp")
            P.add("sp", lambda e: e.dma_start(out=csb[:], in_=cT), writes=[Bc], dma_key="c")
            P.add("sp", lambda e: e.dma_start(out=abT[:], in_=ada_bT), writes=[Bab], dma_key="ab")
            P.add("sp", lambda e: e.dma_start(out=nw[:], in_=nwT), writes=[Bnw], dma_key="nw")
            P.add("pool", lambda e: e.memset(ones_bf[:], 1.0), writes=[B_const])
            P.add("pool", lambda e: e.memset(ident_f[:], 1.0), writes=[B_const])
            P.add("pool", lambda e: e.affine_select(out=ident_f[:], in_=ident_f[:], pattern=[[-1, 128]], compare_op=ALU.is_equal,
                                                    fill=0.0, base=0, channel_multiplier=1), reads=[B_const], writes=[B_const])
            P.add("pool", lambda e: e.tensor_copy(out=ident_b[:], in_=ident_f[:]), reads=[B_const], writes=[B_const])
            P.add("act", lambda e: e.activation(out=sil[:], in_=csb[:], func=AF.Silu), reads=[Bc], writes=[Bs])
            modrow = cx.sb("modrow", [1, 9216])
            Bmr = Buf("modrow")
            one_f = cx.sb("one_f", [1, 1])
            P.add("pool", lambda e: e.memset(one_f[:], 1.0), writes=[B_const])
            ws = WStream(cx, "ada", 4, 4096, dt=F32, eng="sp")
            ws.plan([(8, 512, wslab(ada_w, 8, s_ * 512, 512)) for s_ in range(18)])
            for s_ in range(18):
                wv, Bw = ws.get(s_)
                pr_, Bpr_ = cx.bank()
                for k in range(8):
                    P.add("pe", lambda e, wv=wv, k=k, pr_=pr_: e.matmul(pr_[0:1, :], lhsT=sil[:, k:k + 1], rhs=wv[:, k, :], start=(k == 0), stop=(k == 7)),
                          reads=[Bw, Bs], writes=[Bpr_])
                if s_ % 2 == 0:
                    P.add("act", lambda e, pr_=pr_, s_=s_: e.copy(out=modrow[0:1, s_ * 512:(s_ + 1) * 512], in_=pr_[0:1, :]), reads=[Bpr_], writes=[Bmr])
                else:
                    P.add("dve", lambda e, pr_=pr_, s_=s_: e.tensor_copy(out=modrow[0:1, s_ * 512:(s_ + 1) * 512], in_=pr_[0:1, :]), reads=[Bpr_], writes=[Bmr])
            for j in range(72):
                P.add("pe", lambda e, j=j: e.matmul(pm[:, j:j + 1], lhsT=modrow[0:1, j * 128:(j + 1) * 128], rhs=one_f[0:1, 0:1], start=True, stop=True, skip_group_check=True),
                      reads=[Bmr, B_const], writes=[Bpm])
            P.add("dve", lambda e: e.tensor_tensor(out=modT[:], in0=pm[:, 0:72], in1=abT[:], op=ALU.add), reads=[Bpm, Bab], writes=[B_modT])
            mv = lambda v: modT[:, v * 8:(v + 1) * 8]
            for (gi, nwi, sci) in ((G1, 0, 1), (G2, 1, 4), (G3, 2, 7)):
                P.add("dve", lambda e, sci=sci: e.tensor_scalar(out=tmp[:], in0=mv(sci), scalar1=1.0, scalar2=None, op0=ALU.add),
                      reads=[B_modT], writes=[Btmp])
                P.add("dve", lambda e, gi=gi, nwi=nwi: e.tensor_tensor(out=gi, in0=tmp[:], in1=nw[:, nwi * 8:(nwi + 1) * 8], op=ALU.mult),
                      reads=[Btmp, Bnw], writes=[B_coef])
            for (dst, src, sc) in ((SH1, 0, 1.0), (HG1, 2, 0.5), (SH2, 3, 1.0), (GT2, 5, 1.0), (SH3, 6, 1.0), (HG3, 8, 0.5)):
                P.add("dve", lambda e, dst=dst, src=src, sc=sc: e.tensor_scalar(out=dst, in0=mv(src), scalar1=sc, scalar2=None, op0=ALU.mult),
                      reads=[B_modT], writes=[B_coef])
            P.add("dve", lambda e: e.tensor_copy(out=FW, in_=nw[:, 24:32]), reads=[Bnw], writes=[B_coef])
            if debug:
                P.add("sp", lambda e: e.dma_start(out=modT_d, in_=modT[:]), reads=[B_modT], dma_key="dbg")
            P.emit(st)

        def rms_stats(cx, P, hT, Bh, sq, Bsq, rstd, Brstd):
            pss, Bpss = cx.bank()
            for kt in range(8):
                P.add("act", lambda e, kt=kt: e.activation(out=sq[kt % 2][:], in_=hT[:, kt, :], func=AF.Square),
                      reads=[Bh[kt]], writes=[Bsq[kt % 2]])
                P.add("pe", lambda e, kt=kt: e.matmul(pss[:], lhsT=ones_bf[:], rhs=sq[kt % 2][:], start=(kt == 0), stop=(kt == 7)),
                      reads=[Bsq[kt % 2], B_const], writes=[Bpss])
            P.add("act", lambda e: e.activation(out=rstd[:], in_=pss[:], func=AF.Sqrt, bias=eps_t[:, 0:1], scale=1.0 / D),
                  reads=[Bpss, B_const], writes=[Brstd])
            P.add("dve", lambda e: e.reciprocal(out=rstd[:], in_=rstd[:]), reads=[Brstd], writes=[Brstd])

        def rms_mod(cx, P, hT, Bh, uT, Bu, g, sh, sq, Bsq, rstd, Brstd, tmpf, Btmpf):
            rms_stats(cx, P, hT, Bh, sq, Bsq, rstd, Brstd)
            for kt in range(8):
                P.add("dve", lambda e, kt=kt: e.tensor_tensor(out=tmpf[kt % 2][:], in0=hT[:, kt, :], in1=rstd[:], op=ALU.mult),
                      reads=[Bh[kt], Brstd], writes=[Btmpf[kt % 2]])
                P.add("act", lambda e, kt=kt: e.activation(out=uT[:, kt, :], in_=tmpf[kt % 2][:], func=AF.Identity,
                                                           bias=sh[:, kt:kt + 1], scale=g[:, kt:kt + 1]),
                      reads=[Btmpf[kt % 2], B_coef], writes=[Bu[kt]])

        def ffn(cx, P, ws, wi0, uT, Bu, hT, Bh, hidT, Bhid, sa, Bsa, hg):
            wi = wi0
            for s6 in range(6):
                ncols = 512 if s6 < 5 else 256
                wa, Bwa = ws.get(wi)
                wb, Bwb = ws.get(wi + 1)
                wi += 2
                for jj in range(ncols // 128):
                    j = s6 * 4 + jj
                    pa, Bpa = cx.bank()
                    pb, Bpb = cx.bank()
                    for kt in range(8):
                        P.add("pe", lambda e, pa=pa, wa=wa, jj=jj, kt=kt: e.matmul(pa[:], lhsT=wa[:, kt, jj * 128:(jj + 1) * 128], rhs=uT[:, kt, :],
                                                                                  start=(kt == 0), stop=(kt == 7)),
                              reads=[Bwa, Bu[kt]], writes=[Bpa])
                    for kt in range(8):
                        P.add("pe", lambda e, pb=pb, wb=wb, jj=jj, kt=kt: e.matmul(pb[:], lhsT=wb[:, kt, jj * 128:(jj + 1) * 128], rhs=uT[:, kt, :],
                                                                                  start=(kt == 0), stop=(kt == 7)),
                              reads=[Bwb, Bu[kt]], writes=[Bpb])
                    P.add("act", lambda e, pa=pa, j=j: e.activation(out=sa[j % 2][:], in_=pa[:], func=AF.Silu),
                          reads=[Bpa], writes=[Bsa[j % 2]])
                    P.add("dve", lambda e, pb=pb, j=j: e.tensor_tensor(out=hidT[:, j, :], in0=pb[:], in1=sa[j % 2][:], op=ALU.mult),
                          reads=[Bpb, Bsa[j % 2]], writes=[Bhid[j]])
            for m2 in range(4):
                wo, Bwo = ws.get(wi)
                wi += 1
                for mm in range(2):
                    m = m2 * 2 + mm
                    py, Bpy = cx.bank()
                    for ht in range(22):
                        P.add("pe", lambda e, py=py, wo=wo, mm=mm, ht=ht: e.matmul(py[:], lhsT=wo[:, ht, mm * 128:(mm + 1) * 128], rhs=hidT[:, ht, :],
                                                                                  start=(ht == 0), stop=(ht == 21)),
                              reads=[Bwo, Bhid[ht]], writes=[Bpy])
                    P.add("dve", lambda e, py=py, m=m: e.scalar_tensor_tensor(out=hT[:, m, :], in0=py[:], scalar=hg[:, m:m + 1], in1=hT[:, m, :],
                                                                            op0=ALU.mult, op1=ALU.add),
                          reads=[Bpy, Bh[m], B_coef], writes=[Bh[m]])
            return wi

        def ffn_loads(w_in_ap, w_out_ap):
            L = []
            for s6 in range(6):
                ncols = 512 if s6 < 5 else 256
                L.append((8, ncols, wslab(w_in_ap, 8, s6 * 512, ncols)))
                L.append((8, ncols, wslab(w_in_ap, 8, DFF + s6 * 512, ncols)))
            for m2 in range(4):
                L.append((22, 256, wslab(w_out_ap, 22, m2 * 256, 256)))
            return L

        eps_t = gst.enter_context(nc.sbuf_tensor("eps_t", [128, 1], F32))

        if 1 in phases:
            with ExitStack() as st:
                P = Prog(nc, "p1")
                cx = Ctx(nc, P, st)
                cx.psum_banks(7)
                P.add("pool", lambda e: e.memset(eps_t[:], EPS), writes=[B_const])
                xs = [cx.sb("xs%d" % i, [128, 4, D]) for i in range(1)] * 2
                Bxs = [Buf("xs%d" % i) for i in range(1)] * 2
                hT = [cx.sb("hT%d" % i, [128, 8, CT]) for i in range(2)]
                Bh = [[Buf("hT%d_%d" % (i, k)) for k in range(8)] for i in range(2)]
                u1 = cx.sb("u1", [128, 8, CT], BF16)
                Bu1 = [Buf("u1_%d" % k) for k in range(8)]
                u2 = cx.sb("u2", [128, 8, CT], BF16)
                Bu2 = [Buf("u2_%d" % k) for k in range(8)]
                hid = cx.sb("hid", [128, 22, CT], BF16)
                Bhid = [Buf("hid%d" % k) for k in range(22)]
                sa = [cx.sb("sa%d" % i, [128, CT], BF16) for i in range(2)]
                Bsa = [Buf("sa%d" % i) for i in range(2)]
                sq = [cx.sb("sq%d" % i, [128, CT], BF16) for i in range(2)]
                Bsq = [Buf("sq%d" % i) for i in range(2)]
                tmpf = [cx.sb("tmpf%d" % i, [128, CT]) for i in range(2)]
                Btmpf = [Buf("tmpf%d" % i) for i in range(2)]
                rstd = cx.sb("rstd", [128, CT])
                Brstd = Buf("rstd")
                qst = cx.sb("qst", [64, 8, CT], BF16)
                Bqst = Buf("qst")
                kvst = cx.sb("kvst", [128, 4, CT], BF16)
                Bkvst = Buf("kvst")
                gst = cx.sb("gst", [24, CT])
                Bgst = Buf("gst")
                tokf = cx.sb("tokf", [128, 4, 1560])
                Btokf = Buf("tokf")
                tokb = cx.sb("tokb", [128, 4, 768], BF16)
                Btokb = Buf("tokb")
                ws = WStream(cx, "w1", 4, 5632)
                PSL = [(C_NQ, 512), (C_NKC, 512), (C_NKW, 280), (C_RQ, 512), (C_RK, 512), (C_RV, 512), (C_RG, 512)]
                loads = []
                for c in range(NCH):
                    loads += ffn_loads(ffn1_w_in, ffn1_w_out)
                    loads += [(8, n, wslab(w_in, 8, c0, n)) for (c0, n) in PSL]
                ws.plan(loads)
                LPC = len(loads) // NCH
                xv = x.rearrange("(c j p) d -> c p j d", j=4, p=128)
                h1v = h1T.rearrange("(k p) t -> p k t", p=128)
                u2v = u2T.rearrange("(k p) t -> p k t", p=128)
                QTv = QT.rearrange("h d t -> d h t")
                KVTv = KVT.rearrange("f p t -> p f t")
                for c in range(NCH):
                    b = c % 2
                    t0 = c * CT
                    P.add("sp", lambda e, c=c, b=b: e.dma_start(out=xs[b][:], in_=xv[c]), writes=[Bxs[b]], dma_key="xs")
                    for j in range(4):
                        for a in range(2):
                            pt, Bpt = cx.bank()
                            for q in range(4):
                                kt = a * 4 + q
                                P.add("pe", lambda e, pt=pt, j=j, kt=kt, q=q, b=b: e.transpose(pt[:, q * 128:(q + 1) * 128], xs[b][:, j, kt * 128:(kt + 1) * 128], ident_f[:]),
                                      reads=[Bxs[b], B_const], writes=[Bpt])
                            eng = "act" if (j * 2 + a) % 2 == 0 else "dve"
                            dst = hT[b][:, a * 4:(a + 1) * 4, j * 128:(j + 1) * 128]
                            srcv = pt[:].rearrange("p (q t) -> p q t", q=4)
                            if eng == "act":
                                P.add("act", lambda e, dst=dst, srcv=srcv: e.copy(out=dst, in_=srcv), reads=[Bpt], writes=Bh[b][a * 4:(a + 1) * 4])
                            else:
                                P.add("dve", lambda e, dst=dst, srcv=srcv: e.tensor_copy(out=dst, in_=srcv), reads=[Bpt], writes=Bh[b][a * 4:(a + 1) * 4])
                    rms_mod(cx, P, hT[b], Bh[b], u1, Bu1, G1, SH1, sq, Bsq, rstd, Brstd, tmpf, Btmpf)
                    wi = ffn(cx, P, ws, c * LPC, u1, Bu1, hT[b], Bh[b], hid, Bhid, sa, Bsa, HG1)
                    P.add("sp", lambda e, b=b, t0=t0: e.dma_start(out=h1v[:, :, t0:t0 + CT], in_=hT[b][:]), reads=Bh[b], dma_key=("h1", b))
                    rms_mod(cx, P, hT[b], Bh[b], u2, Bu2, G2, SH2, sq, Bsq, rstd, Brstd, tmpf, Btmpf)
                    P.add("sp", lambda e, t0=t0: e.dma_start(out=u2v[:, :, t0:t0 + CT], in_=u2[:]), reads=Bu2, dma_key="u2st")
                    TOKFv = TOKF.rearrange("(c j p) n -> c p j n", j=4, p=128)
                    TOKBv = TOKB.rearrange("(c j p) n -> c p j n", j=4, p=128)

                    def tokmm(wsl, Bwsl, cc, ncol, evac):
                        for j in range(4):
                            pp, Bpp = cx.bank()
                            for kt in range(8):
                                P.add("pe", lambda e, pp=pp, kt=kt, j=j: e.matmul(
                                    pp[:, 0:ncol], lhsT=u2[:, kt, j * 128:(j + 1) * 128], rhs=wsl[:, kt, cc:cc + ncol],
                                    start=(kt == 0), stop=(kt == 7)), reads=[Bwsl, Bu2[kt]], writes=[Bpp])
                            evac(pp, Bpp, j)

                    def ev_b(off, ncol):
                        def f(pp, Bpp, j):
                            P.add("dve", lambda e: e.tensor_copy(out=tokb[:, j, off:off + ncol], in_=pp[:, 0:ncol]), reads=[Bpp], writes=[Btokb])
                        return f

                    def ev_f(off, fn, eng):
                        def f(pp, Bpp, j):
                            if eng == "dve":
                                P.add("dve", lambda e: e.tensor_copy(out=tokf[:, j, off:off + 512], in_=pp[:, 0:512]), reads=[Bpp], writes=[Btokf])
                            else:
                                P.add("act", lambda e: e.activation(out=tokf[:, j, off:off + 512], in_=pp[:, 0:512], func=fn), reads=[Bpp], writes=[Btokf])
                        return f

                    def ev_vwg(pp, Bpp, j):
                        P.add("dve", lambda e: e.tensor_copy(out=tokb[:, j, 128:256], in_=pp[:, 0:128]), reads=[Bpp], writes=[Btokb])
                        P.add("act", lambda e: e.activation(out=tokf[:, j, 1536:1560], in_=pp[:, 128:152], func=AF.Sigmoid), reads=[Bpp], writes=[Btokf])

                    def featmm(wsl, Bwsl, cc, f):
                        pk, Bpk = cx.bank()
                        for kt in range(8):
                            P.add("pe", lambda e, kt=kt: e.matmul(pk[:], lhsT=wsl[:, kt, cc:cc + 128], rhs=u2[:, kt, :], start=(kt == 0), stop=(kt == 7)),
                                  reads=[Bwsl, Bu2[kt]], writes=[Bpk])
                        if f % 2 == 0:
                            P.add("act", lambda e: e.copy(out=kvst[:, f, :], in_=pk[:]), reads=[Bpk], writes=[Bkvst])
                        else:
                            P.add("dve", lambda e: e.tensor_copy(out=kvst[:, f, :], in_=pk[:]), reads=[Bpk], writes=[Bkvst])

                    wq, Bwq = ws.get(wi)
                    for h in range(8):
                        pq, Bpq = cx.bank()
                        for kt in range(8):
                            P.add("pe", lambda e, pq=pq, h=h, kt=kt, wq=wq: e.matmul(pq[0:64, :], lhsT=wq[:, kt, h * 64:(h + 1) * 64], rhs=u2[:, kt, :],
                                                                             start=(kt == 0), stop=(kt == 7)),
                                  reads=[Bwq, Bu2[kt]], writes=[Bpq])
                        if h % 2 == 0:
                            P.add("act", lambda e, pq=pq, h=h: e.mul(out=qst[:, h, :], in_=pq[0:64, :], mul=0.125), reads=[Bpq], writes=[Bqst])
                        else:
                            P.add("dve", lambda e, pq=pq, h=h: e.tensor_scalar(out=qst[:, h, :], in0=pq[0:64, :], scalar1=0.125, scalar2=None, op0=ALU.mult),
                                  reads=[Bpq], writes=[Bqst])
                    P.add("sp", lambda e, t0=t0: e.dma_start(out=QTv[:, :, t0:t0 + CT], in_=qst[:]), reads=[Bqst], dma_key="qst")
                    wk1, Bwk1 = ws.get(wi + 1)
                    featmm(wk1, Bwk1, 0, 0)
                    featmm(wk1, Bwk1, 128, 1)
                    featmm(wk1, Bwk1, 256, 2)
                    tokmm(wk1, Bwk1, 384, 128, ev_b(0, 128))
                    wk2, Bwk2 = ws.get(wi + 2)
                    featmm(wk2, Bwk2, 0, 3)
                    pg_, Bpg_ = cx.bank()
                    for kt in range(8):
                        P.add("pe", lambda e, kt=kt, pg_=pg_, wk2=wk2: e.matmul(pg_[0:24, :], lhsT=wk2[:, kt, 256:280], rhs=u2[:, kt, :], start=(kt == 0), stop=(kt == 7)),
                              reads=[Bwk2, Bu2[kt]], writes=[Bpg_])
                    P.add("act", lambda e, pg_=pg_: e.activation(out=gst[:], in_=pg_[0:24, :], func=AF.Sigmoid), reads=[Bpg_], writes=[Bgst])
                    P.add("sp", lambda e, t0=t0: e.dma_start(out=GT[:, t0:t0 + CT], in_=gst[:]), reads=[Bgst], dma_key="gst")
                    P.add("sp", lambda e, t0=t0: e.dma_start(out=KVTv[:, :, t0:t0 + CT], in_=kvst[:]), reads=[Bkvst], dma_key="kvst")
                    tokmm(wk2, Bwk2, 128, 152, ev_vwg)
                    wr, Bwr = ws.get(wi + 3)
                    tokmm(wr, Bwr, 0, 512, ev_f(0, None, "dve"))
                    wr, Bwr = ws.get(wi + 4)
                    tokmm(wr, Bwr, 0, 512, ev_f(512, AF.Copy, "act"))
                    wr, Bwr = ws.get(wi + 5)
                    tokmm(wr, Bwr, 0, 512, ev_b(256, 512))
                    wr, Bwr = ws.get(wi + 6)
                    tokmm(wr, Bwr, 0, 512, ev_f(1024, AF.Silu, "act"))
                    P.add("sp", lambda e, c=c: e.dma_start(out=TOKFv[c], in_=tokf[:]), reads=[Btokf], dma_key="tokf")
                    P.add("sp", lambda e, c=c: e.dma_start(out=TOKBv[c], in_=tokb[:]), reads=[Btokb], dma_key="tokb")
                P.emit(st)

        if 2 in phases:
            with ExitStack() as st:
                P = Prog(nc, "p2a")
                cx = Ctx(nc, P, st)
                cx.psum_banks(8)
                Sbanks = cx.banks[0:3]
                Obanks = cx.banks[3:5]
                Cbanks = cx.banks[5:7]
                Mbank = cx.banks[7]
                rr = {"s": 0, "o": 0, "pt": 0}
                Qaug = [cx.sb("qaug%d" % i, [128, S], BF16) for i in range(4)]
                BQ = [[Buf("q%d_%d" % (i, c)) for c in range(NCH)] for i in range(4)]
                Ksl = cx.sb("ksl", [128, S], BF16)
                BKsl = Buf("ksl")
                Kw = cx.sb("kw", [128, S], BF16)
                BKw = Buf("kw")
                Vs = cx.sb("vs", [128, 32, 65], BF16)
                BVs = Buf("vs")
                Vw = cx.sb("vw", [128, 32, 65], BF16)
                BVw = Buf("vw")
                kcr = cx.sb("kcr", [64, S], BF16)
                vcr = cx.sb("vcr", [64, S], BF16)
                Bkcr, Bvcr = Buf("kcr"), Buf("vcr")
                Kc = cx.sb("kc", [128, 256], BF16)
                BKc = Buf("kc")
                Vc = cx.sb("vc", [128, 2, 129], BF16)
                BVc = Buf("vc")
                cmask = cx.sb("cmask", [128, 2, S], BF16)
                G = cx.sb("gates", [128, 32, 24])
                BG = Buf("gates")
                BIAS = cx.sb("bias", [128, 32, 64])
                caus = cx.sb("caus", [128, 128], BF16)
                anti = cx.sb("anti", [128, 128], BF16)
                Bc2 = Buf("const2")
                acc = [cx.sb("acc%d" % i, [128, 4, 256]) for i in range(2)]
                Bacc = [[Buf("acc%d_%d" % (i, h)) for h in range(4)] for i in range(2)]
                accb = cx.sb("accb", [128, 4, 256], BF16)
                Baccb = Buf("accb")
                impacc = [cx.sb("imp%d" % i, [128, 4, 64]) for i in range(2)]
                Bimp = [Buf("imp%d" % i) for i in range(2)]
                PT = [cx.sb("pt%d" % i, [128, 512], BF16) for i in range(8)]
                BPT = [Buf("pt%d" % i) for i in range(8)]
                w1k = cx.sb("w1k", [64, 32, 128], BF16)
                w1v = cx.sb("w1v", [64, 32, 128], BF16)
                w2k = cx.sb("w2k", [128, 64], BF16)
                w2v = cx.sb("w2v", [128, 64], BF16)
                peTk = cx.sb("peTk", [64, 32], BF16)
                peTv = cx.sb("peTv", [64, 32], BF16)
                Bcw = Buf("cw")
                cbias = cx.sb("cbias", [128, 2])
                Bcb = Buf("cbias")
                hidc = cx.sb("hidc", [128, 2, 256], BF16)
                Bhidc = [Buf("hidck"), Buf("hidcv")]
                sm = cx.sb("sm", [128, 64])
                Bsm = Buf("sm")
                vt = cx.sb("vt", [128, 2, 64])
                Bvt = Buf("vt")
                nm = cx.sb("nm", [128, 64], BF16)
                Bnm = Buf("nm")
                nmT = cx.sb("nmT", [128, CT], BF16)
                BnmT = Buf("nmT")
                onst = cx.sb("onst", [128, 2, CT], BF16)
                Bonst = Buf("onst")

                P.add("dve", lambda e: e.memset(Ksl[64:128, :], 1.0), writes=[Bc2])
                P.add("pool", lambda e: e.affine_select(out=Ksl[64:128, :], in_=Ksl[64:128, :], pattern=[[1, S]], compare_op=ALU.is_ge, fill=0.0,
                                                        base=0, channel_multiplier=-64), reads=[Bc2], writes=[Bc2])
                P.add("pool", lambda e: e.affine_select(out=Ksl[64:128, :], in_=Ksl[64:128, :], pattern=[[-1, S]], compare_op=ALU.is_ge, fill=0.0,
                                                        base=63, channel_multiplier=64), reads=[Bc2], writes=[Bc2])
                P.add("dve", lambda e: e.memset(Kw[64:128, :], 0.0), writes=[Bc2])
                for hh in range(4):
                    P.add("dve", lambda e, hh=hh: e.memset(Qaug[hh][64:128, :], 0.0), writes=BQ[hh])
                P.add("pool", lambda e: e.memset(Kc[64:128, :], 0.0), writes=[Bc2])
                P.add("dve", lambda e: e.memset(cmask[:], 0.0), writes=[Bc2])
                for nt in range(2):
                    P.add("pool", lambda e, nt=nt: e.affine_select(out=cmask[:, nt, :], in_=cmask[:, nt, :], pattern=[[1, S]], compare_op=ALU.is_ge, fill=NEGM,
                                                                  base=-31 - 16 * 128 * nt, channel_multiplier=-16), reads=[Bc2], writes=[Bc2])
                P.add("pool", lambda e: e.memset(caus[:], 0.0), writes=[Bc2])
                P.add("pool", lambda e: e.affine_select(out=caus[:], in_=caus[:], pattern=[[1, 128]], compare_op=ALU.is_ge, fill=NEGM, base=0, channel_multiplier=-1),
                      reads=[Bc2], writes=[Bc2])
                P.add("pool", lambda e: e.memset(anti[:], 0.0), writes=[Bc2])
                P.add("pool", lambda e: e.affine_select(out=anti[:], in_=anti[:], pattern=[[-1, 128]], compare_op=ALU.is_gt, fill=NEGM, base=0, channel_multiplier=1),
                      reads=[Bc2], writes=[Bc2])
                P.add("dve", lambda e: e.memset(BIAS[:], 0.0), writes=[Bc2])
                for half in range(2):
                    rows = BIAS[half * 64:(half + 1) * 64, :, :]
                    P.add("pool", lambda e, rows=rows, half=half: e.affine_select(out=rows, in_=rows, pattern=[[2, 32], [-1, 64]], compare_op=ALU.is_ge, fill=-1e4,
                                                                                 base=half, channel_multiplier=0), reads=[Bc2], writes=[Bc2])
                    P.add("pool", lambda e, rows=rows, half=half: e.affine_select(out=rows, in_=rows, pattern=[[-2, 32], [1, 64]], compare_op=ALU.not_equal, fill=1e4,
                                                                                 base=-half, channel_multiplier=0), reads=[Bc2], writes=[Bc2])
                    P.add("pool", lambda e, rows=rows, half=half: e.affine_select(out=rows, in_=rows, pattern=[[-2, 32], [1, 64]], compare_op=ALU.not_equal, fill=1e4,
                                                                                 base=1 - half, channel_multiplier=0), reads=[Bc2], writes=[Bc2])
                    P.add("pool", lambda e, rows=rows: e.affine_select(out=rows, in_=rows, pattern=[[0, 32], [1, 64]], compare_op=ALU.not_equal, fill=1e4,
                                                                      base=0, channel_multiplier=0), reads=[Bc2], writes=[Bc2])
                P.add("pool", lambda e: e.memset(Vs[:, :, 64:65], 1.0), writes=[Bc2])
                P.add("pool", lambda e: e.memset(Vw[:, :, 64:65], 1.0), writes=[Bc2])
                P.add("pool", lambda e: e.memset(Vc[:, :, 64:129], 1.0), writes=[Bc2])
                for nt in range(2):
                    ov = Vc[:, nt, 65:129]
                    P.add("pool", lambda e, ov=ov, nt=nt: e.affine_select(out=ov, in_=ov, pattern=[[-4, 64]], compare_op=ALU.is_ge, fill=0.0,
                                                                         base=nt * 128 + 1, channel_multiplier=1), reads=[Bc2], writes=[Bc2])
                    P.add("pool", lambda e, ov=ov, nt=nt: e.affine_select(out=ov, in_=ov, pattern=[[4, 64]], compare_op=ALU.is_ge, fill=0.0,
                                                                         base=3 - nt * 128, channel_multiplier=-1), reads=[Bc2], writes=[Bc2])
                P.add("dve", lambda e: e.memset(hidc[:], 0.0), writes=Bhidc)
                P.add("pool", lambda e: e.dma_start(out=w1k[:], in_=cmp_k_w1.rearrange("(l d) h -> d l h", d=64)), writes=[Bcw], dma_key="cw0")
                P.add("pool", lambda e: e.dma_start(out=w1v[:], in_=cmp_v_w1.rearrange("(l d) h -> d l h", d=64)), writes=[Bcw], dma_key="cw1")
                P.add("pool", lambda e: e.dma_start(out=w2k[:], in_=cmp_k_w2), writes=[Bcw], dma_key="cw2")
                P.add("pool", lambda e: e.dma_start(out=w2v[:], in_=cmp_v_w2), writes=[Bcw], dma_key="cw3")
                P.add("pool", lambda e: e.dma_start(out=peTk[:], in_=cmp_k_pe.rearrange("l d -> d l"), allow_slow_non_contiguous=True), writes=[Bcw], dma_key="cw4")
                P.add("pool", lambda e: e.dma_start(out=peTv[:], in_=cmp_v_pe.rearrange("l d -> d l"), allow_slow_non_contiguous=True), writes=[Bcw], dma_key="cw5")
                P.add("sp", lambda e: e.dma_start(out=G[:], in_=TOKF[:, 1536:1560].rearrange("(k p) n -> p k n", p=128)), writes=[BG], dma_key="gates")
                for wi_, (w1sb, peT) in enumerate(((w1k, peTk), (w1v, peTv))):
                    pbk, Bpbk = Cbanks[wi_]
                    for l in range(32):
                        P.add("pe", lambda e, pbk=pbk, w1sb=w1sb, peT=peT, l=l: e.matmul(pbk[:, 0:1], lhsT=w1sb[:, l, :], rhs=peT[:, l:l + 1], start=(l == 0), stop=(l == 31)),
                              reads=[Bcw], writes=[Bpbk])
                    P.add("dve", lambda e, pbk=pbk, wi_=wi_: e.tensor_copy(out=cbias[:, wi_:wi_ + 1], in_=pbk[:, 0:1]), reads=[Bpbk], writes=[Bcb])

                def sbank(pool=None):
                    pool = pool or Sbanks
                    b = pool[rr["s"] % len(pool)]
                    rr["s"] += 1
                    return b

                def ptbuf():
                    i = rr["pt"] % 8
                    rr["pt"] += 1
                    return PT[i], BPT[i]

                def run_tiles(tiles, depth=2, pool=None):
                    pend = []

                    def pv(t):
                        for j in range(t["col0"] // 128, (t["col0"] + t["ncols"]) // 128):
                            o_ap, Bo = t["O"][j]
                            P.add("pe", lambda e, t=t, j=j, o_ap=o_ap: e.matmul(o_ap, lhsT=t["pt"][:, j * 128:(j + 1) * 128], rhs=t["V"], start=False, stop=True,
                                                                               skip_group_check=True),
                                  reads=[t["Bpt"], t["BV"]], writes=[Bo])
                        if t.get("after") is not None:
                            t["after"]()

                    for t in tiles:
                        while len(pend) >= depth:
                            pv(pend.pop(0))
                        if t.get("before") is not None:
                            t["before"]()
                        ps, Bps = sbank(pool)
                        c0, ncl = t["col0"], t["ncols"]
                        mk = t.get("mask")
                        P.add("pe", lambda e, t=t, ps=ps, c0=c0, ncl=ncl, mk=mk: e.matmul(ps[:, c0:c0 + ncl], lhsT=t["K"], rhs=t["Q"][:, t["q0"] + c0:t["q0"] + c0 + ncl],
                                                                                        start=True, stop=(mk is None)),
                              reads=[t["BK"], t["BQ"]], writes=[Bps])
                        if mk is not None:
                            m_ap, mcol, mn = mk
                            P.add("pe", lambda e, ps=ps, m_ap=m_ap, mcol=mcol, mn=mn: e.matmul(ps[:, mcol:mcol + mn], lhsT=ident_b[:], rhs=m_ap, start=False, stop=True),
                                  reads=[Bc2, B_const], writes=[Bps])
                        pt, Bpt = ptbuf()
                        t["pt"], t["Bpt"] = pt, Bpt
                        P.add("act", lambda e, pt=pt, ps=ps, c0=c0, ncl=ncl: e.activation(out=pt[:, c0:c0 + ncl], in_=ps[:, c0:c0 + ncl], func=AF.Exp),
                              reads=[Bps], writes=[Bpt])
                        pend.append(t)
                    while pend:
                        pv(pend.pop(0))

                QTv2 = QT
                for g in range(2):
                    for hh in range(4):
                        P.add("sp", lambda e, hh=hh, g=g: e.dma_start(out=Qaug[hh][0:64, :], in_=QTv2[4 * g + hh]), writes=BQ[hh], dma_key=("q", hh))
                    P.add("sp", lambda e, g=g: e.dma_start(out=Ksl[0:64, :], in_=KVT[2, g * 64:(g + 1) * 64, :]), writes=[BKsl], dma_key="ksl")
                    P.add("sp", lambda e, g=g: e.dma_start(out=Kw[0:64, :], in_=KVT[3, g * 64:(g + 1) * 64, :]), writes=[BKw], dma_key="kw")
                    P.add("sp", lambda e, g=g: e.dma_start(out=kcr[:], in_=KVT[0, g * 64:(g + 1) * 64, :]), writes=[Bkcr], dma_key="kcr")
                    P.add("sp", lambda e, g=g: e.dma_start(out=vcr[:], in_=KVT[1, g * 64:(g + 1) * 64, :]), writes=[Bvcr], dma_key="vcr")
                    P.add("sp", lambda e, g=g: e.dma_start(out=Vs[:, :, 0:64], in_=TOKB[:, g * 64:(g + 1) * 64].rearrange("(k p) d -> p k d", p=128)),
                          writes=[BVs], dma_key="vs")
                    P.add("sp", lambda e, g=g: e.dma_start(out=Vw[:, :, 0:64], in_=TOKB[:, 128 + g * 64:128 + (g + 1) * 64].rearrange("(k p) d -> p k d", p=128)),
                          writes=[BVw], dma_key="vw")
                    for wi_, (raw, Braw, w1sb, w2sb) in enumerate(((kcr, Bkcr, w1k, w2k), (vcr, Bvcr, w1v, w2v))):
                        ph, Bph = Cbanks[wi_]
                        for l in range(32):
                            P.add("pe", lambda e, ph=ph, w1sb=w1sb, raw=raw, l=l: e.matmul(ph[:, 0:255], lhsT=w1sb[:, l, :], rhs=raw[:, l:l + 16 * 254 + 1:16],
                                                                                          start=(l == 0), stop=(l == 31)),
                                  reads=[Bcw, Braw], writes=[Bph])
                        P.add("act", lambda e, ph=ph, wi_=wi_: e.activation(out=hidc[:, wi_, 0:255], in_=ph[:, 0:255], func=AF.Silu, bias=cbias[:, wi_:wi_ + 1]),
                              reads=[Bph, Bcb], writes=[Bhidc[wi_]])
                    pk, Bpk = Cbanks[0]
                    P.add("pe", lambda e, pk=pk: e.matmul(pk[0:64, 0:256], lhsT=w2k[:], rhs=hidc[:, 0, :], start=True, stop=True), reads=[Bcw, Bhidc[0]], writes=[Bpk])
                    P.add("dve", lambda e, pk=pk: e.tensor_copy(out=Kc[0:64, :], in_=pk[0:64, 0:256]), reads=[Bpk], writes=[BKc])
                    pv_, Bpv_ = Cbanks[1]
                    for nt in range(2):
                        P.add("pe", lambda e, pv_=pv_, nt=nt: e.matmul(pv_[:, nt * 64:(nt + 1) * 64], lhsT=hidc[:, 1, nt * 128:(nt + 1) * 128], rhs=w2v[:], start=True, stop=True),
                              reads=[Bcw, Bhidc[1]], writes=[Bpv_])
                    P.add("dve", lambda e, pv_=pv_: e.tensor_copy(out=Vc[:, :, 0:64], in_=pv_[:, 0:128].rearrange("p (n d) -> p n d", n=2)), reads=[Bpv_], writes=[BVc])

                    for qc in range(NCH):
                        q0 = qc * CT
                        ab = qc % 2
                        tiles = []
                        nts = [0] if qc <= 3 else [0, 1]
                        for hh in range(4):
                            h = 4 * g + hh
                            OA, BOA = (Cbanks if hh % 2 == 0 else Obanks)[0]
                            OB, BOB = (Cbanks if hh % 2 == 0 else Obanks)[1]
                            Oj = [(OA[:, 0:129], BOA), (OA[:, 129:258], BOA), (OB[:, 0:129], BOB), (OB[:, 129:258], BOB)]

                            def before(OA=OA, OB=OB, BOA=BOA, BOB=BOB):
                                P.add("dve", lambda e: e.memset(OA[:, 0:258], 0.0), writes=[BOA])
                                P.add("dve", lambda e: e.memset(OB[:, 0:258], 0.0), writes=[BOB])

                            def after(OA=OA, OB=OB, BOA=BOA, BOB=BOB, hh=hh, h=h, qc=qc, ab=ab):
                                for half, (Ob, BOb) in enumerate(((OA, BOA), (OB, BOB))):
                                    den = Ob[:, 0:258].rearrange("p (j c) -> p j c", j=2)[:, :, 64]
                                    rd = sm[:, half * 2:half * 2 + 2]
                                    P.add("dve", lambda e, den=den, rd=rd: e.tensor_scalar(out=rd, in0=den, scalar1=1e-30, scalar2=None, op0=ALU.max), reads=[BOb], writes=[Bsm])
                                    P.add("dve", lambda e, rd=rd: e.reciprocal(out=rd, in_=rd), reads=[Bsm], writes=[Bsm])
                                    fc = sm[:, 4 + half * 2:4 + half * 2 + 2]
                                    gsl = G[:, 4 * qc + half * 2:4 * qc + half * 2 + 2, 3 * h + 0]
                                    P.add("dve", lambda e, fc=fc, rd=rd, gsl=gsl: e.tensor_tensor(out=fc, in0=rd, in1=gsl, op=ALU.mult), reads=[Bsm, BG], writes=[Bsm])
                                    for jj in range(2):
                                        j = half * 2 + jj
                                        P.add("dve", lambda e, Ob=Ob, jj=jj, j=j: e.tensor_scalar(out=acc[ab][:, j, hh * 64:(hh + 1) * 64], in0=Ob[:, jj * 129:jj * 129 + 64],
                                                                                              scalar1=sm[:, 4 + j:5 + j], scalar2=None, op0=ALU.mult),
                                              reads=[BOb, Bsm], writes=[Bacc[ab][hh]])
                                        if hh == 0:
                                            P.add("dve", lambda e, Ob=Ob, jj=jj, j=j: e.tensor_scalar(out=impacc[ab][:, j, :], in0=Ob[:, jj * 129 + 65:jj * 129 + 129],
                                                                                                  scalar1=sm[:, j:j + 1], scalar2=None, op0=ALU.mult),
                                                  reads=[BOb, Bsm], writes=[Bimp[ab]])
                                        else:
                                            P.add("dve", lambda e, Ob=Ob, jj=jj, j=j: e.scalar_tensor_tensor(out=impacc[ab][:, j, :], in0=Ob[:, jj * 129 + 65:jj * 129 + 129],
                                                                                                         scalar=sm[:, j:j + 1], in1=impacc[ab][:, j, :], op0=ALU.mult, op1=ALU.add),
                                                  reads=[BOb, Bsm, Bimp[ab]], writes=[Bimp[ab]])

                            for ti, nt in enumerate(nts):
                                need_mask = (nt == 1) or (qc <= 4)
                                tiles.append(dict(K=Kc[:, nt * 128:(nt + 1) * 128], BK=BKc, Q=Qaug[hh], BQ=BQ[hh][qc], q0=q0, col0=0, ncols=512,
                                                  mask=(cmask[:, nt, q0:q0 + CT], 0, 512) if need_mask else None,
                                                  V=Vc[:, nt, :], BV=BVc, O=Oj,
                                                  before=before if ti == 0 else None, after=after if ti == len(nts) - 1 else None))
                        run_tiles(tiles)
                        pm_, Bpm_ = Mbank
                        for j in range(4):
                            qt = 4 * qc + j
                            vv = vt[:, j % 2, :]
                            P.add("dve", lambda e, vv=vv, j=j, qt=qt, ab=ab: e.tensor_tensor(out=vv, in0=impacc[ab][:, j, :], in1=BIAS[:, qt, :], op=ALU.add),
                                  reads=[Bimp[ab], Bc2], writes=[Bvt])
                            P.add("dve", lambda e, vv=vv: e.max(out=sm[:, 8:16], in_=vv), reads=[Bvt], writes=[Bsm])
                            P.add("dve", lambda e, vv=vv, j=j: e.match_replace(out=vt[:, (j + 1) % 2, :], in_to_replace=sm[:, 8:16], in_values=vv, imm_value=-1e9),
                                  reads=[Bvt, Bsm], writes=[Bvt])
                            P.add("dve", lambda e, j=j: e.max(out=sm[:, 16:24], in_=vt[:, (j + 1) % 2, :]), reads=[Bvt], writes=[Bsm])
                            P.add("dve", lambda e, vv=vv: e.tensor_scalar(out=nm[:], in0=vv, scalar1=sm[:, 23:24], scalar2=NEGM, op0=ALU.is_lt, op1=ALU.mult),
                                  reads=[Bvt, Bsm], writes=[Bnm])
                            P.add("pe", lambda e, pm_=pm_, j=j: e.matmul(pm_[64:128, j * 128:(j + 1) * 128], lhsT=nm[:], rhs=ident_b[:], start=True, stop=True),
                                  reads=[Bnm, B_const], writes=[Bpm_])
                        P.add("act", lambda e, pm_=pm_: e.copy(out=nmT[64:128, :], in_=pm_[64:128, :]), reads=[Bpm_], writes=[BnmT])
                        for hh in range(4):
                            eng = "pool" if hh % 2 == 0 else "dve"
                            P.add(eng, lambda e, hh=hh, q0=q0: e.tensor_copy(out=Qaug[hh][64:128, q0:q0 + CT], in_=nmT[64:128, :]), reads=[BnmT], writes=[BQ[hh][qc]])
                        tiles = []
                        for br, (Ka, BKa, Va, BVa) in ((1, (Ksl, BKsl, Vs, BVs)), (2, (Kw, BKw, Vw, BVw))):
                            for hh in range(4):
                                h = 4 * g + hh
                                Ob, BOb = Obanks[rr["o"] % 2]
                                rr["o"] += 1
                                Oj = [(Ob[:, j * 65:(j + 1) * 65], BOb) for j in range(4)]
                                tl = []
                                if br == 1:
                                    for kt in range(4 * qc):
                                        tl.append((kt, 0, 512, None))
                                else:
                                    for i in range(4):
                                        kt = 4 * qc - 4 + i
                                        if kt >= 0:
                                            tl.append((kt, 0, 128 * (i + 1), (anti[:], 128 * i, 128)))
                                for i in range(4):
                                    tl.append((4 * qc + i, 128 * i, 512 - 128 * i, (caus[:], 128 * i, 128)))

                                def before(Ob=Ob, BOb=BOb):
                                    P.add("dve", lambda e: e.memset(Ob[:, 0:260], 0.0), writes=[BOb])

                                def after(Ob=Ob, BOb=BOb, hh=hh, h=h, br=br, qc=qc, ab=ab):
                                    den = Ob[:, 0:260].rearrange("p (j c) -> p j c", j=4)[:, :, 64]
                                    P.add("dve", lambda e: e.tensor_scalar(out=sm[:, 0:4], in0=den, scalar1=1e-30, scalar2=None, op0=ALU.max), reads=[BOb], writes=[Bsm])
                                    P.add("dve", lambda e: e.reciprocal(out=sm[:, 0:4], in_=sm[:, 0:4]), reads=[Bsm], writes=[Bsm])
                                    gsl = G[:, 4 * qc:4 * qc + 4, 3 * h + br]
                                    P.add("dve", lambda e: e.tensor_tensor(out=sm[:, 4:8], in0=sm[:, 0:4], in1=gsl, op=ALU.mult), reads=[Bsm, BG], writes=[Bsm])
                                    for j in range(4):
                                        P.add("dve", lambda e, j=j: e.scalar_tensor_tensor(out=acc[ab][:, j, hh * 64:(hh + 1) * 64], in0=Ob[:, j * 65:j * 65 + 64],
                                                                                        scalar=sm[:, 4 + j:5 + j], in1=acc[ab][:, j, hh * 64:(hh + 1) * 64],
                                                                                        op0=ALU.mult, op1=ALU.add),
                                              reads=[BOb, Bsm, Bacc[ab][hh]], writes=[Bacc[ab][hh]])

                                for ti, (kt, c0, ncl, mk) in enumerate(tl):
                                    tiles.append(dict(K=Ka[:, kt * 128:(kt + 1) * 128], BK=BKa, Q=Qaug[hh], BQ=BQ[hh][qc], q0=q0, col0=c0, ncols=ncl, mask=mk,
                                                      V=Va[:, kt, :], BV=BVa, O=Oj,
                                                      before=before if ti == 0 else None, after=after if ti == len(tl) - 1 else None))
                        run_tiles(tiles, depth=4, pool=Sbanks + Cbanks)
                        P.add("act", lambda e, ab=ab: e.copy(out=accb[:], in_=acc[ab][:]), reads=Bacc[ab], writes=[Baccb])
                        ptb, Bptb = Mbank
                        ptv = ptb[:].bitcast(BF16)
                        for f in range(2):
                            for j in range(4):
                                P.add("pe", lambda e, f=f, j=j, ptv=ptv: e.transpose(ptv[:, f * 512 + j * 128:f * 512 + (j + 1) * 128], accb[:, j, f * 128:(f + 1) * 128], ident_b[:]),
                                      reads=[Baccb, B_const], writes=[Bptb])
                        P.add("dve", lambda e, ptv=ptv: e.tensor_copy(out=onst[:], in_=ptv.rearrange("p (f t) -> p f t", f=2)), reads=[Bptb], writes=[Bonst])
                        P.add("sp", lambda e, g=g, q0=q0: e.dma_start(out=ONT[g * 256:(g + 1) * 256, q0:q0 + CT].rearrange("(f p) t -> p f t", p=128), in_=onst[:]),
                              reads=[Bonst], dma_key="onst")
                P.emit(st)

        if 2 in phases:
            with ExitStack() as st:
                P = Prog(nc, "p2b")
                cx = Ctx(nc, P, st)
                cx.psum_banks(8)
                bE, bO, bOo, bS, bTq, bTk, bTy = cx.banks[0:7]
                rope = cx.sb("rope", [128, 32, 64])
                rt = cx.sb("rt", [128, 24])
                rnw = cx.sb("rnw", [128, 512])
                mask01 = cx.sb("mask01", [128, 128])
                Bk = Buf("consts")
                qk = [cx.sb("qk%d" % i, [128, 1024]) for i in range(2)]
                rgs = [cx.sb("rg%d" % i, [128, 512]) for i in range(2)]
                rv = [cx.sb("rv%d" % i, [128, 512], BF16) for i in range(2)]
                Bqk = [Buf("qk%d" % i) for i in range(2)]
                Brg = [Buf("rg%d" % i) for i in range(2)]
                Brv = [Buf("rv%d" % i) for i in range(2)]
                tq = cx.sb("tq", [128, 4, 256])
                tk = cx.sb("tk", [128, 4, 256])
                Btq, Btk = Buf("tq"), Buf("tk")
                qr = cx.sb("qr", [128, 512])
                kr = cx.sb("kr", [128, 512])
                Bqr, Bkr = Buf("qr"), Buf("kr")
                qd = cx.sb("qd", [128, 512], BF16)
                kinv = cx.sb("kinv", [128, 512], BF16)
                Bqd, Bkinv = Buf("qd"), Buf("kinv")
                QdT = cx.sb("QdT", [128, 4, 128], BF16)
                KinvT = cx.sb("KinvT", [128, 4, 128], BF16)
                BQdT, BKinvT = Buf("QdT"), Buf("KinvT")
                innerT = cx.sb("innerT", [128, 8, 128], BF16)
                Binn = [Buf("innE"), Buf("innO")]
                St = cx.sb("St", [128, 4, 64])
                Sbf = cx.sb("Sbf", [128, 8, 64], BF16)
                BSt, BSbf = Buf("St"), Buf("Sbf")
                osb = cx.sb("osb", [128, 512])
                osq = cx.sb("osq", [128, 512])
                Bosb, Bosq = Buf("osb"), Buf("osq")
                stt = cx.sb("stt", [128, 40])
                Bstt = Buf("stt")
                ybf = cx.sb("ybf", [128, 512], BF16)
                Bybf = Buf("ybf")
                orst = cx.sb("orst", [128, 4, 128], BF16)
                Borst = Buf("orst")
                P.add("sp", lambda e: e.dma_start(out=rope[:], in_=rope_tab), writes=[Bk], dma_key="c0")
                P.add("sp", lambda e: e.dma_start(out=rt[:], in_=ret_tab), writes=[Bk], dma_key="c1")
                P.add("sp", lambda e: e.dma_start(out=rnw[:], in_=ret_norm_w.to_broadcast([128, 512])), writes=[Bk], dma_key="c2")
                P.add("pool", lambda e: e.memset(mask01[:], 1.0), writes=[Bk])
                P.add("pool", lambda e: e.affine_select(out=mask01[:], in_=mask01[:], pattern=[[1, 128]], compare_op=ALU.is_ge, fill=0.0, base=0, channel_multiplier=-1),
                      reads=[Bk], writes=[Bk])
                P.add("pool", lambda e: e.memset(St[:], 0.0), writes=[BSt])
                P.add("pool", lambda e: e.memset(Sbf[:], 0.0), writes=[BSbf])

                def load(i):
                    b = i % 2
                    r0 = i * 128
                    P.add("sp", lambda e: e.dma_start(out=qk[b][:], in_=TOKF[r0:r0 + 128, 0:1024]), writes=[Bqk[b]], dma_key=("qk", b))
                    P.add("sp", lambda e: e.dma_start(out=rgs[b][:], in_=TOKF[r0:r0 + 128, 1024:1536]), writes=[Brg[b]], dma_key=("rg", b))
                    P.add("sp", lambda e: e.dma_start(out=rv[b][:], in_=TOKB[r0:r0 + 128, 256:768]), writes=[Brv[b]], dma_key=("rv", b))

                def rope_apply(eng, src, Bsrc, tt, Btt, dst, Bdst, i):
                    x = src.rearrange("p (h d) -> p h d", h=8)
                    x1, x2 = x[:, :, 0:32], x[:, :, 32:64]
                    cos = rope[:, i:i + 1, 0:32].to_broadcast([128, 8, 32])
                    sin = rope[:, i:i + 1, 32:64].to_broadcast([128, 8, 32])
                    tv = [tt[:, k, :].rearrange("p (h d) -> p h d", h=8) for k in range(4)]
                    d = dst.rearrange("p (h d) -> p h d", h=8)
                    P.add(eng, lambda e: e.tensor_tensor(out=tv[0], in0=x1, in1=cos, op=ALU.mult), reads=[Bsrc, Bk], writes=[Btt])
                    P.add(eng, lambda e: e.tensor_tensor(out=tv[1], in0=x2, in1=sin, op=ALU.mult), reads=[Bsrc, Bk], writes=[Btt])
                    P.add(eng, lambda e: e.tensor_tensor(out=tv[2], in0=x1, in1=sin, op=ALU.mult), reads=[Bsrc, Bk], writes=[Btt])
                    P.add(eng, lambda e: e.tensor_tensor(out=tv[3], in0=x2, in1=cos, op=ALU.mult), reads=[Bsrc, Bk], writes=[Btt])
                    P.add(eng, lambda e: e.tensor_tensor(out=d[:, :, 0:32], in0=tv[0], in1=tv[1], op=ALU.subtract), reads=[Btt], writes=[Bdst])
                    P.add(eng, lambda e: e.tensor_tensor(out=d[:, :, 32:64], in0=tv[2], in1=tv[3], op=ALU.add), reads=[Btt], writes=[Bdst])

                ORv = ORT.rearrange("(f p) t -> p f t", p=128)
                tqv = bTq[0][:].bitcast(BF16)
                tkv = bTk[0][:].bitcast(BF16)

                def front(i):
                    b = i % 2
                    rope_apply("dve", qk[b][:, 0:512], Bqk[b], tq, Btq, qr[:], Bqr, i)
                    rope_apply("pool", qk[b][:, 512:1024], Bqk[b], tk, Btk, kr[:], Bkr, i)
                    P.add("dve", lambda e: e.tensor_tensor(out=qd[:].rearrange("p (h d) -> p h d", h=8), in0=qr[:].rearrange("p (h d) -> p h d", h=8),
                                                           in1=rt[:, 0:8].unsqueeze(2).to_broadcast([128, 8, 64]), op=ALU.mult), reads=[Bqr, Bk], writes=[Bqd])
                    P.add("pool", lambda e: e.tensor_tensor(out=kinv2[b][:].rearrange("p (h d) -> p h d", h=8), in0=kr[:].rearrange("p (h d) -> p h d", h=8),
                                                            in1=rt[:, 8:16].unsqueeze(2).to_broadcast([128, 8, 64]), op=ALU.mult), reads=[Bkr, Bk], writes=[Bkinv2[b]])
                    for pr in range(4):
                        P.add("pe", lambda e, pr=pr: e.transpose(tqv[:, pr * 128:(pr + 1) * 128], qd[:, pr * 128:(pr + 1) * 128], ident_b[:]),
                              reads=[Bqd, B_const], writes=[bTq[1]])
                    for pr in range(4):
                        P.add("pe", lambda e, pr=pr: e.transpose(tkv[:, pr * 128:(pr + 1) * 128], kinv2[b][:, pr * 128:(pr + 1) * 128], ident_b[:]),
                              reads=[Bkinv2[b], B_const], writes=[bTk[1]])
                    P.add("act", lambda e: e.copy(out=QdT2[b][:], in_=tqv[:, 0:512].rearrange("p (a t) -> p a t", a=4)), reads=[bTq[1]], writes=[BQdT2[b]])
                    P.add("act", lambda e: e.copy(out=KinvT2[b][:], in_=tkv[:, 0:512].rearrange("p (a t) -> p a t", a=4)), reads=[bTk[1]], writes=[BKinvT2[b]])

                kinv2 = [kinv, cx.sb("kinvB", [128, 512], BF16)]
                Bkinv2 = [Bkinv, Buf("kinvB")]
                QdT2 = [QdT, cx.sb("QdTB", [128, 4, 128], BF16)]
                BQdT2 = [BQdT, Buf("QdTB")]
                KinvT2 = [KinvT, cx.sb("KinvTB", [128, 4, 128], BF16)]
                BKinvT2 = [BKinvT, Buf("KinvTB")]
                def mid(i):
                    b = i % 2
                    QdT, BQdT, KinvT, BKinvT, kinv, Bkinv = QdT2[b], BQdT2[b], KinvT2[b], BKinvT2[b], kinv2[b], Bkinv2[b]
                    for par, (bk_, ) in enumerate(((bE,), (bO,))):
                        pb_, Bpb_ = bk_
                        lo = par * 64
                        for pr in range(4):
                            P.add("pe", lambda e, pb_=pb_, pr=pr, lo=lo, KinvT=KinvT, QdT=QdT: e.matmul(pb_[:, pr * 128:(pr + 1) * 128], lhsT=KinvT[lo:lo + 64, pr, :], rhs=QdT[lo:lo + 64, pr, :],
                                                                               start=True, stop=True), reads=[BKinvT, BQdT], writes=[Bpb_])
                        P.add("dve", lambda e, pb_=pb_, par=par: e.tensor_tensor(out=innerT[:, par * 4:(par + 1) * 4, :], in0=pb_[:].rearrange("p (a t) -> p a t", a=4),
                                                                                in1=mask01[:].unsqueeze(1).to_broadcast([128, 4, 128]), op=ALU.mult),
                              reads=[Bpb_, Bk], writes=[Binn[par]])
                    po, Bpo = bOo
                    for h in range(8):
                        pr, par = h // 2, h % 2
                        P.add("pe", lambda e, h=h, pr=pr, par=par, b=b: e.matmul(po[:, h * 64:(h + 1) * 64], lhsT=innerT[:, par * 4 + pr, :], rhs=rv[b][:, h * 64:(h + 1) * 64],
                                                                              start=True, stop=False, skip_group_check=True), reads=[Binn[par], Brv[b]], writes=[Bpo])
                        P.add("pe", lambda e, h=h, pr=pr, QdT=QdT: e.matmul(po[:, h * 64:(h + 1) * 64], lhsT=QdT[:, pr, :], rhs=Sbf[:, h, :], start=False, stop=True, skip_group_check=True),
                              reads=[BQdT, BSbf], writes=[Bpo])
                    pS, BpS = bS
                    for h in range(8):
                        pr, par = h // 2, h % 2
                        P.add("pe", lambda e, h=h, pr=pr, par=par, b=b, kinv=kinv: e.matmul(pS[par * 64:(par + 1) * 64, pr * 64:(pr + 1) * 64], lhsT=kinv[:, h * 64:(h + 1) * 64],
                                                                              rhs=rv[b][:, h * 64:(h + 1) * 64], start=True, stop=True, skip_group_check=True),
                              reads=[Bkinv, Brv[b]], writes=[BpS])
                    P.add("dve", lambda e: e.tensor_tensor(out=St[:], in0=pS[:, 0:256].rearrange("p (a d) -> p a d", a=4), in1=St[:], op=ALU.add), reads=[BpS, BSt], writes=[BSt])
                    P.add("dve", lambda e: e.tensor_tensor(out=St[:], in0=St[:], in1=rt[:, 16:20].unsqueeze(2).to_broadcast([128, 4, 64]), op=ALU.mult), reads=[BSt, Bk], writes=[BSt])
                    sbv = Sbf[:].rearrange("p (a two) d -> p a two d", two=2)
                    P.add("pool", lambda e: e.tensor_copy(out=sbv[0:64, :, 0, :], in_=St[0:64, :, :]), reads=[BSt], writes=[BSbf])
                    P.add("pool", lambda e: e.tensor_copy(out=sbv[64:128, :, 1, :], in_=St[64:128, :, :]), reads=[BSt], writes=[BSbf])
                    P.add("act", lambda e: e.copy(out=osb[:], in_=po[:]), reads=[Bpo], writes=[Bosb])
                    P.add("act", lambda e: e.activation(out=osq[:], in_=po[:], func=AF.Square), reads=[Bpo], writes=[Bosq])

                def tail(i):
                    b = i % 2
                    o3 = osb[:].rearrange("p (h d) -> p h d", h=8)
                    P.add("dve", lambda e: e.tensor_reduce(out=stt[:, 0:8], in_=o3, axis=AX.X, op=ALU.add), reads=[Bosb], writes=[Bstt])
                    P.add("dve", lambda e: e.tensor_reduce(out=stt[:, 8:16], in_=osq[:].rearrange("p (h d) -> p h d", h=8), axis=AX.X, op=ALU.add), reads=[Bosq], writes=[Bstt])
                    P.add("dve", lambda e: e.tensor_scalar(out=stt[:, 16:24], in0=stt[:, 0:8], scalar1=1.0 / 64, scalar2=None, op0=ALU.mult), reads=[Bstt], writes=[Bstt])
                    P.add("dve", lambda e: e.tensor_tensor(out=stt[:, 32:40], in0=stt[:, 16:24], in1=stt[:, 16:24], op=ALU.mult), reads=[Bstt], writes=[Bstt])
                    P.add("dve", lambda e: e.scalar_tensor_tensor(out=stt[:, 24:32], in0=stt[:, 8:16], scalar=1.0 / 64, in1=stt[:, 32:40], op0=ALU.mult, op1=ALU.subtract),
                          reads=[Bstt], writes=[Bstt])
                    P.add("act", lambda e: e.activation(out=stt[:, 24:32], in_=stt[:, 24:32], func=AF.Sqrt, bias=eps_t[:, 0:1]), reads=[Bstt, B_const], writes=[Bstt])
                    P.add("dve", lambda e: e.reciprocal(out=stt[:, 24:32], in_=stt[:, 24:32]), reads=[Bstt], writes=[Bstt])
                    P.add("dve", lambda e: e.tensor_tensor(out=o3, in0=o3, in1=stt[:, 16:24].unsqueeze(2).to_broadcast([128, 8, 64]), op=ALU.subtract), reads=[Bosb, Bstt], writes=[Bosb])
                    P.add("dve", lambda e: e.tensor_tensor(out=o3, in0=o3, in1=stt[:, 24:32].unsqueeze(2).to_broadcast([128, 8, 64]), op=ALU.mult), reads=[Bosb, Bstt], writes=[Bosb])
                    P.add("pool", lambda e: e.tensor_tensor(out=osb[:], in0=osb[:], in1=rnw[:], op=ALU.mult), reads=[Bosb, Bk], writes=[Bosb])
                    P.add("pool", lambda e, b=b: e.tensor_tensor(out=ybf[:], in0=osb[:], in1=rgs[b][:], op=ALU.mult), reads=[Bosb, Brg[b]], writes=[Bybf])
                    tyv = bTy[0][:].bitcast(BF16)
                    for f in range(4):
                        P.add("pe", lambda e, f=f, tyv=tyv: e.transpose(tyv[:, f * 128:(f + 1) * 128], ybf[:, f * 128:(f + 1) * 128], ident_b[:]), reads=[Bybf, B_const], writes=[bTy[1]])
                    P.add("act", lambda e, tyv=tyv: e.copy(out=orst[:], in_=tyv[:, 0:512].rearrange("p (a t) -> p a t", a=4)), reads=[bTy[1]], writes=[Borst])
                    P.add("sp", lambda e, i=i: e.dma_start(out=ORv[:, :, i * 128:(i + 1) * 128], in_=orst[:]), reads=[Borst], dma_key="orst")

                load(0)
                load(1)
                front(0)
                for i in range(32):
                    mid(i)
                    if i + 1 < 32:
                        front(i + 1)
                    tail(i)
                    if i + 2 < 32:
                        load(i + 2)
                P.emit(st)

        if 3 in phases:
            with ExitStack() as st:
                P = Prog(nc, "p3")
                cx = Ctx(nc, P, st)
                cx.psum_banks(7)
                hT = [cx.sb("hT%d" % i, [128, 8, CT]) for i in range(2)]
                Bh = [[Buf("hT%d_%d" % (i, k)) for k in range(8)] for i in range(2)]
                ont = [cx.sb("ont%d" % i, [128, 4, CT], BF16) for i in range(2)]
                Bont = [Buf("ont%d" % i) for i in range(2)]
                ort = [cx.sb("ort%d" % i, [128, 4, CT], BF16) for i in range(2)]
                Bort = [Buf("ort%d" % i) for i in range(2)]
                u2c = [cx.sb("u2c%d" % i, [128, 8, CT], BF16) for i in range(2)]
                Bu2c = [Buf("u2c%d" % i) for i in range(2)]
                mixed = cx.sb("mixed", [128, 8, CT], BF16)
                Bmixed = [Buf("mixed%d" % k) for k in range(8)]
                u3 = cx.sb("u3", [128, 8, CT], BF16)
                Bu3 = [Buf("u3_%d" % k) for k in range(8)]
                hid = cx.sb("hid", [128, 22, CT], BF16)
                Bhid = [Buf("hid%d" % k) for k in range(22)]
                sa = [cx.sb("sa%d" % i, [128, CT], BF16) for i in range(2)]
                Bsa = [Buf("sa%d" % i) for i in range(2)]
                sq = [cx.sb("sq%d" % i, [128, CT], BF16) for i in range(2)]
                Bsq = [Buf("sq%d" % i) for i in range(2)]
                tmpf = [cx.sb("tmpf%d" % i, [128, CT]) for i in range(2)]
                Btmpf = [Buf("tmpf%d" % i) for i in range(2)]
                rstd = cx.sb("rstd", [128, CT])
                Brstd = Buf("rstd")
                sga = cx.sb("sga", [128, 4, CT])
                Bsga = [Buf("sga%d" % k) for k in range(4)]
                sgb = cx.sb("sgb", [128, 4, CT])
                Bsgb = [Buf("sgb%d" % k) for k in range(4)]
                ostage = cx.sb("ostage", [128, 4, D])
                Bost = Buf("ostage")
                ws = WStream(cx, "w3", 4, 5632)
                loads = []
                for c in range(NCH):
                    for mh in range(2):
                        loads.append((8, 512, wslab(w_in, 8, C_GA + mh * 512, 512)))
                        loads.append((8, 512, wslab(w_in, 8, C_GB + mh * 512, 512)))
                        loads.append((4, 512, wslab(w_nsa_up, 4, mh * 512, 512)))
                        loads.append((4, 512, wslab(w_ret_up, 4, mh * 512, 512)))
                    for mh in range(2):
                        loads.append((8, 512, wslab(w_out, 8, mh * 512, 512)))
                    loads += ffn_loads(ffn2_w_in, ffn2_w_out)
                ws.plan(loads)
                LPC = len(loads) // NCH
                h1v = h1T.rearrange("(k p) t -> p k t", p=128)
                u2v = u2T.rearrange("(k p) t -> p k t", p=128)
                ONv = ONT.rearrange("(k p) t -> p k t", p=128)
                ORv = ORT.rearrange("(k p) t -> p k t", p=128)
                outv = out.rearrange("(c j p) d -> c p j d", j=4, p=128)

                def load_chunk(c):
                    b = c % 2
                    t0 = c * CT
                    P.add("sp", lambda e: e.dma_start(out=u2c[b][:], in_=u2v[:, :, t0:t0 + CT]), writes=[Bu2c[b]], dma_key=("u2c", b))
                    P.add("sp", lambda e: e.dma_start(out=ont[b][:], in_=ONv[:, :, t0:t0 + CT]), writes=[Bont[b]], dma_key=("ont", b))
                    P.add("sp", lambda e: e.dma_start(out=ort[b][:], in_=ORv[:, :, t0:t0 + CT]), writes=[Bort[b]], dma_key=("ort", b))
                    P.add("sp", lambda e: e.dma_start(out=hT[b][:], in_=h1v[:, :, t0:t0 + CT]), writes=Bh[b], dma_key=("h1l", b))

                def mm_group(wsl, Bwsl, nk, mm, rhs_t, Brhs):
                    pg, Bpg = cx.bank()
                    for kt in range(nk):
                        P.add("pe", lambda e, kt=kt: e.matmul(pg[:], lhsT=wsl[:, kt, mm * 128:(mm + 1) * 128], rhs=rhs_t[:, kt, :],
                                                             start=(kt == 0), stop=(kt == nk - 1)),
                              reads=[Bwsl] + Brhs, writes=[Bpg])
                    return pg, Bpg

                load_chunk(0)
                for c in range(NCH):
                    b = c % 2
                    if c + 1 < NCH:
                        load_chunk(c + 1)
                    wi = c * LPC
                    for mh in range(2):
                        wsl, Bwsl = ws.get(wi)
                        for mm in range(4):
                            pg, Bpg = mm_group(wsl, Bwsl, 8, mm, u2c[b], [Bu2c[b]])
                            P.add("act", lambda e, pg=pg, mm=mm: e.activation(out=sga[:, mm, :], in_=pg[:], func=AF.Sigmoid), reads=[Bpg], writes=[Bsga[mm]])
                        wsl, Bwsl = ws.get(wi + 1)
                        for mm in range(4):
                            pg, Bpg = mm_group(wsl, Bwsl, 8, mm, u2c[b], [Bu2c[b]])
                            P.add("act", lambda e, pg=pg, mm=mm: e.activation(out=sgb[:, mm, :], in_=pg[:], func=AF.Sigmoid), reads=[Bpg], writes=[Bsgb[mm]])
                        wsl, Bwsl = ws.get(wi + 2)
                        for mm in range(4):
                            pg, Bpg = mm_group(wsl, Bwsl, 4, mm, ont[b], [Bont[b]])
                            P.add("dve", lambda e, pg=pg, mm=mm: e.tensor_tensor(out=sga[:, mm, :], in0=pg[:], in1=sga[:, mm, :], op=ALU.mult),
                                  reads=[Bpg, Bsga[mm]], writes=[Bsga[mm]])
                        wsl, Bwsl = ws.get(wi + 3)
                        for mm in range(4):
                            pg, Bpg = mm_group(wsl, Bwsl, 4, mm, ort[b], [Bort[b]])
                            P.add("dve", lambda e, pg=pg, mm=mm: e.tensor_tensor(out=sgb[:, mm, :], in0=pg[:], in1=sgb[:, mm, :], op=ALU.mult),
                                  reads=[Bpg, Bsgb[mm]], writes=[Bsgb[mm]])
                            m = mh * 4 + mm
                            P.add("pool", lambda e, m=m, mm=mm: e.tensor_tensor(out=mixed[:, m, :], in0=sga[:, mm, :], in1=sgb[:, mm, :], op=ALU.add),
                                  reads=[Bsga[mm], Bsgb[mm]], writes=[Bmixed[m]])
                        wi += 4
                    for mh in range(2):
                        wsl, Bwsl = ws.get(wi)
                        wi += 1
                        for mm in range(4):
                            m = mh * 4 + mm
                            pg, Bpg = mm_group(wsl, Bwsl, 8, mm, mixed, Bmixed)
                            P.add("dve", lambda e, pg=pg, m=m, b=b: e.scalar_tensor_tensor(out=hT[b][:, m, :], in0=pg[:], scalar=GT2[:, m:m + 1], in1=hT[b][:, m, :],
                                                                                        op0=ALU.mult, op1=ALU.add),
                                  reads=[Bpg, Bh[b][m], B_coef], writes=[Bh[b][m]])
                    rms_mod(cx, P, hT[b], Bh[b], u3, Bu3, G3, SH3, sq, Bsq, rstd, Brstd, tmpf, Btmpf)
                    wi = ffn(cx, P, ws, wi, u3, Bu3, hT[b], Bh[b], hid, Bhid, sa, Bsa, HG3)
                    rms_stats(cx, P, hT[b], Bh[b], sq, Bsq, rstd, Brstd)
                    for kt in range(8):
                        P.add("dve", lambda e, kt=kt, b=b: e.scalar_tensor_tensor(out=hT[b][:, kt, :], in0=hT[b][:, kt, :], scalar=FW[:, kt:kt + 1], in1=rstd[:],
                                                                                 op0=ALU.mult, op1=ALU.mult),
                              reads=[Bh[b][kt], Brstd, B_coef], writes=[Bh[b][kt]])
                    for j in range(4):
                        for a in range(2):
                            pt, Bpt = cx.bank()
                            for q in range(4):
                                kt = a * 4 + q
                                P.add("pe", lambda e, pt=pt, j=j, kt=kt, q=q, b=b: e.transpose(pt[:, q * 128:(q + 1) * 128], hT[b][:, kt, j * 128:(j + 1) * 128], ident_f[:]),
                                      reads=[Bh[b][kt], B_const], writes=[Bpt])
                            dst = ostage[:, j, a * 512:(a + 1) * 512]
                            if (j * 2 + a) % 2 == 0:
                                P.add("act", lambda e, dst=dst, pt=pt: e.copy(out=dst, in_=pt[:]), reads=[Bpt], writes=[Bost])
                            else:
                                P.add("dve", lambda e, dst=dst, pt=pt: e.tensor_copy(out=dst, in_=pt[:]), reads=[Bpt], writes=[Bost])
                    P.add("sp", lambda e, c=c: e.dma_start(out=outv[c], in_=ostage[:]), reads=[Bost], dma_key="ost")
                P.emit(st)
    return nc


def host_tables():
    pos = np.arange(S, dtype=np.float32)
    inv = (10000.0 ** (-np.arange(32, dtype=np.float32) / 32)).astype(np.float32)
    ang = pos[:, None] * inv[None, :]
    tab = np.concatenate([np.cos(ang), np.sin(ang)], axis=1).astype(np.float32)
    rope_tab = np.ascontiguousarray(tab.reshape(32, 128, 64).transpose(1, 0, 2))
    H = 8
    log_g = np.log(1.0 - 2.0 ** (-5.0 - np.arange(H, dtype=np.float64)))
    p = np.arange(128, dtype=np.float64)
    qdec = np.exp(log_g[None, :] * (p[:, None] + 1.0))
    kinv = np.exp(-log_g[None, :] * (p[:, None] + 1.0)) * (64 ** -0.5)
    gC = np.exp(log_g * 128.0)
    gcp = np.zeros((128, 8))
    for pr in range(4):
        gcp[0:64, pr] = gC[2 * pr]
        gcp[64:128, pr] = gC[2 * pr + 1]
    ret_tab = np.concatenate([qdec, kinv, gcp], axis=1).astype(np.float32)
    return rope_tab, ret_tab


def make_in_maps(inputs):
    f = lambda a: np.ascontiguousarray(np.asarray(a, dtype=np.float32))
    colT = lambda v: np.ascontiguousarray(f(v).reshape(-1, 128).T)
    rope_tab, ret_tab = host_tables()
    nwT = np.concatenate([colT(inputs["norm1_w"][0]), colT(inputs["norm2_w"][0]), colT(inputs["norm3_w"][0]), colT(inputs["final_norm_w"])], axis=1)
    shared = {
        "ada_w": f(inputs["ada_w"][0]), "ada_bT": colT(inputs["ada_b"][0]), "nwT": np.ascontiguousarray(nwT),
        "ffn1_w_in": f(inputs["ffn1_w_in"][0]), "ffn1_w_out": f(inputs["ffn1_w_out"][0]), "w_in": f(inputs["w_in"][0]),
        "cmp_k_pe": f(inputs["cmp_k_pe"][0]), "cmp_k_w1": f(inputs["cmp_k_w1"][0]), "cmp_k_w2": f(inputs["cmp_k_w2"][0]),
        "cmp_v_pe": f(inputs["cmp_v_pe"][0]), "cmp_v_w1": f(inputs["cmp_v_w1"][0]), "cmp_v_w2": f(inputs["cmp_v_w2"][0]),
        "ret_norm_w": f(inputs["ret_norm_w"][0]).reshape(1, 512), "w_nsa_up": f(inputs["w_nsa_up"][0]), "w_ret_up": f(inputs["w_ret_up"][0]),
        "w_out": f(inputs["w_out"][0]), "ffn2_w_in": f(inputs["ffn2_w_in"][0]), "ffn2_w_out": f(inputs["ffn2_w_out"][0]),
        "rope_tab": rope_tab, "ret_tab": ret_tab,
    }
    maps = []
    for b in range(8):
        m = dict(shared)
        m["x"] = f(inputs["x"][b])
        m["cT"] = colT(inputs["c"][b])
        maps.append(m)
    return maps


def kernel(**inputs):
    nc = build()
    in_maps = make_in_maps(inputs)
    res = run_bass_kernel_spmd(nc, in_maps, core_ids=list(range(8)))
    return np.stack([np.asarray(r["out"], dtype=np.float32) for r in res.results], axis=0)
```

```python
import numpy as np
from contextlib import ExitStack
import concourse.bass as bass
import concourse.mybir as mybir
from concourse.bass_utils import run_bass_kernel_spmd

F32 = mybir.dt.float32
BF16 = mybir.dt.bfloat16
AF = mybir.ActivationFunctionType
ALU = mybir.AluOpType
AX = mybir.AxisListType

S = 4096
D = 1024
DFF = 2816
INW = 5400
NCH = 8
CT = 512
EPS = 1e-6
NEGM = -30000.0
ENGS = ["pe", "act", "dve", "pool", "sp"]

C_NQ, C_NKC, C_NVC, C_NKS, C_NVS, C_NKW, C_NVW, C_NGT, C_RQ, C_RK, C_RV, C_RG, C_GA, C_GB = (
    0, 512, 640, 768, 896, 1024, 1152, 1280, 1304, 1816, 2328, 2840, 3352, 4376)


class Buf:
    __slots__ = ("name", "last_w", "readers", "excl")

    def __init__(self, name, excl=False):
        self.name = name
        self.last_w = None
        self.readers = []
        self.excl = excl


class Op:
    __slots__ = ("eng", "fn", "deps", "signal", "val", "dma_key", "dma_val", "prog")

    def __init__(self, eng, fn):
        self.prog = None
        self.eng = eng
        self.fn = fn
        self.deps = []
        self.signal = False
        self.val = None
        self.dma_key = None
        self.dma_val = None


class Prog:
    def __init__(self, nc, name):
        self.nc = nc
        self.name = name
        self.ops = {e: [] for e in ENGS}
        self.dma_cnt = {}
        self.dma_last = {}

    def add(self, eng, fn, reads=(), writes=(), dma_key=None):
        op = Op(eng, fn)
        op.prog = self
        deps = []
        for b in reads:
            if b.last_w is not None:
                deps.append(b.last_w)
            if b.excl:
                deps.extend(b.readers)
        for b in writes:
            if b.last_w is not None:
                deps.append(b.last_w)
            deps.extend(b.readers)
        seen = set()
        for d in deps:
            if d is op or id(d) in seen or d.prog is not self:
                continue
            seen.add(id(d))
            if d.dma_key is None and d.eng == eng and eng == "pe":
                continue
            op.deps.append(d)
            if d.dma_key is None:
                d.signal = True
        if dma_key is not None:
            op.dma_key = dma_key
            self.dma_cnt[dma_key] = self.dma_cnt.get(dma_key, 0) + 16
            op.dma_val = self.dma_cnt[dma_key]
            self.dma_last[dma_key] = op
        for b in reads:
            b.readers.append(op)
        for b in writes:
            b.last_w = op
            b.readers = []
        self.ops[eng].append(op)
        return op

    def finish(self):
        op = Op("sp", lambda e: e.nop())
        op.deps = list(self.dma_last.values())
        self.ops["sp"].append(op)

    def emit(self, stack):
        nc = self.nc
        self.finish()
        esem = {e: stack.enter_context(nc.semaphore("s_%s_%s" % (self.name, e))) for e in ENGS}
        dsem = {k: stack.enter_context(nc.semaphore("d_%s_%s" % (self.name, str(k)))) for k in self.dma_cnt}
        for e in ENGS:
            c = 0
            for op in self.ops[e]:
                if op.dma_key is None and op.signal:
                    c += 1
                    op.val = c
        block = stack.enter_context(nc.Block())

        def run(e, engobj):
            waited = {}
            for op in self.ops[e]:
                for d in op.deps:
                    if d.dma_key is not None:
                        s, v = dsem[d.dma_key], d.dma_val
                    else:
                        s, v = esem[d.eng], d.val
                    key = id(s)
                    if waited.get(key, 0) >= v:
                        continue
                    waited[key] = v
                    engobj.wait_ge(s, v)
                ins = op.fn(engobj)
                if op.dma_key is not None:
                    ins.then_inc(dsem[op.dma_key], 16)
                elif op.signal:
                    ins.then_inc(esem[e], 1)

        @block.tensor
        def _(eng):
            run("pe", eng)

        @block.scalar
        def _(eng):
            run("act", eng)

        @block.vector
        def _(eng):
            run("dve", eng)

        @block.gpsimd
        def _(eng):
            run("pool", eng)

        @block.sync
        def _(eng):
            run("sp", eng)


class Ctx:
    def __init__(self, nc, P, st):
        self.nc, self.P, self.st = nc, P, st
        self.banks = []
        self.bank_i = 0

    def sb(self, name, shape, dt=F32):
        return self.st.enter_context(self.nc.sbuf_tensor(self.P.name + "_" + name, shape, dt))

    def psum_banks(self, n, ncols=512, dt=F32):
        for i in range(n):
            t = self.st.enter_context(self.nc.psum_tensor("%s_ps%d" % (self.P.name, len(self.banks)), [128, ncols], dt))
            self.banks.append((t, Buf("ps%d" % len(self.banks), excl=True)))

    def bank(self):
        b = self.banks[self.bank_i % len(self.banks)]
        self.bank_i += 1
        return b


class WStream:
    def __init__(self, cx, name, nbuf, slot_elems, dt=BF16, eng="pool"):
        self.cx, self.name, self.nbuf, self.eng = cx, name, nbuf, eng
        self.tiles = [cx.sb("%s%d" % (name, i), [128, slot_elems], dt) for i in range(nbuf)]
        self.bufs = [Buf("%s%d" % (name, i)) for i in range(nbuf)]
        self.loads = []
        self.issued = 0

    def plan(self, loads):
        self.loads = loads

    def _issue(self, i):
        kt, ncols, src = self.loads[i]
        slot = i % self.nbuf
        view = self.tiles[slot][:, 0:kt * ncols].rearrange("p (k c) -> p k c", k=kt)
        self.cx.P.add(self.eng, lambda e, view=view, src=src: e.dma_start(out=view, in_=src),
                      writes=[self.bufs[slot]], dma_key=(self.name, slot))

    def get(self, i):
        while self.issued < min(len(self.loads), i + self.nbuf - 1):
            self._issue(self.issued)
            self.issued += 1
        kt, ncols, src = self.loads[i]
        slot = i % self.nbuf
        view = self.tiles[slot][:, 0:kt * ncols].rearrange("p (k c) -> p k c", k=kt)
        return view, self.bufs[slot]


def wslab(w_ap, kt, c0, ncols):
    return w_ap.rearrange("(k p) n -> p k n", p=128)[:, 0:kt, c0:c0 + ncols]


def build(debug=False, phases=(0, 1, 2, 3)):
    nc = bass.Bass("TRN2", target_bir_lowering=False)
    dram_in = lambda name, shape, dt=F32: nc.dram_tensor(name, shape, dt, kind="ExternalInput").ap()
    x = dram_in("x", [S, D])
    cT = dram_in("cT", [128, 8])
    ada_w = dram_in("ada_w", [D, 9 * D])
    ada_bT = dram_in("ada_bT", [128, 72])
    nwT = dram_in("nwT", [128, 32])
    ffn1_w_in = dram_in("ffn1_w_in", [D, 2 * DFF])
    ffn1_w_out = dram_in("ffn1_w_out", [DFF, D])
    w_in = dram_in("w_in", [D, INW])
    cmp_k_pe = dram_in("cmp_k_pe", [32, 64])
    cmp_k_w1 = dram_in("cmp_k_w1", [2048, 128])
    cmp_k_w2 = dram_in("cmp_k_w2", [128, 64])
    cmp_v_pe = dram_in("cmp_v_pe", [32, 64])
    cmp_v_w1 = dram_in("cmp_v_w1", [2048, 128])
    cmp_v_w2 = dram_in("cmp_v_w2", [128, 64])
    ret_norm_w = dram_in("ret_norm_w", [1, 512])
    w_nsa_up = dram_in("w_nsa_up", [512, D])
    w_ret_up = dram_in("w_ret_up", [512, D])
    w_out = dram_in("w_out", [D, D])
    ffn2_w_in = dram_in("ffn2_w_in", [D, 2 * DFF])
    ffn2_w_out = dram_in("ffn2_w_out", [DFF, D])
    rope_tab = dram_in("rope_tab", [128, 32, 64])
    ret_tab = dram_in("ret_tab", [128, 24])
    out = nc.dram_tensor("out", [S, D], F32, kind="ExternalOutput").ap()

    skind = "ExternalOutput" if debug else "Internal"
    scr = lambda name, shape, dt: nc.dram_tensor(name, shape, dt, kind=skind).ap()
    h1T = scr("h1T", [D, S], F32)
    u2T = scr("u2T", [D, S], BF16)
    QT = scr("QT", [8, 64, S], BF16)
    KVT = scr("KVT", [4, 128, S], BF16)
    TOKF = scr("TOKF", [S, 1560], F32)
    TOKB = scr("TOKB", [S, 768], BF16)
    ONT = scr("ONT", [512, S], BF16)
    ORT = scr("ORT", [512, S], BF16)
    modT_d = scr("modT_d", [128, 72], F32)
    GT = scr("GT", [24, S], F32)

    with ExitStack() as gst:
        modT = gst.enter_context(nc.sbuf_tensor("modT", [128, 72], F32))
        coef = gst.enter_context(nc.sbuf_tensor("coef", [128, 96], F32))
        ones_bf = gst.enter_context(nc.sbuf_tensor("ones_bf", [128, 128], BF16))
        ident_f = gst.enter_context(nc.sbuf_tensor("ident_f", [128, 128], F32))
        ident_b = gst.enter_context(nc.sbuf_tensor("ident_b", [128, 128], BF16))
        B_modT, B_coef, B_const = Buf("modT"), Buf("coef"), Buf("const")
        G1, SH1, HG1, G2, SH2, GT2, G3, SH3, HG3, FW = [coef[:, i * 8:(i + 1) * 8] for i in range(10)]

        with ExitStack() as st:
            P = Prog(nc, "p0")
            cx = Ctx(nc, P, st)
            cx.psum_banks(4)
            pm, Bpm = cx.banks.pop(0)
            csb = cx.sb("csb", [128, 8])
            sil = cx.sb("sil", [128, 8])
            abT = cx.sb("abT", [128, 72])
            nw = cx.sb("nw", [128, 32])
            tmp = cx.sb("tmp", [128, 8])
            Bc, Bs, Bab, Bnw, Btmp = Buf("c"), Buf("sil"), Buf("ab"), Buf("nw"), Buf("tmp")
            P.add("sp", lambda e: e.dma_start(out=csb[:], in_=cT), writes=[Bc], dma_key="c")
            P.add("sp", lambda e: e.dma_start(out=abT[:], in_=ada_bT), writes=[Bab], dma_key="ab")
            P.add("sp", lambda e: e.dma_start(out=nw[:], in_=nwT), writes=[Bnw], dma_key="nw")
            P.add("pool", lambda e: e.memset(ones_bf[:], 1.0), writes=[B_const])
            P.add("pool", lambda e: e.memset(ident_f[:], 1.0), writes=[B_const])
            P.add("pool", lambda e: e.affine_select(out=ident_f[:], in_=ident_f[:], pattern=[[-1, 128]], compare_op=ALU.is_equal,
                                                    fill=0.0, base=0, channel_multiplier=1), reads=[B_const], writes=[B_const])
            P.add("pool", lambda e: e.tensor_copy(out=ident_b[:], in_=ident_f[:]), reads=[B_const], writes=[B_const])
            P.add("act", lambda e: e.activation(out=sil[:], in_=csb[:], func=AF.Silu), reads=[Bc], writes=[Bs])
            modrow = cx.sb("modrow", [1, 9216])
            Bmr = Buf("modrow")
            one_f = cx.sb("one_f", [1, 1])
            P.add("pool", lambda e: e.memset(one_f[:], 1.0), writes=[B_const])
            ws = WStream(cx, "ada", 4, 4096, dt=F32, eng="sp")
            ws.plan([(8, 512, wslab(ada_w, 8, s_ * 512, 512)) for s_ in range(18)])
            for s_ in range(18):
                wv, Bw = ws.get(s_)
                pr_, Bpr_ = cx.bank()
                for k in range(8):
                    P.add("pe", lambda e, wv=wv, k=k, pr_=pr_: e.matmul(pr_[0:1, :], lhsT=sil[:, k:k + 1], rhs=wv[:, k, :], start=(k == 0), stop=(k == 7)),
                          reads=[Bw, Bs], writes=[Bpr_])
                if s_ % 2 == 0:
                    P.add("act", lambda e, pr_=pr_, s_=s_: e.copy(out=modrow[0:1, s_ * 512:(s_ + 1) * 512], in_=pr_[0:1, :]), reads=[Bpr_], writes=[Bmr])
                else:
                    P.add("dve", lambda e, pr_=pr_, s_=s_: e.tensor_copy(out=modrow[0:1, s_ * 512:(s_ + 1) * 512], in_=pr_[0:1, :]), reads=[Bpr_], writes=[Bmr])
            for j in range(72):
                P.add("pe", lambda e, j=j: e.matmul(pm[:, j:j + 1], lhsT=modrow[0:1, j * 128:(j + 1) * 128], rhs=one_f[0:1, 0:1], start=True, stop=True, skip_group_check=True),
                      reads=[Bmr, B_const], writes=[Bpm])
            P.add("dve", lambda e: e.tensor_tensor(out=modT[:], in0=pm[:, 0:72], in1=abT[:], op=ALU.add), reads=[Bpm, Bab], writes=[B_modT])
            mv = lambda v: modT[:, v * 8:(v + 1) * 8]
            for (gi, nwi, sci) in ((G1, 0, 1), (G2, 1, 4), (G3, 2, 7)):
                P.add("dve", lambda e, sci=sci: e.tensor_scalar(out=tmp[:], in0=mv(sci), scalar1=1.0, scalar2=None, op0=ALU.add),
                      reads=[B_modT], writes=[Btmp])
                P.add("dve", lambda e, gi=gi, nwi=nwi: e.tensor_tensor(out=gi, in0=tmp[:], in1=nw[:, nwi * 8:(nwi + 1) * 8], op=ALU.mult),
                      reads=[Btmp, Bnw], writes=[B_coef])
            for (dst, src, sc) in ((SH1, 0, 1.0), (HG1, 2, 0.5), (SH2, 3, 1.0), (GT2, 5, 1.0), (SH3, 6, 1.0), (HG3, 8, 0.5)):
                P.add("dve", lambda e, dst=dst, src=src, sc=sc: e.tensor_scalar(out=dst, in0=mv(src), scalar1=sc, scalar2=None, op0=ALU.mult),
                      reads=[B_modT], writes=[B_coef])
            P.add("dve", lambda e: e.tensor_copy(out=FW, in_=nw[:, 24:32]), reads=[Bnw], writes=[B_coef])
            if debug:
                P.add("sp", lambda e: e.dma_start(out=modT_d, in_=modT[:]), reads=[B_modT], dma_key="dbg")
            P.emit(st)

        def rms_stats(cx, P, hT, Bh, sq, Bsq, rstd, Brstd):
            pss, Bpss = cx.bank()
            for kt in range(8):
                P.add("act", lambda e, kt=kt: e.activation(out=sq[kt % 2][:], in_=hT[:, kt, :], func=AF.Square),
                      reads=[Bh[kt]], writes=[Bsq[kt % 2]])
                P.add("pe", lambda e, kt=kt: e.matmul(pss[:], lhsT=ones_bf[:], rhs=sq[kt % 2][:], start=(kt == 0), stop=(kt == 7)),
                      reads=[Bsq[kt % 2], B_const], writes=[Bpss])
            P.add("act", lambda e: e.activation(out=rstd[:], in_=pss[:], func=AF.Sqrt, bias=eps_t[:, 0:1], scale=1.0 / D),
                  reads=[Bpss, B_const], writes=[Brstd])
            P.add("dve", lambda e: e.reciprocal(out=rstd[:], in_=rstd[:]), reads=[Brstd], writes=[Brstd])

        def rms_mod(cx, P, hT, Bh, uT, Bu, g, sh, sq, Bsq, rstd, Brstd, tmpf, Btmpf):
            rms_stats(cx, P, hT, Bh, sq, Bsq, rstd, Brstd)
            for kt in range(8):
                P.add("dve", lambda e, kt=kt: e.tensor_tensor(out=tmpf[kt % 2][:], in0=hT[:, kt, :], in1=rstd[:], op=ALU.mult),
                      reads=[Bh[kt], Brstd], writes=[Btmpf[kt % 2]])
                P.add("act", lambda e, kt=kt: e.activation(out=uT[:, kt, :], in_=tmpf[kt % 2][:], func=AF.Identity,
                                                           bias=sh[:, kt:kt + 1], scale=g[:, kt:kt + 1]),
                      reads=[Btmpf[kt % 2], B_coef], writes=[Bu[kt]])

        def ffn(cx, P, ws, wi0, uT, Bu, hT, Bh, hidT, Bhid, sa, Bsa, hg):
            wi = wi0
            for s6 in range(6):
                ncols = 512 if s6 < 5 else 256
                wa, Bwa = ws.get(wi)
                wb, Bwb = ws.get(wi + 1)
                wi += 2
                for jj in range(ncols // 128):
                    j = s6 * 4 + jj
                    pa, Bpa = cx.bank()
                    pb, Bpb = cx.bank()
                    for kt in range(8):
                        P.add("pe", lambda e, pa=pa, wa=wa, jj=jj, kt=kt: e.matmul(pa[:], lhsT=wa[:, kt, jj * 128:(jj + 1) * 128], rhs=uT[:, kt, :],
                                                                                  start=(kt == 0), stop=(kt == 7)),
                              reads=[Bwa, Bu[kt]], writes=[Bpa])
                    for kt in range(8):
                        P.add("pe", lambda e, pb=pb, wb=wb, jj=jj, kt=kt: e.matmul(pb[:], lhsT=wb[:, kt, jj * 128:(jj + 1) * 128], rhs=uT[:, kt, :],
                                                                                  start=(kt == 0), stop=(kt == 7)),
                              reads=[Bwb, Bu[kt]], writes=[Bpb])
                    P.add("act", lambda e, pa=pa, j=j: e.activation(out=sa[j % 2][:], in_=pa[:], func=AF.Silu),
                          reads=[Bpa], writes=[Bsa[j % 2]])
                    P.add("dve", lambda e, pb=pb, j=j: e.tensor_tensor(out=hidT[:, j, :], in0=pb[:], in1=sa[j % 2][:], op=ALU.mult),
                          reads=[Bpb, Bsa[j % 2]], writes=[Bhid[j]])
            for m2 in range(4):
                wo, Bwo = ws.get(wi)
                wi += 1
                for mm in range(2):
                    m = m2 * 2 + mm
                    py, Bpy = cx.bank()
                    for ht in range(22):
                        P.add("pe", lambda e, py=py, wo=wo, mm=mm, ht=ht: e.matmul(py[:], lhsT=wo[:, ht, mm * 128:(mm + 1) * 128], rhs=hidT[:, ht, :],
                                                                                  start=(ht == 0), stop=(ht == 21)),
                              reads=[Bwo, Bhid[ht]], writes=[Bpy])
                    P.add("dve", lambda e, py=py, m=m: e.scalar_tensor_tensor(out=hT[:, m, :], in0=py[:], scalar=hg[:, m:m + 1], in1=hT[:, m, :],
                                                                            op0=ALU.mult, op1=ALU.add),
                          reads=[Bpy, Bh[m], B_coef], writes=[Bh[m]])
            return wi

        def ffn_loads(w_in_ap, w_out_ap):
            L = []
            for s6 in range(6):
                ncols = 512 if s6 < 5 else 256
                L.append((8, ncols, wslab(w_in_ap, 8, s6 * 512, ncols)))
                L.append((8, ncols, wslab(w_in_ap, 8, DFF + s6 * 512, ncols)))
            for m2 in range(4):
                L.append((22, 256, wslab(w_out_ap, 22, m2 * 256, 256)))
            return L

        eps_t = gst.enter_context(nc.sbuf_tensor("eps_t", [128, 1], F32))

        if 1 in phases:
            with ExitStack() as st:
                P = Prog(nc, "p1")
                cx = Ctx(nc, P, st)
                cx.psum_banks(7)
                P.add("pool", lambda e: e.memset(eps_t[:], EPS), writes=[B_const])
                xs = [cx.sb("xs%d" % i, [128, 4, D]) for i in range(1)] * 2
                Bxs = [Buf("xs%d" % i) for i in range(1)] * 2
                hT = [cx.sb("hT%d" % i, [128, 8, CT]) for i in range(2)]
                Bh = [[Buf("hT%d_%d" % (i, k)) for k in range(8)] for i in range(2)]
                u1 = cx.sb("u1", [128, 8, CT], BF16)
                Bu1 = [Buf("u1_%d" % k) for k in range(8)]
                u2 = cx.sb("u2", [128, 8, CT], BF16)
                Bu2 = [Buf("u2_%d" % k) for k in range(8)]
                hid = cx.sb("hid", [128, 22, CT], BF16)
                Bhid = [Buf("hid%d" % k) for k in range(22)]
                sa = [cx.sb("sa%d" % i, [128, CT], BF16) for i in range(2)]
                Bsa = [Buf("sa%d" % i) for i in range(2)]
                sq = [cx.sb("sq%d" % i, [128, CT], BF16) for i in range(2)]
                Bsq = [Buf("sq%d" % i) for i in range(2)]
                tmpf = [cx.sb("tmpf%d" % i, [128, CT]) for i in range(2)]
                Btmpf = [Buf("tmpf%d" % i) for i in range(2)]
                rstd = cx.sb("rstd", [128, CT])
                Brstd = Buf("rstd")
                qst = cx.sb("qst", [64, 8, CT], BF16)
                Bqst = Buf("qst")
                kvst = cx.sb("kvst", [128, 4, CT], BF16)
                Bkvst = Buf("kvst")
                gst = cx.sb("gst", [24, CT])
                Bgst = Buf("gst")
                tokf = cx.sb("tokf", [128, 4, 1560])
                Btokf = Buf("tokf")
                tokb = cx.sb("tokb", [128, 4, 768], BF16)
                Btokb = Buf("tokb")
                ws = WStream(cx, "w1", 4, 5632)
                PSL = [(C_NQ, 512), (C_NKC, 512), (C_NKW, 280), (C_RQ, 512), (C_RK, 512), (C_RV, 512), (C_RG, 512)]
                loads = []
                for c in range(NCH):
                    loads += ffn_loads(ffn1_w_in, ffn1_w_out)
                    loads += [(8, n, wslab(w_in, 8, c0, n)) for (c0, n) in PSL]
                ws.plan(loads)
                LPC = len(loads) // NCH
                xv = x.rearrange("(c j p) d -> c p j d", j=4, p=128)
                h1v = h1T.rearrange("(k p) t -> p k t", p=128)
                u2v = u2T.rearrange("(k p) t -> p k t", p=128)
                QTv = QT.rearrange("h d t -> d h t")
                KVTv = KVT.rearrange("f p t -> p f t")
                for c in range(NCH):
                    b = c % 2
                    t0 = c * CT
                    P.add("sp", lambda e, c=c, b=b: e.dma_start(out=xs[b][:], in_=xv[c]), writes=[Bxs[b]], dma_key="xs")
                    for j in range(4):
                        for a in range(2):
                            pt, Bpt = cx.bank()
                            for q in range(4):
                                kt = a * 4 + q
                                P.add("pe", lambda e, pt=pt, j=j, kt=kt, q=q, b=b: e.transpose(pt[:, q * 128:(q + 1) * 128], xs[b][:, j, kt * 128:(kt + 1) * 128], ident_f[:]),
                                      reads=[Bxs[b], B_const], writes=[Bpt])
                            eng = "act" if (j * 2 + a) % 2 == 0 else "dve"
                            dst = hT[b][:, a * 4:(a + 1) * 4, j * 128:(j + 1) * 128]
                            srcv = pt[:].rearrange("p (q t) -> p q t", q=4)
                            if eng == "act":
                                P.add("act", lambda e, dst=dst, srcv=srcv: e.copy(out=dst, in_=srcv), reads=[Bpt], writes=Bh[b][a * 4:(a + 1) * 4])
                            else:
                                P.add("dve", lambda e, dst=dst, srcv=srcv: e.tensor_copy(out=dst, in_=srcv), reads=[Bpt], writes=Bh[b][a * 4:(a + 1) * 4])
                    rms_mod(cx, P, hT[b], Bh[b], u1, Bu1, G1, SH1, sq, Bsq, rstd, Brstd, tmpf, Btmpf)
                    wi = ffn(cx, P, ws, c * LPC, u1, Bu1, hT[b], Bh[b], hid, Bhid, sa, Bsa, HG1)
                    P.add("sp", lambda e, b=b, t0=t0: e.dma_start(out=h1v[:, :, t0:t0 + CT], in_=hT[b][:]), reads=Bh[b], dma_key=("h1", b))
                    rms_mod(cx, P, hT[b], Bh[b], u2, Bu2, G2, SH2, sq, Bsq, rstd, Brstd, tmpf, Btmpf)
                    P.add("sp", lambda e, t0=t0: e.dma_start(out=u2v[:, :, t0:t0 + CT], in_=u2[:]), reads=Bu2, dma_key="u2st")
                    TOKFv = TOKF.rearrange("(c j p) n -> c p j n", j=4, p=128)
                    TOKBv = TOKB.rearrange("(c j p) n -> c p j n", j=4, p=128)

                    def tokmm(wsl, Bwsl, cc, ncol, evac):
                        for j in range(4):
                            pp, Bpp = cx.bank()
                            for kt in range(8):
                                P.add("pe", lambda e, pp=pp, kt=kt, j=j: e.matmul(
                                    pp[:, 0:ncol], lhsT=u2[:, kt, j * 128:(j + 1) * 128], rhs=wsl[:, kt, cc:cc + ncol],
                                    start=(kt == 0), stop=(kt == 7)), reads=[Bwsl, Bu2[kt]], writes=[Bpp])
                            evac(pp, Bpp, j)

                    def ev_b(off, ncol):
                        def f(pp, Bpp, j):
                            P.add("dve", lambda e: e.tensor_copy(out=tokb[:, j, off:off + ncol], in_=pp[:, 0:ncol]), reads=[Bpp], writes=[Btokb])
                        return f

                    def ev_f(off, fn, eng):
                        def f(pp, Bpp, j):
                            if eng == "dve":
                                P.add("dve", lambda e: e.tensor_copy(out=tokf[:, j, off:off + 512], in_=pp[:, 0:512]), reads=[Bpp], writes=[Btokf])
                            else:
                                P.add("act", lambda e: e.activation(out=tokf[:, j, off:off + 512], in_=pp[:, 0:512], func=fn), reads=[Bpp], writes=[Btokf])
                        return f

                    def ev_vwg(pp, Bpp, j):
                        P.add("dve", lambda e: e.tensor_copy(out=tokb[:, j, 128:256], in_=pp[:, 0:128]), reads=[Bpp], writes=[Btokb])
                        P.add("act", lambda e: e.activation(out=tokf[:, j, 1536:1560], in_=pp[:, 128:152], func=AF.Sigmoid), reads=[Bpp], writes=[Btokf])

                    def featmm(wsl, Bwsl, cc, f):
                        pk, Bpk = cx.bank()
                        for kt in range(8):
                            P.add("pe", lambda e, kt=kt: e.matmul(pk[:], lhsT=wsl[:, kt, cc:cc + 128], rhs=u2[:, kt, :], start=(kt == 0), stop=(kt == 7)),
                                  reads=[Bwsl, Bu2[kt]], writes=[Bpk])
                        if f % 2 == 0:
                            P.add("act", lambda e: e.copy(out=kvst[:, f, :], in_=pk[:]), reads=[Bpk], writes=[Bkvst])
                        else:
                            P.add("dve", lambda e: e.tensor_copy(out=kvst[:, f, :], in_=pk[:]), reads=[Bpk], writes=[Bkvst])

                    wq, Bwq = ws.get(wi)
                    for h in range(8):
                        pq, Bpq = cx.bank()
                        for kt in range(8):
                            P.add("pe", lambda e, pq=pq, h=h, kt=kt, wq=wq: e.matmul(pq[0:64, :], lhsT=wq[:, kt, h * 64:(h + 1) * 64], rhs=u2[:, kt, :],
                                                                             start=(kt == 0), stop=(kt == 7)),
                                  reads=[Bwq, Bu2[kt]], writes=[Bpq])
                        if h % 2 == 0:
                            P.add("act", lambda e, pq=pq, h=h: e.mul(out=qst[:, h, :], in_=pq[0:64, :], mul=0.125), reads=[Bpq], writes=[Bqst])
                        else:
                            P.add("dve", lambda e, pq=pq, h=h: e.tensor_scalar(out=qst[:, h, :], in0=pq[0:64, :], scalar1=0.125, scalar2=None, op0=ALU.mult),
                                  reads=[Bpq], writes=[Bqst])
                    P.add("sp", lambda e, t0=t0: e.dma_start(out=QTv[:, :, t0:t0 + CT], in_=qst[:]), reads=[Bqst], dma_key="qst")
                    wk1, Bwk1 = ws.get(wi + 1)
                    featmm(wk1, Bwk1, 0, 0)
                    featmm(wk1, Bwk1, 128, 1)
                    featmm(wk1, Bwk1, 256, 2)
                    tokmm(wk1, Bwk1, 384, 128, ev_b(0, 128))
                    wk2, Bwk2 = ws.get(wi + 2)
                    featmm(wk2, Bwk2, 0, 3)
                    pg_, Bpg_ = cx.bank()
                    for kt in range(8):
                        P.add("pe", lambda e, kt=kt, pg_=pg_, wk2=wk2: e.matmul(pg_[0:24, :], lhsT=wk2[:, kt, 256:280], rhs=u2[:, kt, :], start=(kt == 0), stop=(kt == 7)),
                              reads=[Bwk2, Bu2[kt]], writes=[Bpg_])
                    P.add("act", lambda e, pg_=pg_: e.activation(out=gst[:], in_=pg_[0:24, :], func=AF.Sigmoid), reads=[Bpg_], writes=[Bgst])
                    P.add("sp", lambda e, t0=t0: e.dma_start(out=GT[:, t0:t0 + CT], in_=gst[:]), reads=[Bgst], dma_key="gst")
                    P.add("sp", lambda e, t0=t0: e.dma_start(out=KVTv[:, :, t0:t0 + CT], in_=kvst[:]), reads=[Bkvst], dma_key="kvst")
                    tokmm(wk2, Bwk2, 128, 152, ev_vwg)
                    wr, Bwr = ws.get(wi + 3)
                    tokmm(wr, Bwr, 0, 512, ev_f(0, None, "dve"))
                    wr, Bwr = ws.get(wi + 4)
                    tokmm(wr, Bwr, 0, 512, ev_f(512, AF.Copy, "act"))
                    wr, Bwr = ws.get(wi + 5)
                    tokmm(wr, Bwr, 0, 512, ev_b(256, 512))
                    wr, Bwr = ws.get(wi + 6)
                    tokmm(wr, Bwr, 0, 512, ev_f(1024, AF.Silu, "act"))
                    P.add("sp", lambda e, c=c: e.dma_start(out=TOKFv[c], in_=tokf[:]), reads=[Btokf], dma_key="tokf")
                    P.add("sp", lambda e, c=c: e.dma_start(out=TOKBv[c], in_=tokb[:]), reads=[Btokb], dma_key="tokb")
                P.emit(st)

        if 2 in phases:
            with ExitStack() as st:
                P = Prog(nc, "p2a")
                cx = Ctx(nc, P, st)
                cx.psum_banks(8)
                Sbanks = cx.banks[0:3]
                Obanks = cx.banks[3:5]
                Cbanks = cx.banks[5:7]
                Mbank = cx.banks[7]
                rr = {"s": 0, "o": 0, "pt": 0}
                Qaug = [cx.sb("qaug%d" % i, [128, S], BF16) for i in range(4)]
                BQ = [[Buf("q%d_%d" % (i, c)) for c in range(NCH)] for i in range(4)]
                Ksl = cx.sb("ksl", [128, S], BF16)
                BKsl = Buf("ksl")
                Kw = cx.sb("kw", [128, S], BF16)
                BKw = Buf("kw")
                Vs = cx.sb("vs", [128, 32, 65], BF16)
                BVs = Buf("vs")
                Vw = cx.sb("vw", [128, 32, 65], BF16)
                BVw = Buf("vw")
                kcr = cx.sb("kcr", [64, S], BF16)
                vcr = cx.sb("vcr", [64, S], BF16)
                Bkcr, Bvcr = Buf("kcr"), Buf("vcr")
                Kc = cx.sb("kc", [128, 256], BF16)
                BKc = Buf("kc")
                Vc = cx.sb("vc", [128, 2, 129], BF16)
                BVc = Buf("vc")
                cmask = cx.sb("cmask", [128, 2, S], BF16)
                G = cx.sb("gates", [128, 32, 24])
                BG = Buf("gates")
                BIAS = cx.sb("bias", [128, 32, 64])
                caus = cx.sb("caus", [128, 128], BF16)
                anti = cx.sb("anti", [128, 128], BF16)
                Bc2 = Buf("const2")
                acc = [cx.sb("acc%d" % i, [128, 4, 256]) for i in range(2)]
                Bacc = [[Buf("acc%d_%d" % (i, h)) for h in range(4)] for i in range(2)]
                accb = cx.sb("accb", [128, 4, 256], BF16)
                Baccb = Buf("accb")
                impacc = [cx.sb("imp%d" % i, [128, 4, 64]) for i in range(2)]
                Bimp = [Buf("imp%d" % i) for i in range(2)]
                PT = [cx.sb("pt%d" % i, [128, 512], BF16) for i in range(8)]
                BPT = [Buf("pt%d" % i) for i in range(8)]
                w1k = cx.sb("w1k", [64, 32, 128], BF16)
                w1v = cx.sb("w1v", [64, 32, 128], BF16)
                w2k = cx.sb("w2k", [128, 64], BF16)
                w2v = cx.sb("w2v", [128, 64], BF16)
                peTk = cx.sb("peTk", [64, 32], BF16)
                peTv = cx.sb("peTv", [64, 32], BF16)
                Bcw = Buf("cw")
                cbias = cx.sb("cbias", [128, 2])
                Bcb = Buf("cbias")
                hidc = cx.sb("hidc", [128, 2, 256], BF16)
                Bhidc = [Buf("hidck"), Buf("hidcv")]
                sm = cx.sb("sm", [128, 64])
                Bsm = Buf("sm")
                vt = cx.sb("vt", [128, 2, 64])
                Bvt = Buf("vt")
                nm = cx.sb("nm", [128, 64], BF16)
                Bnm = Buf("nm")
                nm4 = cx.sb("nm4", [128, 4, 64], BF16)
                Bnm4 = [Buf("nm4_%d" % j) for j in range(4)]
                nmT = cx.sb("nmT", [128, CT], BF16)
                BnmT = Buf("nmT")
                onst = cx.sb("onst", [128, 2, CT], BF16)
                Bonst = Buf("onst")

                P.add("dve", lambda e: e.memset(Ksl[64:128, :], 1.0), writes=[Bc2])
                P.add("pool", lambda e: e.affine_select(out=Ksl[64:128, :], in_=Ksl[64:128, :], pattern=[[1, S]], compare_op=ALU.is_ge, fill=0.0,
                                                        base=0, channel_multiplier=-64), reads=[Bc2], writes=[Bc2])
                P.add("pool", lambda e: e.affine_select(out=Ksl[64:128, :], in_=Ksl[64:128, :], pattern=[[-1, S]], compare_op=ALU.is_ge, fill=0.0,
                                                        base=63, channel_multiplier=64), reads=[Bc2], writes=[Bc2])
                P.add("dve", lambda e: e.memset(Kw[64:128, :], 0.0), writes=[Bc2])
                for hh in range(4):
                    P.add("dve", lambda e, hh=hh: e.memset(Qaug[hh][64:128, :], 0.0), writes=BQ[hh])
                P.add("pool", lambda e: e.memset(Kc[64:128, :], 0.0), writes=[Bc2])
                P.add("dve", lambda e: e.memset(cmask[:], 0.0), writes=[Bc2])
                for nt in range(2):
                    P.add("pool", lambda e, nt=nt: e.affine_select(out=cmask[:, nt, :], in_=cmask[:, nt, :], pattern=[[1, S]], compare_op=ALU.is_ge, fill=NEGM,
                                                                  base=-31 - 16 * 128 * nt, channel_multiplier=-16), reads=[Bc2], writes=[Bc2])
                P.add("pool", lambda e: e.memset(caus[:], 0.0), writes=[Bc2])
                P.add("pool", lambda e: e.affine_select(out=caus[:], in_=caus[:], pattern=[[1, 128]], compare_op=ALU.is_ge, fill=NEGM, base=0, channel_multiplier=-1),
                      reads=[Bc2], writes=[Bc2])
                P.add("pool", lambda e: e.memset(anti[:], 0.0), writes=[Bc2])
                P.add("pool", lambda e: e.affine_select(out=anti[:], in_=anti[:], pattern=[[-1, 128]], compare_op=ALU.is_gt, fill=NEGM, base=0, channel_multiplier=1),
                      reads=[Bc2], writes=[Bc2])
                P.add("dve", lambda e: e.memset(BIAS[:], 0.0), writes=[Bc2])
                for half in range(2):
                    rows = BIAS[half * 64:(half + 1) * 64, :, :]
                    P.add("pool", lambda e, rows=rows, half=half: e.affine_select(out=rows, in_=rows, pattern=[[2, 32], [-1, 64]], compare_op=ALU.is_ge, fill=-1e4,
                                                                                 base=half, channel_multiplier=0), reads=[Bc2], writes=[Bc2])
                    P.add("pool", lambda e, rows=rows, half=half: e.affine_select(out=rows, in_=rows, pattern=[[-2, 32], [1, 64]], compare_op=ALU.not_equal, fill=1e4,
                                                                                 base=-half, channel_multiplier=0), reads=[Bc2], writes=[Bc2])
                    P.add("pool", lambda e, rows=rows, half=half: e.affine_select(out=rows, in_=rows, pattern=[[-2, 32], [1, 64]], compare_op=ALU.not_equal, fill=1e4,
                                                                                 base=1 - half, channel_multiplier=0), reads=[Bc2], writes=[Bc2])
                    P.add("pool", lambda e, rows=rows: e.affine_select(out=rows, in_=rows, pattern=[[0, 32], [1, 64]], compare_op=ALU.not_equal, fill=1e4,
                                                                      base=0, channel_multiplier=0), reads=[Bc2], writes=[Bc2])
                P.add("pool", lambda e: e.memset(Vs[:, :, 64:65], 1.0), writes=[Bc2])
                P.add("pool", lambda e: e.memset(Vw[:, :, 64:65], 1.0), writes=[Bc2])
                P.add("pool", lambda e: e.memset(Vc[:, :, 64:129], 1.0), writes=[Bc2])
                for nt in range(2):
                    ov = Vc[:, nt, 65:129]
                    P.add("pool", lambda e, ov=ov, nt=nt: e.affine_select(out=ov, in_=ov, pattern=[[-4, 64]], compare_op=ALU.is_ge, fill=0.0,
                                                                         base=nt * 128 + 1, channel_multiplier=1), reads=[Bc2], writes=[Bc2])
                    P.add("pool", lambda e, ov=ov, nt=nt: e.affine_select(out=ov, in_=ov, pattern=[[4, 64]], compare_op=ALU.is_ge, fill=0.0,
                                                                         base=3 - nt * 128, channel_multiplier=-1), reads=[Bc2], writes=[Bc2])
                P.add("dve", lambda e: e.memset(hidc[:], 0.0), writes=Bhidc)
                P.add("pool", lambda e: e.dma_start(out=w1k[:], in_=cmp_k_w1.rearrange("(l d) h -> d l h", d=64)), writes=[Bcw], dma_key="cw0")
                P.add("pool", lambda e: e.dma_start(out=w1v[:], in_=cmp_v_w1.rearrange("(l d) h -> d l h", d=64)), writes=[Bcw], dma_key="cw1")
                P.add("pool", lambda e: e.dma_start(out=w2k[:], in_=cmp_k_w2), writes=[Bcw], dma_key="cw2")
                P.add("pool", lambda e: e.dma_start(out=w2v[:], in_=cmp_v_w2), writes=[Bcw], dma_key="cw3")
                P.add("pool", lambda e: e.dma_start(out=peTk[:], in_=cmp_k_pe.rearrange("l d -> d l"), allow_slow_non_contiguous=True), writes=[Bcw], dma_key="cw4")
                P.add("pool", lambda e: e.dma_start(out=peTv[:], in_=cmp_v_pe.rearrange("l d -> d l"), allow_slow_non_contiguous=True), writes=[Bcw], dma_key="cw5")
                P.add("sp", lambda e: e.dma_start(out=G[:], in_=TOKF[:, 1536:1560].rearrange("(k p) n -> p k n", p=128)), writes=[BG], dma_key="gates")
                for wi_, (w1sb, peT) in enumerate(((w1k, peTk), (w1v, peTv))):
                    pbk, Bpbk = Cbanks[wi_]
                    for l in range(32):
                        P.add("pe", lambda e, pbk=pbk, w1sb=w1sb, peT=peT, l=l: e.matmul(pbk[:, 0:1], lhsT=w1sb[:, l, :], rhs=peT[:, l:l + 1], start=(l == 0), stop=(l == 31)),
                              reads=[Bcw], writes=[Bpbk])
                    P.add("dve", lambda e, pbk=pbk, wi_=wi_: e.tensor_copy(out=cbias[:, wi_:wi_ + 1], in_=pbk[:, 0:1]), reads=[Bpbk], writes=[Bcb])

                def sbank(pool=None):
                    pool = pool or Sbanks
                    b = pool[rr["s"] % len(pool)]
                    rr["s"] += 1
                    return b

                def ptbuf():
                    i = rr["pt"] % 8
                    rr["pt"] += 1
                    return PT[i], BPT[i]

                def run_tiles(tiles, depth=2, pool=None):
                    pend = []

                    def pv(t):
                        for j in range(t["col0"] // 128, (t["col0"] + t["ncols"]) // 128):
                            o_ap, Bo = t["O"][j]
                            P.add("pe", lambda e, t=t, j=j, o_ap=o_ap: e.matmul(o_ap, lhsT=t["pt"][:, j * 128:(j + 1) * 128], rhs=t["V"], start=False, stop=True,
                                                                               skip_group_check=True),
                                  reads=[t["Bpt"], t["BV"]], writes=[Bo])
                        if t.get("after") is not None:
                            t["after"]()

                    for t in tiles:
                        while len(pend) >= depth:
                            pv(pend.pop(0))
                        if t.get("before") is not None:
                            t["before"]()
                        ps, Bps = sbank(pool)
                        c0, ncl = t["col0"], t["ncols"]
                        mk = t.get("mask")
                        P.add("pe", lambda e, t=t, ps=ps, c0=c0, ncl=ncl, mk=mk: e.matmul(ps[:, c0:c0 + ncl], lhsT=t["K"], rhs=t["Q"][:, t["q0"] + c0:t["q0"] + c0 + ncl],
                                                                                        start=True, stop=(mk is None)),
                              reads=[t["BK"], t["BQ"]], writes=[Bps])
                        if mk is not None:
                            m_ap, mcol, mn = mk
                            P.add("pe", lambda e, ps=ps, m_ap=m_ap, mcol=mcol, mn=mn: e.matmul(ps[:, mcol:mcol + mn], lhsT=ident_b[:], rhs=m_ap, start=False, stop=True),
                                  reads=[Bc2, B_const], writes=[Bps])
                        pt, Bpt = ptbuf()
                        t["pt"], t["Bpt"] = pt, Bpt
                        P.add("act", lambda e, pt=pt, ps=ps, c0=c0, ncl=ncl: e.activation(out=pt[:, c0:c0 + ncl], in_=ps[:, c0:c0 + ncl], func=AF.Exp),
                              reads=[Bps], writes=[Bpt])
                        pend.append(t)
                    while pend:
                        pv(pend.pop(0))

                QTv2 = QT
                for g in range(2):
                    for hh in range(4):
                        P.add("sp", lambda e, hh=hh, g=g: e.dma_start(out=Qaug[hh][0:64, :], in_=QTv2[4 * g + hh]), writes=BQ[hh], dma_key=("q", hh))
                    P.add("sp", lambda e, g=g: e.dma_start(out=Ksl[0:64, :], in_=KVT[2, g * 64:(g + 1) * 64, :]), writes=[BKsl], dma_key="ksl")
                    P.add("sp", lambda e, g=g: e.dma_start(out=Kw[0:64, :], in_=KVT[3, g * 64:(g + 1) * 64, :]), writes=[BKw], dma_key="kw")
                    P.add("sp", lambda e, g=g: e.dma_start(out=kcr[:], in_=KVT[0, g * 64:(g + 1) * 64, :]), writes=[Bkcr], dma_key="kcr")
                    P.add("sp", lambda e, g=g: e.dma_start(out=vcr[:], in_=KVT[1, g * 64:(g + 1) * 64, :]), writes=[Bvcr], dma_key="vcr")
                    P.add("sp", lambda e, g=g: e.dma_start(out=Vs[:, :, 0:64], in_=TOKB[:, g * 64:(g + 1) * 64].rearrange("(k p) d -> p k d", p=128)),
                          writes=[BVs], dma_key="vs")
                    P.add("sp", lambda e, g=g: e.dma_start(out=Vw[:, :, 0:64], in_=TOKB[:, 128 + g * 64:128 + (g + 1) * 64].rearrange("(k p) d -> p k d", p=128)),
                          writes=[BVw], dma_key="vw")
                    for wi_, (raw, Braw, w1sb, w2sb) in enumerate(((kcr, Bkcr, w1k, w2k), (vcr, Bvcr, w1v, w2v))):
                        ph, Bph = Cbanks[wi_]
                        for l in range(32):
                            P.add("pe", lambda e, ph=ph, w1sb=w1sb, raw=raw, l=l: e.matmul(ph[:, 0:255], lhsT=w1sb[:, l, :], rhs=raw[:, l:l + 16 * 254 + 1:16],
                                                                                          start=(l == 0), stop=(l == 31)),
                                  reads=[Bcw, Braw], writes=[Bph])
                        P.add("act", lambda e, ph=ph, wi_=wi_: e.activation(out=hidc[:, wi_, 0:255], in_=ph[:, 0:255], func=AF.Silu, bias=cbias[:, wi_:wi_ + 1]),
                              reads=[Bph, Bcb], writes=[Bhidc[wi_]])
                    pk, Bpk = Cbanks[0]
                    P.add("pe", lambda e, pk=pk: e.matmul(pk[0:64, 0:256], lhsT=w2k[:], rhs=hidc[:, 0, :], start=True, stop=True), reads=[Bcw, Bhidc[0]], writes=[Bpk])
                    P.add("dve", lambda e, pk=pk: e.tensor_copy(out=Kc[0:64, :], in_=pk[0:64, 0:256]), reads=[Bpk], writes=[BKc])
                    pv_, Bpv_ = Cbanks[1]
                    for nt in range(2):
                        P.add("pe", lambda e, pv_=pv_, nt=nt: e.matmul(pv_[:, nt * 64:(nt + 1) * 64], lhsT=hidc[:, 1, nt * 128:(nt + 1) * 128], rhs=w2v[:], start=True, stop=True),
                              reads=[Bcw, Bhidc[1]], writes=[Bpv_])
                    P.add("dve", lambda e, pv_=pv_: e.tensor_copy(out=Vc[:, :, 0:64], in_=pv_[:, 0:128].rearrange("p (n d) -> p n d", n=2)), reads=[Bpv_], writes=[BVc])

                    for qc in range(NCH):
                        q0 = qc * CT
                        ab = qc % 2
                        tiles = []
                        nts = [0] if qc <= 3 else [0, 1]
                        for hh in range(4):
                            h = 4 * g + hh
                            OA, BOA = (Cbanks if hh % 2 == 0 else Obanks)[0]
                            OB, BOB = (Cbanks if hh % 2 == 0 else Obanks)[1]
                            Oj = [(OA[:, 0:129], BOA), (OA[:, 129:258], BOA), (OB[:, 0:129], BOB), (OB[:, 129:258], BOB)]

                            def before(OA=OA, OB=OB, BOA=BOA, BOB=BOB):
                                P.add("dve", lambda e: e.memset(OA[:, 0:258], 0.0), writes=[BOA])
                                P.add("dve", lambda e: e.memset(OB[:, 0:258], 0.0), writes=[BOB])

                            def after(OA=OA, OB=OB, BOA=BOA, BOB=BOB, hh=hh, h=h, qc=qc, ab=ab):
                                for half, (Ob, BOb) in enumerate(((OA, BOA), (OB, BOB))):
                                    den = Ob[:, 0:258].rearrange("p (j c) -> p j c", j=2)[:, :, 64]
                                    rd = sm[:, half * 2:half * 2 + 2]
                                    P.add("dve", lambda e, den=den, rd=rd: e.tensor_scalar(out=rd, in0=den, scalar1=1e-30, scalar2=None, op0=ALU.max), reads=[BOb], writes=[Bsm])
                                    P.add("dve", lambda e, rd=rd: e.reciprocal(out=rd, in_=rd), reads=[Bsm], writes=[Bsm])
                                    fc = sm[:, 4 + half * 2:4 + half * 2 + 2]
                                    gsl = G[:, 4 * qc + half * 2:4 * qc + half * 2 + 2, 3 * h + 0]
                                    P.add("dve", lambda e, fc=fc, rd=rd, gsl=gsl: e.tensor_tensor(out=fc, in0=rd, in1=gsl, op=ALU.mult), reads=[Bsm, BG], writes=[Bsm])
                                    for jj in range(2):
                                        j = half * 2 + jj
                                        P.add("dve", lambda e, Ob=Ob, jj=jj, j=j: e.tensor_scalar(out=acc[ab][:, j, hh * 64:(hh + 1) * 64], in0=Ob[:, jj * 129:jj * 129 + 64],
                                                                                              scalar1=sm[:, 4 + j:5 + j], scalar2=None, op0=ALU.mult),
                                              reads=[BOb, Bsm], writes=[Bacc[ab][hh]])
                                        if hh == 0:
                                            P.add("dve", lambda e, Ob=Ob, jj=jj, j=j: e.tensor_scalar(out=impacc[ab][:, j, :], in0=Ob[:, jj * 129 + 65:jj * 129 + 129],
                                                                                                  scalar1=sm[:, j:j + 1], scalar2=None, op0=ALU.mult),
                                                  reads=[BOb, Bsm], writes=[Bimp[ab]])
                                        else:
                                            P.add("dve", lambda e, Ob=Ob, jj=jj, j=j: e.scalar_tensor_tensor(out=impacc[ab][:, j, :], in0=Ob[:, jj * 129 + 65:jj * 129 + 129],
                                                                                                         scalar=sm[:, j:j + 1], in1=impacc[ab][:, j, :], op0=ALU.mult, op1=ALU.add),
                                                  reads=[BOb, Bsm, Bimp[ab]], writes=[Bimp[ab]])

                            for ti, nt in enumerate(nts):
                                need_mask = (nt == 1) or (qc <= 4)
                                tiles.append(dict(K=Kc[:, nt * 128:(nt + 1) * 128], BK=BKc, Q=Qaug[hh], BQ=BQ[hh][qc], q0=q0, col0=0, ncols=512,
                                                  mask=(cmask[:, nt, q0:q0 + CT], 0, 512) if need_mask else None,
                                                  V=Vc[:, nt, :], BV=BVc, O=Oj,
                                                  before=before if ti == 0 else None, after=after if ti == len(nts) - 1 else None))
                        run_tiles(tiles)
                        units = {1: [], 2: []}
                        for br, (Ka, BKa, Va, BVa) in ((2, (Kw, BKw, Vw, BVw)), (1, (Ksl, BKsl, Vs, BVs))):
                            for hh in range(4):
                                h = 4 * g + hh
                                Ob, BOb = Obanks[rr["o"] % 2]
                                rr["o"] += 1
                                Oj = [(Ob[:, j * 65:(j + 1) * 65], BOb) for j in range(4)]
                                tl = []
                                if br == 1:
                                    for kt in range(4 * qc):
                                        tl.append((kt, 0, 512, None))
                                else:
                                    for i in range(4):
                                        kt = 4 * qc - 4 + i
                                        if kt >= 0:
                                            tl.append((kt, 0, 128 * (i + 1), (anti[:], 128 * i, 128)))
                                for i in range(4):
                                    tl.append((4 * qc + i, 128 * i, 512 - 128 * i, (caus[:], 128 * i, 128)))

                                def before(Ob=Ob, BOb=BOb):
                                    P.add("dve", lambda e: e.memset(Ob[:, 0:260], 0.0), writes=[BOb])

                                def after(Ob=Ob, BOb=BOb, hh=hh, h=h, br=br, qc=qc, ab=ab):
                                    den = Ob[:, 0:260].rearrange("p (j c) -> p j c", j=4)[:, :, 64]
                                    P.add("dve", lambda e: e.tensor_scalar(out=sm[:, 0:4], in0=den, scalar1=1e-30, scalar2=None, op0=ALU.max), reads=[BOb], writes=[Bsm])
                                    P.add("dve", lambda e: e.reciprocal(out=sm[:, 0:4], in_=sm[:, 0:4]), reads=[Bsm], writes=[Bsm])
                                    gsl = G[:, 4 * qc:4 * qc + 4, 3 * h + br]
                                    P.add("dve", lambda e: e.tensor_tensor(out=sm[:, 4:8], in0=sm[:, 0:4], in1=gsl, op=ALU.mult), reads=[Bsm, BG], writes=[Bsm])
                                    for j in range(4):
                                        P.add("dve", lambda e, j=j: e.scalar_tensor_tensor(out=acc[ab][:, j, hh * 64:(hh + 1) * 64], in0=Ob[:, j * 65:j * 65 + 64],
                                                                                        scalar=sm[:, 4 + j:5 + j], in1=acc[ab][:, j, hh * 64:(hh + 1) * 64],
                                                                                        op0=ALU.mult, op1=ALU.add),
                                              reads=[BOb, Bsm, Bacc[ab][hh]], writes=[Bacc[ab][hh]])

                                ut = []
                                for ti, (kt, c0, ncl, mk) in enumerate(tl):
                                    ut.append(dict(K=Ka[:, kt * 128:(kt + 1) * 128], BK=BKa, Q=Qaug[hh], BQ=BQ[hh][qc], q0=q0, col0=c0, ncols=ncl, mask=mk,
                                                   V=Va[:, kt, :], BV=BVa, O=Oj,
                                                   before=before if ti == 0 else None, after=after if ti == len(tl) - 1 else None))
                                units[br].append(ut)
                        early = units[2][0] + units[2][1]
                        for ut in units[2][0:2]:
                            ut[0]["before"]()
                            ut[0]["before"] = None
                        pm_, Bpm_ = Mbank
                        for j in range(4):
                            qt = 4 * qc + j
                            vv = vt[:, j % 2, :]
                            P.add("dve", lambda e, vv=vv, j=j, qt=qt, ab=ab: e.tensor_tensor(out=vv, in0=impacc[ab][:, j, :], in1=BIAS[:, qt, :], op=ALU.add),
                                  reads=[Bimp[ab], Bc2], writes=[Bvt])
                            P.add("dve", lambda e, vv=vv: e.max(out=sm[:, 8:16], in_=vv), reads=[Bvt], writes=[Bsm])
                            P.add("dve", lambda e, vv=vv, j=j: e.match_replace(out=vt[:, (j + 1) % 2, :], in_to_replace=sm[:, 8:16], in_values=vv, imm_value=-1e9),
                                  reads=[Bvt, Bsm], writes=[Bvt])
                            P.add("dve", lambda e, j=j: e.max(out=sm[:, 16:24], in_=vt[:, (j + 1) % 2, :]), reads=[Bvt], writes=[Bsm])
                            P.add("dve", lambda e, vv=vv, j=j: e.tensor_scalar(out=nm4[:, j, :], in0=vv, scalar1=sm[:, 23:24], scalar2=NEGM, op0=ALU.is_lt, op1=ALU.mult),
                                  reads=[Bvt, Bsm], writes=[Bnm4[j]])
                        run_tiles(early, depth=4, pool=Sbanks + Cbanks)
                        for j in range(4):
                            P.add("pe", lambda e, pm_=pm_, j=j: e.matmul(pm_[64:128, j * 128:(j + 1) * 128], lhsT=nm4[:, j, :], rhs=ident_b[:], start=True, stop=True),
                                  reads=[Bnm4[j], B_const], writes=[Bpm_])
                        P.add("act", lambda e, pm_=pm_: e.copy(out=nmT[64:128, :], in_=pm_[64:128, :]), reads=[Bpm_], writes=[BnmT])
                        for hh in range(4):
                            eng = "pool" if hh % 2 == 0 else "dve"
                            P.add(eng, lambda e, hh=hh, q0=q0: e.tensor_copy(out=Qaug[hh][64:128, q0:q0 + CT], in_=nmT[64:128, :]), reads=[BnmT], writes=[BQ[hh][qc]])
                        rest = units[2][2] + units[2][3] + units[1][0] + units[1][1] + units[1][2] + units[1][3]
                        run_tiles(rest, depth=4, pool=Sbanks + Cbanks)
                        P.add("act", lambda e, ab=ab: e.copy(out=accb[:], in_=acc[ab][:]), reads=Bacc[ab], writes=[Baccb])
                        ptb, Bptb = Mbank
                        ptv = ptb[:].bitcast(BF16)
                        for f in range(2):
                            for j in range(4):
                                P.add("pe", lambda e, f=f, j=j, ptv=ptv: e.transpose(ptv[:, f * 512 + j * 128:f * 512 + (j + 1) * 128], accb[:, j, f * 128:(f + 1) * 128], ident_b[:]),
                                      reads=[Baccb, B_const], writes=[Bptb])
                        P.add("dve", lambda e, ptv=ptv: e.tensor_copy(out=onst[:], in_=ptv.rearrange("p (f t) -> p f t", f=2)), reads=[Bptb], writes=[Bonst])
                        P.add("sp", lambda e, g=g, q0=q0: e.dma_start(out=ONT[g * 256:(g + 1) * 256, q0:q0 + CT].rearrange("(f p) t -> p f t", p=128), in_=onst[:]),
                              reads=[Bonst], dma_key="onst")
                P.emit(st)

        if 2 in phases:
            with ExitStack() as st:
                P = Prog(nc, "p2b")
                cx = Ctx(nc, P, st)
                cx.psum_banks(8)
                bE, bO, bOo, bS, bTq, bTk, bTy = cx.banks[0:7]
                rope = cx.sb("rope", [128, 32, 64])
                rt = cx.sb("rt", [128, 24])
                rnw = cx.sb("rnw", [128, 512])
                mask01 = cx.sb("mask01", [128, 128])
                Bk = Buf("consts")
                qk = [cx.sb("qk%d" % i, [128, 1024]) for i in range(2)]
                rgs = [cx.sb("rg%d" % i, [128, 512]) for i in range(2)]
                rv = [cx.sb("rv%d" % i, [128, 512], BF16) for i in range(2)]
                Bqk = [Buf("qk%d" % i) for i in range(2)]
                Brg = [Buf("rg%d" % i) for i in range(2)]
                Brv = [Buf("rv%d" % i) for i in range(2)]
                tq = cx.sb("tq", [128, 4, 256])
                tk = cx.sb("tk", [128, 4, 256])
                Btq, Btk = Buf("tq"), Buf("tk")
                qr = cx.sb("qr", [128, 512])
                kr = cx.sb("kr", [128, 512])
                Bqr, Bkr = Buf("qr"), Buf("kr")
                qd = cx.sb("qd", [128, 512], BF16)
                kinv = cx.sb("kinv", [128, 512], BF16)
                Bqd, Bkinv = Buf("qd"), Buf("kinv")
                QdT = cx.sb("QdT", [128, 4, 128], BF16)
                KinvT = cx.sb("KinvT", [128, 4, 128], BF16)
                BQdT, BKinvT = Buf("QdT"), Buf("KinvT")
                innerT = cx.sb("innerT", [128, 8, 128], BF16)
                Binn = [Buf("innE"), Buf("innO")]
                St = cx.sb("St", [128, 4, 64])
                Sbf = cx.sb("Sbf", [128, 8, 64], BF16)
                BSt, BSbf = Buf("St"), Buf("Sbf")
                osb = cx.sb("osb", [128, 512])
                osq = cx.sb("osq", [128, 512])
                Bosb, Bosq = Buf("osb"), Buf("osq")
                stt = cx.sb("stt", [128, 40])
                Bstt = Buf("stt")
                ybf = cx.sb("ybf", [128, 512], BF16)
                Bybf = Buf("ybf")
                orst = cx.sb("orst", [128, 4, 128], BF16)
                Borst = Buf("orst")
                P.add("sp", lambda e: e.dma_start(out=rope[:], in_=rope_tab), writes=[Bk], dma_key="c0")
                P.add("sp", lambda e: e.dma_start(out=rt[:], in_=ret_tab), writes=[Bk], dma_key="c1")
                P.add("sp", lambda e: e.dma_start(out=rnw[:], in_=ret_norm_w.to_broadcast([128, 512])), writes=[Bk], dma_key="c2")
                P.add("pool", lambda e: e.memset(mask01[:], 1.0), writes=[Bk])
                P.add("pool", lambda e: e.affine_select(out=mask01[:], in_=mask01[:], pattern=[[1, 128]], compare_op=ALU.is_ge, fill=0.0, base=0, channel_multiplier=-1),
                      reads=[Bk], writes=[Bk])
                P.add("pool", lambda e: e.memset(St[:], 0.0), writes=[BSt])
                P.add("pool", lambda e: e.memset(Sbf[:], 0.0), writes=[BSbf])

                def load(i):
                    b = i % 2
                    r0 = i * 128
                    P.add("sp", lambda e: e.dma_start(out=qk[b][:], in_=TOKF[r0:r0 + 128, 0:1024]), writes=[Bqk[b]], dma_key=("qk", b))
                    P.add("sp", lambda e: e.dma_start(out=rgs[b][:], in_=TOKF[r0:r0 + 128, 1024:1536]), writes=[Brg[b]], dma_key=("rg", b))
                    P.add("sp", lambda e: e.dma_start(out=rv[b][:], in_=TOKB[r0:r0 + 128, 256:768]), writes=[Brv[b]], dma_key=("rv", b))

                def rope_apply(eng, src, Bsrc, tt, Btt, dst, Bdst, i):
                    x = src.rearrange("p (h d) -> p h d", h=8)
                    x1, x2 = x[:, :, 0:32], x[:, :, 32:64]
                    cos = rope[:, i:i + 1, 0:32].to_broadcast([128, 8, 32])
                    sin = rope[:, i:i + 1, 32:64].to_broadcast([128, 8, 32])
                    tv = [tt[:, k, :].rearrange("p (h d) -> p h d", h=8) for k in range(4)]
                    d = dst.rearrange("p (h d) -> p h d", h=8)
                    P.add(eng, lambda e: e.tensor_tensor(out=tv[0], in0=x1, in1=cos, op=ALU.mult), reads=[Bsrc, Bk], writes=[Btt])
                    P.add(eng, lambda e: e.tensor_tensor(out=tv[1], in0=x2, in1=sin, op=ALU.mult), reads=[Bsrc, Bk], writes=[Btt])
                    P.add(eng, lambda e: e.tensor_tensor(out=tv[2], in0=x1, in1=sin, op=ALU.mult), reads=[Bsrc, Bk], writes=[Btt])
                    P.add(eng, lambda e: e.tensor_tensor(out=tv[3], in0=x2, in1=cos, op=ALU.mult), reads=[Bsrc, Bk], writes=[Btt])
                    P.add(eng, lambda e: e.tensor_tensor(out=d[:, :, 0:32], in0=tv[0], in1=tv[1], op=ALU.subtract), reads=[Btt], writes=[Bdst])
                    P.add(eng, lambda e: e.tensor_tensor(out=d[:, :, 32:64], in0=tv[2], in1=tv[3], op=ALU.add), reads=[Btt], writes=[Bdst])

                ORv = ORT.rearrange("(f p) t -> p f t", p=128)
                tqv = bTq[0][:].bitcast(BF16)
                tkv = bTk[0][:].bitcast(BF16)

                def front(i):
                    b = i % 2
                    rope_apply("dve", qk[b][:, 0:512], Bqk[b], tq, Btq, qr[:], Bqr, i)
                    rope_apply("pool", qk[b][:, 512:1024], Bqk[b], tk, Btk, kr[:], Bkr, i)
                    P.add("dve", lambda e: e.tensor_tensor(out=qd[:].rearrange("p (h d) -> p h d", h=8), in0=qr[:].rearrange("p (h d) -> p h d", h=8),
                                                           in1=rt[:, 0:8].unsqueeze(2).to_broadcast([128, 8, 64]), op=ALU.mult), reads=[Bqr, Bk], writes=[Bqd])
                    P.add("pool", lambda e: e.tensor_tensor(out=kinv2[b][:].rearrange("p (h d) -> p h d", h=8), in0=kr[:].rearrange("p (h d) -> p h d", h=8),
                                                            in1=rt[:, 8:16].unsqueeze(2).to_broadcast([128, 8, 64]), op=ALU.mult), reads=[Bkr, Bk], writes=[Bkinv2[b]])
                    for pr in range(4):
                        P.add("pe", lambda e, pr=pr: e.transpose(tqv[:, pr * 128:(pr + 1) * 128], qd[:, pr * 128:(pr + 1) * 128], ident_b[:]),
                              reads=[Bqd, B_const], writes=[bTq[1]])
                    for pr in range(4):
                        P.add("pe", lambda e, pr=pr: e.transpose(tkv[:, pr * 128:(pr + 1) * 128], kinv2[b][:, pr * 128:(pr + 1) * 128], ident_b[:]),
                              reads=[Bkinv2[b], B_const], writes=[bTk[1]])
                    P.add("act", lambda e: e.copy(out=QdT2[b][:], in_=tqv[:, 0:512].rearrange("p (a t) -> p a t", a=4)), reads=[bTq[1]], writes=[BQdT2[b]])
                    P.add("act", lambda e: e.copy(out=KinvT2[b][:], in_=tkv[:, 0:512].rearrange("p (a t) -> p a t", a=4)), reads=[bTk[1]], writes=[BKinvT2[b]])

                kinv2 = [kinv, cx.sb("kinvB", [128, 512], BF16)]
                Bkinv2 = [Bkinv, Buf("kinvB")]
                QdT2 = [QdT, cx.sb("QdTB", [128, 4, 128], BF16)]
                BQdT2 = [BQdT, Buf("QdTB")]
                KinvT2 = [KinvT, cx.sb("KinvTB", [128, 4, 128], BF16)]
                BKinvT2 = [BKinvT, Buf("KinvTB")]
                def mid(i):
                    b = i % 2
                    QdT, BQdT, KinvT, BKinvT, kinv, Bkinv = QdT2[b], BQdT2[b], KinvT2[b], BKinvT2[b], kinv2[b], Bkinv2[b]
                    for par, (bk_, ) in enumerate(((bE,), (bO,))):
                        pb_, Bpb_ = bk_
                        lo = par * 64
                        for pr in range(4):
                            P.add("pe", lambda e, pb_=pb_, pr=pr, lo=lo, KinvT=KinvT, QdT=QdT: e.matmul(pb_[:, pr * 128:(pr + 1) * 128], lhsT=KinvT[lo:lo + 64, pr, :], rhs=QdT[lo:lo + 64, pr, :],
                                                                               start=True, stop=True), reads=[BKinvT, BQdT], writes=[Bpb_])
                        P.add("dve", lambda e, pb_=pb_, par=par: e.tensor_tensor(out=innerT[:, par * 4:(par + 1) * 4, :], in0=pb_[:].rearrange("p (a t) -> p a t", a=4),
                                                                                in1=mask01[:].unsqueeze(1).to_broadcast([128, 4, 128]), op=ALU.mult),
                              reads=[Bpb_, Bk], writes=[Binn[par]])
                    po, Bpo = bOo
                    for h in range(8):
                        pr, par = h // 2, h % 2
                        P.add("pe", lambda e, h=h, pr=pr, par=par, b=b: e.matmul(po[:, h * 64:(h + 1) * 64], lhsT=innerT[:, par * 4 + pr, :], rhs=rv[b][:, h * 64:(h + 1) * 64],
                                                                              start=True, stop=False, skip_group_check=True), reads=[Binn[par], Brv[b]], writes=[Bpo])
                        P.add("pe", lambda e, h=h, pr=pr, QdT=QdT: e.matmul(po[:, h * 64:(h + 1) * 64], lhsT=QdT[:, pr, :], rhs=Sbf[:, h, :], start=False, stop=True, skip_group_check=True),
                              reads=[BQdT, BSbf], writes=[Bpo])
                    pS, BpS = bS
                    for h in range(8):
                        pr, par = h // 2, h % 2
                        P.add("pe", lambda e, h=h, pr=pr, par=par, b=b, kinv=kinv: e.matmul(pS[par * 64:(par + 1) * 64, pr * 64:(pr + 1) * 64], lhsT=kinv[:, h * 64:(h + 1) * 64],
                                                                              rhs=rv[b][:, h * 64:(h + 1) * 64], start=True, stop=True, skip_group_check=True),
                              reads=[Bkinv, Brv[b]], writes=[BpS])
                    P.add("dve", lambda e: e.tensor_tensor(out=St[:], in0=pS[:, 0:256].rearrange("p (a d) -> p a d", a=4), in1=St[:], op=ALU.add), reads=[BpS, BSt], writes=[BSt])
                    P.add("dve", lambda e: e.tensor_tensor(out=St[:], in0=St[:], in1=rt[:, 16:20].unsqueeze(2).to_broadcast([128, 4, 64]), op=ALU.mult), reads=[BSt, Bk], writes=[BSt])
                    sbv = Sbf[:].rearrange("p (a two) d -> p a two d", two=2)
                    P.add("pool", lambda e: e.tensor_copy(out=sbv[0:64, :, 0, :], in_=St[0:64, :, :]), reads=[BSt], writes=[BSbf])
                    P.add("pool", lambda e: e.tensor_copy(out=sbv[64:128, :, 1, :], in_=St[64:128, :, :]), reads=[BSt], writes=[BSbf])
                    P.add("act", lambda e: e.copy(out=osb[:], in_=po[:]), reads=[Bpo], writes=[Bosb])
                    P.add("act", lambda e: e.activation(out=osq[:], in_=po[:], func=AF.Square), reads=[Bpo], writes=[Bosq])

                def tail(i):
                    b = i % 2
                    o3 = osb[:].rearrange("p (h d) -> p h d", h=8)
                    P.add("dve", lambda e: e.tensor_reduce(out=stt[:, 0:8], in_=o3, axis=AX.X, op=ALU.add), reads=[Bosb], writes=[Bstt])
                    P.add("dve", lambda e: e.tensor_reduce(out=stt[:, 8:16], in_=osq[:].rearrange("p (h d) -> p h d", h=8), axis=AX.X, op=ALU.add), reads=[Bosq], writes=[Bstt])
                    P.add("dve", lambda e: e.tensor_scalar(out=stt[:, 16:24], in0=stt[:, 0:8], scalar1=1.0 / 64, scalar2=None, op0=ALU.mult), reads=[Bstt], writes=[Bstt])
                    P.add("dve", lambda e: e.tensor_tensor(out=stt[:, 32:40], in0=stt[:, 16:24], in1=stt[:, 16:24], op=ALU.mult), reads=[Bstt], writes=[Bstt])
                    P.add("dve", lambda e: e.scalar_tensor_tensor(out=stt[:, 24:32], in0=stt[:, 8:16], scalar=1.0 / 64, in1=stt[:, 32:40], op0=ALU.mult, op1=ALU.subtract),
                          reads=[Bstt], writes=[Bstt])
                    P.add("act", lambda e: e.activation(out=stt[:, 24:32], in_=stt[:, 24:32], func=AF.Sqrt, bias=eps_t[:, 0:1]), reads=[Bstt, B_const], writes=[Bstt])
                    P.add("dve", lambda e: e.reciprocal(out=stt[:, 24:32], in_=stt[:, 24:32]), reads=[Bstt], writes=[Bstt])
                    P.add("dve", lambda e: e.tensor_tensor(out=o3, in0=o3, in1=stt[:, 16:24].unsqueeze(2).to_broadcast([128, 8, 64]), op=ALU.subtract), reads=[Bosb, Bstt], writes=[Bosb])
                    P.add("dve", lambda e: e.tensor_tensor(out=o3, in0=o3, in1=stt[:, 24:32].unsqueeze(2).to_broadcast([128, 8, 64]), op=ALU.mult), reads=[Bosb, Bstt], writes=[Bosb])
                    P.add("pool", lambda e: e.tensor_tensor(out=osb[:], in0=osb[:], in1=rnw[:], op=ALU.mult), reads=[Bosb, Bk], writes=[Bosb])
                    P.add("pool", lambda e, b=b: e.tensor_tensor(out=ybf[:], in0=osb[:], in1=rgs[b][:], op=ALU.mult), reads=[Bosb, Brg[b]], writes=[Bybf])
                    tyv = bTy[0][:].bitcast(BF16)
                    for f in range(4):
                        P.add("pe", lambda e, f=f, tyv=tyv: e.transpose(tyv[:, f * 128:(f + 1) * 128], ybf[:, f * 128:(f + 1) * 128], ident_b[:]), reads=[Bybf, B_const], writes=[bTy[1]])
                    P.add("act", lambda e, tyv=tyv: e.copy(out=orst[:], in_=tyv[:, 0:512].rearrange("p (a t) -> p a t", a=4)), reads=[bTy[1]], writes=[Borst])
                    P.add("sp", lambda e, i=i: e.dma_start(out=ORv[:, :, i * 128:(i + 1) * 128], in_=orst[:]), reads=[Borst], dma_key="orst")

                load(0)
                load(1)
                front(0)
                for i in range(32):
                    mid(i)
                    if i + 1 < 32:
                        front(i + 1)
                    tail(i)
                    if i + 2 < 32:
                        load(i + 2)
                P.emit(st)

        if 3 in phases:
            with ExitStack() as st:
                P = Prog(nc, "p3")
                cx = Ctx(nc, P, st)
                cx.psum_banks(7)
                hT = [cx.sb("hT%d" % i, [128, 8, CT]) for i in range(2)]
                Bh = [[Buf("hT%d_%d" % (i, k)) for k in range(8)] for i in range(2)]
                ont = [cx.sb("ont%d" % i, [128, 4, CT], BF16) for i in range(2)]
                Bont = [Buf("ont%d" % i) for i in range(2)]
                ort = [cx.sb("ort%d" % i, [128, 4, CT], BF16) for i in range(2)]
                Bort = [Buf("ort%d" % i) for i in range(2)]
                u2c = [cx.sb("u2c%d" % i, [128, 8, CT], BF16) for i in range(2)]
                Bu2c = [Buf("u2c%d" % i) for i in range(2)]
                mixed = cx.sb("mixed", [128, 8, CT], BF16)
                Bmixed = [Buf("mixed%d" % k) for k in range(8)]
                u3 = cx.sb("u3", [128, 8, CT], BF16)
                Bu3 = [Buf("u3_%d" % k) for k in range(8)]
                hid = cx.sb("hid", [128, 22, CT], BF16)
                Bhid = [Buf("hid%d" % k) for k in range(22)]
                sa = [cx.sb("sa%d" % i, [128, CT], BF16) for i in range(2)]
                Bsa = [Buf("sa%d" % i) for i in range(2)]
                sq = [cx.sb("sq%d" % i, [128, CT], BF16) for i in range(2)]
                Bsq = [Buf("sq%d" % i) for i in range(2)]
                tmpf = [cx.sb("tmpf%d" % i, [128, CT]) for i in range(2)]
                Btmpf = [Buf("tmpf%d" % i) for i in range(2)]
                rstd = cx.sb("rstd", [128, CT])
                Brstd = Buf("rstd")
                sga = cx.sb("sga", [128, 4, CT])
                Bsga = [Buf("sga%d" % k) for k in range(4)]
                sgb = cx.sb("sgb", [128, 4, CT])
                Bsgb = [Buf("sgb%d" % k) for k in range(4)]
                ostage = cx.sb("ostage", [128, 4, D])
                Bost = Buf("ostage")
                ws = WStream(cx, "w3", 4, 5632)
                loads = []
                for c in range(NCH):
                    for mh in range(2):
                        loads.append((8, 512, wslab(w_in, 8, C_GA + mh * 512, 512)))
                        loads.append((8, 512, wslab(w_in, 8, C_GB + mh * 512, 512)))
                        loads.append((4, 512, wslab(w_nsa_up, 4, mh * 512, 512)))
                        loads.append((4, 512, wslab(w_ret_up, 4, mh * 512, 512)))
                    for mh in range(2):
                        loads.append((8, 512, wslab(w_out, 8, mh * 512, 512)))
                    loads += ffn_loads(ffn2_w_in, ffn2_w_out)
                ws.plan(loads)
                LPC = len(loads) // NCH
                h1v = h1T.rearrange("(k p) t -> p k t", p=128)
                u2v = u2T.rearrange("(k p) t -> p k t", p=128)
                ONv = ONT.rearrange("(k p) t -> p k t", p=128)
                ORv = ORT.rearrange("(k p) t -> p k t", p=128)
                outv = out.rearrange("(c j p) d -> c p j d", j=4, p=128)

                def load_chunk(c):
                    b = c % 2
                    t0 = c * CT
                    P.add("sp", lambda e: e.dma_start(out=u2c[b][:], in_=u2v[:, :, t0:t0 + CT]), writes=[Bu2c[b]], dma_key=("u2c", b))
                    P.add("sp", lambda e: e.dma_start(out=ont[b][:], in_=ONv[:, :, t0:t0 + CT]), writes=[Bont[b]], dma_key=("ont", b))
                    P.add("sp", lambda e: e.dma_start(out=ort[b][:], in_=ORv[:, :, t0:t0 + CT]), writes=[Bort[b]], dma_key=("ort", b))
                    P.add("sp", lambda e: e.dma_start(out=hT[b][:], in_=h1v[:, :, t0:t0 + CT]), writes=Bh[b], dma_key=("h1l", b))

                def mm_group(wsl, Bwsl, nk, mm, rhs_t, Brhs):
                    pg, Bpg = cx.bank()
                    for kt in range(nk):
                        P.add("pe", lambda e, kt=kt: e.matmul(pg[:], lhsT=wsl[:, kt, mm * 128:(mm + 1) * 128], rhs=rhs_t[:, kt, :],
                                                             start=(kt == 0), stop=(kt == nk - 1)),
                              reads=[Bwsl] + Brhs, writes=[Bpg])
                    return pg, Bpg

                load_chunk(0)
                for c in range(NCH):
                    b = c % 2
                    if c + 1 < NCH:
                        load_chunk(c + 1)
                    wi = c * LPC
                    for mh in range(2):
                        wsl, Bwsl = ws.get(wi)
                        for mm in range(4):
                            pg, Bpg = mm_group(wsl, Bwsl, 8, mm, u2c[b], [Bu2c[b]])
                            P.add("act", lambda e, pg=pg, mm=mm: e.activation(out=sga[:, mm, :], in_=pg[:], func=AF.Sigmoid), reads=[Bpg], writes=[Bsga[mm]])
                        wsl, Bwsl = ws.get(wi + 1)
                        for mm in range(4):
                            pg, Bpg = mm_group(wsl, Bwsl, 8, mm, u2c[b], [Bu2c[b]])
                            P.add("act", lambda e, pg=pg, mm=mm: e.activation(out=sgb[:, mm, :], in_=pg[:], func=AF.Sigmoid), reads=[Bpg], writes=[Bsgb[mm]])
                        wsl, Bwsl = ws.get(wi + 2)
                        for mm in range(4):
                            pg, Bpg = mm_group(wsl, Bwsl, 4, mm, ont[b], [Bont[b]])
                            P.add("dve", lambda e, pg=pg, mm=mm: e.tensor_tensor(out=sga[:, mm, :], in0=pg[:], in1=sga[:, mm, :], op=ALU.mult),
                                  reads=[Bpg, Bsga[mm]], writes=[Bsga[mm]])
                        wsl, Bwsl = ws.get(wi + 3)
                        for mm in range(4):
                            pg, Bpg = mm_group(wsl, Bwsl, 4, mm, ort[b], [Bort[b]])
                            P.add("dve", lambda e, pg=pg, mm=mm: e.tensor_tensor(out=sgb[:, mm, :], in0=pg[:], in1=sgb[:, mm, :], op=ALU.mult),
                                  reads=[Bpg, Bsgb[mm]], writes=[Bsgb[mm]])
                            m = mh * 4 + mm
                            P.add("pool", lambda e, m=m, mm=mm: e.tensor_tensor(out=mixed[:, m, :], in0=sga[:, mm, :], in1=sgb[:, mm, :], op=ALU.add),
                                  reads=[Bsga[mm], Bsgb[mm]], writes=[Bmixed[m]])
                        wi += 4
                    for mh in range(2):
                        wsl, Bwsl = ws.get(wi)
                        wi += 1
                        for mm in range(4):
                            m = mh * 4 + mm
                            pg, Bpg = mm_group(wsl, Bwsl, 8, mm, mixed, Bmixed)
                            P.add("dve", lambda e, pg=pg, m=m, b=b: e.scalar_tensor_tensor(out=hT[b][:, m, :], in0=pg[:], scalar=GT2[:, m:m + 1], in1=hT[b][:, m, :],
                                                                                        op0=ALU.mult, op1=ALU.add),
                                  reads=[Bpg, Bh[b][m], B_coef], writes=[Bh[b][m]])
                    rms_mod(cx, P, hT[b], Bh[b], u3, Bu3, G3, SH3, sq, Bsq, rstd, Brstd, tmpf, Btmpf)
                    wi = ffn(cx, P, ws, wi, u3, Bu3, hT[b], Bh[b], hid, Bhid, sa, Bsa, HG3)
                    rms_stats(cx, P, hT[b], Bh[b], sq, Bsq, rstd, Brstd)
                    for kt in range(8):
                        P.add("dve", lambda e, kt=kt, b=b: e.scalar_tensor_tensor(out=hT[b][:, kt, :], in0=hT[b][:, kt, :], scalar=FW[:, kt:kt + 1], in1=rstd[:],
                                                                                 op0=ALU.mult, op1=ALU.mult),
                              reads=[Bh[b][kt], Brstd, B_coef], writes=[Bh[b][kt]])
                    for j in range(4):
                        for a in range(2):
                            pt, Bpt = cx.bank()
                            for q in range(4):
                                kt = a * 4 + q
                                P.add("pe", lambda e, pt=pt, j=j, kt=kt, q=q, b=b: e.transpose(pt[:, q * 128:(q + 1) * 128], hT[b][:, kt, j * 128:(j + 1) * 128], ident_f[:]),
                                      reads=[Bh[b][kt], B_const], writes=[Bpt])
                            dst = ostage[:, j, a * 512:(a + 1) * 512]
                            if (j * 2 + a) % 2 == 0:
                                P.add("act", lambda e, dst=dst, pt=pt: e.copy(out=dst, in_=pt[:]), reads=[Bpt], writes=[Bost])
                            else:
                                P.add("dve", lambda e, dst=dst, pt=pt: e.tensor_copy(out=dst, in_=pt[:]), reads=[Bpt], writes=[Bost])
                    P.add("sp", lambda e, c=c: e.dma_start(out=outv[c], in_=ostage[:]), reads=[Bost], dma_key="ost")
                P.emit(st)
    return nc


def host_tables():
    pos = np.arange(S, dtype=np.float32)
    inv = (10000.0 ** (-np.arange(32, dtype=np.float32) / 32)).astype(np.float32)
    ang = pos[:, None] * inv[None, :]
    tab = np.concatenate([np.cos(ang), np.sin(ang)], axis=1).astype(np.float32)
    rope_tab = np.ascontiguousarray(tab.reshape(32, 128, 64).transpose(1, 0, 2))
    H = 8
    log_g = np.log(1.0 - 2.0 ** (-5.0 - np.arange(H, dtype=np.float64)))
    p = np.arange(128, dtype=np.float64)
    qdec = np.exp(log_g[None, :] * (p[:, None] + 1.0))
    kinv = np.exp(-log_g[None, :] * (p[:, None] + 1.0)) * (64 ** -0.5)
    gC = np.exp(log_g * 128.0)
    gcp = np.zeros((128, 8))
    for pr in range(4):
        gcp[0:64, pr] = gC[2 * pr]
        gcp[64:128, pr] = gC[2 * pr + 1]
    ret_tab = np.concatenate([qdec, kinv, gcp], axis=1).astype(np.float32)
    return rope_tab, ret_tab


def make_in_maps(inputs):
    f = lambda a: np.ascontiguousarray(np.asarray(a, dtype=np.float32))
    colT = lambda v: np.ascontiguousarray(f(v).reshape(-1, 128).T)
    rope_tab, ret_tab = host_tables()
    nwT = np.concatenate([colT(inputs["norm1_w"][0]), colT(inputs["norm2_w"][0]), colT(inputs["norm3_w"][0]), colT(inputs["final_norm_w"])], axis=1)
    shared = {
        "ada_w": f(inputs["ada_w"][0]), "ada_bT": colT(inputs["ada_b"][0]), "nwT": np.ascontiguousarray(nwT),
        "ffn1_w_in": f(inputs["ffn1_w_in"][0]), "ffn1_w_out": f(inputs["ffn1_w_out"][0]), "w_in": f(inputs["w_in"][0]),
        "cmp_k_pe": f(inputs["cmp_k_pe"][0]), "cmp_k_w1": f(inputs["cmp_k_w1"][0]), "cmp_k_w2": f(inputs["cmp_k_w2"][0]),
        "cmp_v_pe": f(inputs["cmp_v_pe"][0]), "cmp_v_w1": f(inputs["cmp_v_w1"][0]), "cmp_v_w2": f(inputs["cmp_v_w2"][0]),
        "ret_norm_w": f(inputs["ret_norm_w"][0]).reshape(1, 512), "w_nsa_up": f(inputs["w_nsa_up"][0]), "w_ret_up": f(inputs["w_ret_up"][0]),
        "w_out": f(inputs["w_out"][0]), "ffn2_w_in": f(inputs["ffn2_w_in"][0]), "ffn2_w_out": f(inputs["ffn2_w_out"][0]),
        "rope_tab": rope_tab, "ret_tab": ret_tab,
    }
    maps = []
    for b in range(8):
        m = dict(shared)
        m["x"] = f(inputs["x"][b])
        m["cT"] = colT(inputs["c"][b])
        maps.append(m)
    return maps


def kernel(**inputs):
    nc = build()
    in_maps = make_in_maps(inputs)
    res = run_bass_kernel_spmd(nc, in_maps, core_ids=list(range(8)))
    return np.stack([np.asarray(r["out"], dtype=np.float32) for r in res.results], axis=0)
```
